# Optimizing a Trainium2 kernel written in Bass

```python
import jax, jax.numpy as jnp
from jax import lax
import numpy as np

D_MODEL = 2048
BATCH = 4
SEQ = 4096
DEPTH = 1

CHUNK = 64
Q_BLOCK = 128
EPS = 1e-6

DN_HEADS = 8
DN_DK = 128
DN_DV = 128
CONV_K = 4
FOX_HEADS = 8
FOX_DH = 128

DN_QK = DN_HEADS * DN_DK
DN_V = DN_HEADS * DN_DV
FOX_W = FOX_HEADS * FOX_DH
MIX_WIDTH = DN_V + FOX_W
CONV_CH = 2 * DN_QK + DN_V
IN_SIZES = (DN_QK, DN_QK, DN_V, DN_V, DN_HEADS, DN_HEADS, FOX_W, FOX_W, FOX_W, FOX_HEADS)
IN_WIDTH = 2 * DN_QK + 2 * DN_V + 2 * DN_HEADS + 3 * FOX_W + FOX_HEADS

N_GROUPS = 4
EXPERTS_PER_GROUP = 8
TOP_K = 2
D_EXPERT = 512

kernel_name = 'hybrid_deltanet_fox_hmoe_block'


def rmsnorm(x, g):
    x32 = x.astype(jnp.float32)
    r = lax.rsqrt(jnp.mean(x32 * x32, axis=-1, keepdims=True) + EPS)
    return (x32 * r).astype(x.dtype) * g


def l2norm(x):
    x32 = x.astype(jnp.float32)
    return x32 * lax.rsqrt(jnp.sum(x32 * x32, axis=-1, keepdims=True) + EPS)


def split_cols(u, sizes):
    points, acc = [], 0
    for s in sizes[:-1]:
        acc += s
        points.append(acc)
    return jnp.split(u, points, axis=-1)


def causal_conv_silu(u, w):
    t = u.shape[1]
    up = jnp.pad(u, ((0, 0), (CONV_K - 1, 0), (0, 0)))
    y = up[:, 0:t] * w[0]
    for j in range(1, CONV_K):
        y = y + up[:, j:j + t] * w[j]
    return jax.nn.silu(y)


def gated_delta_rule(q, k, v, g, beta):
    b, t, h, dk = q.shape
    dv = v.shape[-1]
    nc = t // CHUNK
    f32 = jnp.float32

    def blk(u):
        return u.reshape(b, nc, CHUNK, h, -1).transpose(0, 3, 1, 2, 4).astype(f32)

    qc = blk(q) * (dk ** -0.5)
    kc = blk(k)
    vc = blk(v)
    gch = g.astype(f32).reshape(b, nc, CHUNK, h).transpose(0, 3, 1, 2)
    bch = beta.astype(f32).reshape(b, nc, CHUNK, h).transpose(0, 3, 1, 2)
    gc = jnp.cumsum(gch, axis=-1)
    idx = jnp.arange(CHUNK)
    incl = idx[:, None] >= idx[None, :]
    strict = idx[:, None] > idx[None, :]
    decay = jnp.exp(jnp.where(incl, gc[..., :, None] - gc[..., None, :], -jnp.inf))
    kb = kc * bch[..., None]
    a = jnp.where(strict, jnp.einsum('bhnid,bhnjd->bhnij', kb, kc) * decay, 0.0)
    eye = jnp.broadcast_to(jnp.eye(CHUNK, dtype=f32), a.shape)
    t_inv = lax.linalg.triangular_solve(eye + a, eye, left_side=True, lower=True, unit_diagonal=True)
    u_val = jnp.einsum('bhnij,bhnjd->bhnid', t_inv, vc * bch[..., None])
    w_key = jnp.einsum('bhnij,bhnjd->bhnid', t_inv, kb * jnp.exp(gc)[..., None])
    qk = jnp.where(incl, jnp.einsum('bhnid,bhnjd->bhnij', qc, kc) * decay, 0.0)
    g_last = gc[..., -1:]
    q_dec = qc * jnp.exp(gc)[..., None]
    k_dec = kc * jnp.exp(g_last - gc)[..., None]
    last = jnp.exp(g_last[..., 0])
    xs = tuple(jnp.moveaxis(z, 2, 0) for z in (q_dec, qk, u_val, w_key, k_dec, last))

    def step(s, inp):
        qd, a_in, u_in, w_in, kd, lst = inp
        v_new = u_in - jnp.einsum('bhcd,bhde->bhce', w_in, s)
        o = jnp.einsum('bhcd,bhde->bhce', qd, s) + jnp.einsum('bhij,bhje->bhie', a_in, v_new)
        s = s * lst[..., None, None] + jnp.einsum('bhcd,bhce->bhde', kd, v_new)
        return s, o

    s0 = jnp.zeros((b, h, dk, dv), f32)
    _, o = lax.scan(step, s0, xs)
    return o.transpose(1, 0, 3, 2, 4).reshape(b, t, h, dv)


def forgetting_attention(q, k, v, f_logit):
    b, t, h, d = q.shape
    nb = t // Q_BLOCK
    scale = d ** -0.5
    qh = q.transpose(0, 2, 1, 3)
    kh = k.transpose(0, 2, 1, 3)
    vh = v.transpose(0, 2, 1, 3)
    cum_f = jnp.cumsum(jax.nn.log_sigmoid(f_logit.astype(jnp.float32)), axis=1).transpose(0, 2, 1)
    qb = qh.reshape(b, h, nb, Q_BLOCK, d).transpose(2, 0, 1, 3, 4)
    fb = cum_f.reshape(b, h, nb, Q_BLOCK).transpose(2, 0, 1, 3)
    starts = jnp.arange(nb) * Q_BLOCK
    kpos = jnp.arange(t)

    def one_block(args):
        qi, fi, s0 = args
        logits = jnp.einsum('bhqd,bhkd->bhqk', qi, kh).astype(jnp.float32) * scale
        logits = logits + (fi[..., :, None] - cum_f[..., None, :])
        qpos = s0 + jnp.arange(Q_BLOCK)
        logits = jnp.where(kpos[None, :] <= qpos[:, None], logits, -jnp.inf)
        p = jax.nn.softmax(logits, axis=-1).astype(vh.dtype)
        return jnp.einsum('bhqk,bhkd->bhqd', p, vh)

    o = lax.map(one_block, (qb, fb, starts))
    return o.transpose(1, 0, 3, 2, 4).reshape(b, t, h * d)


def hierarchical_moe(h, w_rg, b_rg, w_re, b_re, w1, w3, w2):
    b, t, d = h.shape
    hf = h.reshape(-1, d)
    n = hf.shape[0]
    gl = (hf @ w_rg + b_rg).astype(jnp.float32)
    gp = jax.nn.softmax(gl, axis=-1)
    _, gidx = lax.top_k(gl, 1)
    pg = jnp.take_along_axis(gp, gidx, axis=1)
    el = (hf @ w_re + b_re).astype(jnp.float32).reshape(n, N_GROUPS, EXPERTS_PER_GROUP)
    el_sel = jnp.take_along_axis(el, gidx[:, :, None], axis=1)[:, 0]
    tv, ti = lax.top_k(el_sel, TOP_K)
    tw = jax.nn.softmax(tv, axis=-1) * pg
    within = jnp.sum(jax.nn.one_hot(ti, EXPERTS_PER_GROUP, dtype=jnp.float32) * tw[..., None], axis=1)
    combine = (jax.nn.one_hot(gidx[:, 0], N_GROUPS, dtype=jnp.float32)[:, :, None] * within[:, None, :]).astype(h.dtype)
    y = jnp.zeros_like(hf)
    for gi in range(N_GROUPS):
        hid = jax.nn.silu(jnp.einsum('nd,edf->nef', hf, w1[gi])) * jnp.einsum('nd,edf->nef', hf, w3[gi])
        y = y + jnp.einsum('nef,efd->nd', hid * combine[:, gi, :, None], w2[gi])
    return y.reshape(b, t, d)


def setup_inputs(seed: int = 0) -> dict:
    key = jax.random.key(seed)
    ks = jax.random.split(key, 24)
    f32 = jnp.float32
    L, D = DEPTH, D_MODEL
    nrm = lambda k_, shape, fan: jax.random.normal(k_, shape, f32) * (fan ** -0.5)
    dt = jax.random.uniform(ks[8], (L, DN_HEADS), f32, 0.001, 0.1)
    return {
        'x': jax.random.normal(ks[0], (BATCH, SEQ, D), f32),
        'c': jax.random.normal(ks[1], (BATCH, D), f32),
        'w_ada': nrm(ks[2], (L, D, 6 * D), D),
        'b_ada': 0.01 * jax.random.normal(ks[3], (L, 6 * D), f32),
        'norm1_g': 1.0 + 0.01 * jax.random.normal(ks[4], (L, D), f32),
        'w_in': nrm(ks[5], (L, D, IN_WIDTH), D),
        'conv_w': nrm(ks[6], (L, CONV_K, CONV_CH), CONV_K),
        'a_log': jnp.log(jax.random.uniform(ks[7], (L, DN_HEADS), f32, 1.0, 16.0)),
        'dt_bias': dt + jnp.log(-jnp.expm1(-dt)),
        'dn_onorm_g': 1.0 + 0.01 * jax.random.normal(ks[9], (L, DN_DV), f32),
        'fox_f_bias': jax.random.uniform(ks[10], (L, FOX_HEADS), f32, 1.0, 3.0),
        'w_out': nrm(ks[11], (L, MIX_WIDTH, D), MIX_WIDTH),
        'norm2_g': 1.0 + 0.01 * jax.random.normal(ks[12], (L, D), f32),
        'w_router_group': nrm(ks[13], (L, D, N_GROUPS), D),
        'b_router_group': 0.01 * jax.random.normal(ks[14], (L, N_GROUPS), f32),
        'w_router_expert': nrm(ks[15], (L, D, N_GROUPS * EXPERTS_PER_GROUP), D),
        'b_router_expert': 0.01 * jax.random.normal(ks[16], (L, N_GROUPS * EXPERTS_PER_GROUP), f32),
        'w1': nrm(ks[17], (L, N_GROUPS, EXPERTS_PER_GROUP, D, D_EXPERT), D),
        'w3': nrm(ks[18], (L, N_GROUPS, EXPERTS_PER_GROUP, D, D_EXPERT), D),
        'w2': nrm(ks[19], (L, N_GROUPS, EXPERTS_PER_GROUP, D_EXPERT, D), D_EXPERT),
        'final_g': 1.0 + 0.01 * jax.random.normal(ks[20], (D,), f32),
    }


def reference(x, c, w_ada, b_ada, norm1_g, w_in, conv_w, a_log, dt_bias, dn_onorm_g, fox_f_bias,
              w_out, norm2_g, w_router_group, b_router_group, w_router_expert, b_router_expert,
              w1, w3, w2, final_g):
    b, t, _ = x.shape
    for l in range(DEPTH):
        mod = jax.nn.silu(c) @ w_ada[l] + b_ada[l]
        sh1, sc1, g1, sh2, sc2, g2 = jnp.split(mod[:, None, :], 6, axis=-1)

        h = rmsnorm(x, norm1_g[l]) * (1.0 + sc1) + sh1
        proj = h @ w_in[l]
        q_a, k_a, v_a, z_a, a_a, b_a, q_b, k_b, v_b, f_b = split_cols(proj, IN_SIZES)

        qkv = causal_conv_silu(jnp.concatenate([q_a, k_a, v_a], axis=-1), conv_w[l])
        q_a, k_a, v_a = split_cols(qkv, (DN_QK, DN_QK, DN_V))
        q_a = l2norm(q_a.reshape(b, t, DN_HEADS, DN_DK))
        k_a = l2norm(k_a.reshape(b, t, DN_HEADS, DN_DK))
        v_a = v_a.reshape(b, t, DN_HEADS, DN_DV)
        g_dec = -jnp.exp(a_log[l].astype(jnp.float32)) * jax.nn.softplus(a_a.astype(jnp.float32) + dt_bias[l])
        beta = jax.nn.sigmoid(b_a.astype(jnp.float32))
        o_a = gated_delta_rule(q_a, k_a, v_a, g_dec, beta).astype(x.dtype)
        o_a = rmsnorm(o_a, dn_onorm_g[l]) * jax.nn.silu(z_a.reshape(b, t, DN_HEADS, DN_DV))
        o_a = o_a.reshape(b, t, DN_V)

        o_b = forgetting_attention(q_b.reshape(b, t, FOX_HEADS, FOX_DH),
                                   k_b.reshape(b, t, FOX_HEADS, FOX_DH),
                                   v_b.reshape(b, t, FOX_HEADS, FOX_DH),
                                   f_b + fox_f_bias[l])

        mix = jnp.concatenate([o_a, o_b.astype(x.dtype)], axis=-1) @ w_out[l]
        x = x + g1 * mix

        h2 = rmsnorm(x, norm2_g[l]) * (1.0 + sc2) + sh2
        x = x + g2 * hierarchical_moe(h2, w_router_group[l], b_router_group[l],
                                      w_router_expert[l], b_router_expert[l],
                                      w1[l], w3[l], w2[l])
    return rmsnorm(x, final_g)
```

```python
from contextlib import ExitStack
import numpy as np
import concourse.bass as bass
import concourse.mybir as mybir
from concourse.bass_utils import run_bass_kernel_spmd

F32 = mybir.dt.float32
BF16 = mybir.dt.bfloat16
I32 = mybir.dt.int32
AF = mybir.ActivationFunctionType
ALU = mybir.AluOpType
AX = mybir.AxisListType

D = 2048
KC = 16
H = 8
BLK = 256
NE = 32
EPS = 1e-6
IN_W = 7192
C_QA, C_KA, C_VA, C_ZA, C_AA, C_BA, C_QB, C_KB, C_VB, C_FB = 0, 1024, 2048, 3072, 4096, 4104, 4112, 5136, 6160, 7184


class Trk:
    __slots__ = ("w", "r", "ps")

    def __init__(self, ps=False):
        self.w = None
        self.r = {}
        self.ps = ps


class Sched:
    def __init__(self, nc):
        self.nc = nc
        self.eng = {"pe": nc.tensor, "act": nc.scalar, "dve": nc.vector, "pool": nc.gpsimd, "sp": nc.sync}
        self.sems, self.cnt, self.isdma = {}, {}, {}
        self.seen = {e: {} for e in self.eng}
        for e in self.eng:
            self.newsem(e, False)
        self.nops = 0

    def newsem(self, key, isdma=True):
        self.sems[key] = self.nc.alloc_semaphore(name=f"s_{key}")
        self.cnt[key] = 0
        self.isdma[key] = isdma
        return key

    def _wait(self, e, key, val):
        if self.isdma[key]:
            val = self.cnt[key]
        if self.seen[e].get(key, 0) >= val:
            return
        self.eng[e].wait_ge(self.sems[key], val)
        self.seen[e][key] = val

    def op(self, e, fn, reads=(), writes=(), inc=True, dma=None):
        deps = {}

        def add(k, v):
            if deps.get(k, 0) < v:
                deps[k] = v
        for t in reads:
            if t.w is not None:
                k, v = t.w
                if not (k == e and e == "pe"):
                    add(k, v)
            if t.ps:
                for k, v in t.r.items():
                    if k != e:
                        add(k, v)
        for t in writes:
            if t.w is not None and not (t.w[0] == e and dma is None):
                add(*t.w)
            for k, v in t.r.items():
                if k == e and dma is None:
                    continue
                add(k, v)
        for k, v in deps.items():
            self._wait(e, k, v)
        ins = fn(self.eng[e])
        self.nops += 1
        if dma is not None:
            self.cnt[dma] += 16
            ins.then_inc(self.sems[dma], 16)
            tk = (dma, self.cnt[dma])
        elif inc:
            self.cnt[e] += 1
            ins.then_inc(self.sems[e], 1)
            tk = (e, self.cnt[e])
        else:
            tk = (e, self.cnt[e] + 1)
        for t in reads:
            if t.r.get(tk[0], 0) < tk[1]:
                t.r[tk[0]] = tk[1]
        for t in writes:
            t.w = tk
            t.r = {}
        return tk

    def barrier(self):
        for e in self.eng:
            for k in self.sems:
                if k != e and self.cnt[k] > 0:
                    v = self.cnt[k]
                    if self.seen[e].get(k, 0) < v:
                        self.eng[e].wait_ge(self.sems[k], v)
                        self.seen[e][k] = v


_STK = [None]


def _alloc_sb(nc, name, shape, dtype):
    return _STK[0].enter_context(nc.sbuf_tensor(name, list(shape), dtype))


class Ring:
    def __init__(self, S, nc, name, shape, dtype, n, dma=False):
        self.t = [_alloc_sb(nc, f"{name}{i}", shape, dtype) for i in range(n)]
        self.k = [Trk() for _ in range(n)]
        self.d = [S.newsem(f"d_{name}{i}") for i in range(n)] if dma else [None] * n
        self.i = 0
        self.n = n

    def next(self):
        j = self.i % self.n
        self.i += 1
        return self.t[j], self.k[j], self.d[j]


class PsPool:
    def __init__(self, nc, n):
        self.t = [nc.alloc_psum_tensor(f"ps{i}", [128, 512], F32) for i in range(n)]
        self.k = [Trk(ps=True) for _ in range(n)]
        self.busy = [False] * n
        self.i = 0
        self.n = n

    def get(self):
        for _ in range(self.n):
            j = self.i % self.n
            self.i += 1
            if not self.busy[j]:
                self.busy[j] = True
                return j
        raise RuntimeError("out of PSUM banks")

    def rel(self, j):
        self.busy[j] = False


class StopBuild(Exception):
    pass


_LAST = {}


def build(NP, NO, dbg=None, stop=None):
    def chk(n):
        if stop == n:
            finalize()
            _LAST["nc"] = nc
            raise StopBuild()
    nc = bass.Bass("TRN2", target_bir_lowering=False)
    S = Sched(nc)

    def finalize():
        for k in S.sems:
            if S.isdma[k] and S.cnt[k] > 0:
                S.eng["sp"].wait_ge(S.sems[k], S.cnt[k])
        S.barrier()
    T = NP + NO
    NBP, NBO = NP // BLK, NO // BLK
    NB = NBP + NBO
    NKT = T // 128
    NTO = NO // 128

    def din(name, shape, dt=F32):
        return nc.dram_tensor(name, list(shape), dt, kind="ExternalInput").ap()

    xp = din("xp", [NP, D]); xo = din("xo", [NO, D]); cT = din("cT", [128, KC])
    pmv = din("pmv", [128, 2])
    w_ada = din("w_ada", [D, 6 * D]); b_adaT = din("b_adaT", [128, 96]); n1g = din("n1g", [128, KC])
    w_in = din("w_in", [D, IN_W]); convw = din("convw", [128, 24 * 4])
    alog = din("alog", [128, H]); dtb = din("dtb", [128, H]); ong = din("ong", [128, 1]); ffb = din("ffb", [128, H])
    w_out = din("w_out", [D, D]); n2g = din("n2g", [128, KC])
    w_r = din("w_r", [D, 36]); b_r = din("b_r", [128, 36])
    w1 = din("w1", [NE, D, 512]); w3 = din("w3", [NE, D, 512]); w2 = din("w2", [NE, 512, D])
    fgb = din("fgb", [128, D])
    lvlm = din("lvlm", [128, 8 * 128])
    out = nc.dram_tensor("out", [NO, D], F32, kind="ExternalOutput").ap()
    dbg_t = None
    if dbg:
        dbg_t = {k: nc.dram_tensor("dbg_" + k, list(shp), F32, kind="ExternalOutput").ap() for k, shp in dbg.items()}

    win_d = nc.dram_tensor("win_d", [D, IN_W], BF16, kind="Internal").ap()
    kt_d = nc.dram_tensor("kt_d", [H, 128, T], BF16, kind="Internal").ap()
    v_d = nc.dram_tensor("v_d", [T, H * 128], BF16, kind="Internal").ap()
    x1_d = nc.dram_tensor("x1_d", [NO, D], F32, kind="Internal").ap()
    h2_d = nc.dram_tensor("h2_d", [KC, 128, NO], BF16, kind="Internal").ap()
    k_win = Trk(); k_ktd = [Trk() for _ in range(NB)]; k_vd = [Trk() for _ in range(NB)]
    k_x1d = [Trk() for _ in range(NTO)]; k_h2d = [Trk() for _ in range(NBO)]

    stack_main = ExitStack()
    stack_B = ExitStack()
    _STK[0] = stack_main

    def sb(name, shape, dt=F32):
        return _alloc_sb(nc, name, shape, dt)

    def op(e, fn, r=(), w=(), inc=True, dma=None):
        return S.op(e, fn, r, w, inc, dma)

    PS = PsPool(nc, 8)

    ident_f = sb("ident_f", [128, 128]); ident_b = sb("ident_b", [128, 128], BF16)
    ones_f = sb("ones_f", [128, 128]); ones_b = sb("ones_b", [128, 128], BF16)
    utri = sb("utri", [128, 128])
    m_posL = sb("m_posL", [128, 128])
    m_negUs = sb("m_negUs", [128, 128])
    m_negUi = sb("m_negUi", [128, 128])
    sel = sb("sel", [24, H * 128], BF16)
    iot = sb("iot", [24, 128], I32); iotf = sb("iotf", [24, 128])
    kc = Trk()
    op("pool", lambda e: e.memset(ident_f[:], 0.0), w=[kc])
    op("pool", lambda e: e.affine_select(out=ident_f[:], in_=ident_f[:], pattern=[[-1, 128]], compare_op=ALU.not_equal, fill=1.0, base=0, channel_multiplier=1), r=[kc], w=[kc])
    op("pool", lambda e: e.tensor_copy(out=ident_b[:], in_=ident_f[:]), r=[kc], w=[kc])
    op("pool", lambda e: e.memset(ones_f[:], 1.0), w=[kc])
    op("pool", lambda e: e.memset(ones_b[:], 1.0), w=[kc])
    op("pool", lambda e: e.affine_select(out=utri[:], in_=ones_f[:], pattern=[[1, 128]], compare_op=ALU.is_ge, fill=0.0, base=0, channel_multiplier=-1), r=[kc], w=[kc])
    op("pool", lambda e: e.memset(m_posL[:], 0.0), w=[kc])
    op("pool", lambda e: e.affine_select(out=m_posL[:], in_=m_posL[:], pattern=[[-1, 128]], compare_op=ALU.is_ge, fill=1.0e4, base=-1, channel_multiplier=1), r=[kc], w=[kc])
    op("pool", lambda e: e.memset(m_negUs[:], 0.0), w=[kc])
    op("pool", lambda e: e.affine_select(out=m_negUs[:], in_=m_negUs[:], pattern=[[1, 128]], compare_op=ALU.is_ge, fill=-1.0e4, base=-1, channel_multiplier=-1), r=[kc], w=[kc])
    op("pool", lambda e: e.memset(m_negUi[:], 0.0), w=[kc])
    op("pool", lambda e: e.affine_select(out=m_negUi[:], in_=m_negUi[:], pattern=[[1, 128]], compare_op=ALU.is_ge, fill=-1.0e4, base=0, channel_multiplier=-1), r=[kc], w=[kc])
    op("pool", lambda e: e.iota(iot[:], pattern=[[0, 128]], base=0, channel_multiplier=1), w=[kc])
    op("pool", lambda e: e.tensor_copy(out=iotf[:], in_=iot[:]), r=[kc], w=[kc])
    for h in range(H):
        op("dve", lambda e: e.tensor_scalar(out=iot[:].bitcast(F32), in0=iotf[:], scalar1=float(-h), scalar2=None, op0=ALU.add), r=[kc], w=[kc])
        tmpf = iot[:].bitcast(F32)
        op("dve", lambda e: e.scalar_tensor_tensor(out=iotf[:], in0=tmpf, scalar=-8.0, in1=tmpf, op0=ALU.add, op1=ALU.mult), r=[kc], w=[kc])
        op("dve", lambda e: e.scalar_tensor_tensor(out=iotf[:], in0=tmpf, scalar=-16.0, in1=iotf[:], op0=ALU.add, op1=ALU.mult), r=[kc], w=[kc])
        op("dve", lambda e: e.tensor_scalar(out=sel[:, h * 128:(h + 1) * 128], in0=iotf[:], scalar1=0.0, scalar2=None, op0=ALU.is_equal), r=[kc], w=[kc])
        op("dve", lambda e: e.tensor_scalar(out=iotf[:], in0=tmpf, scalar1=float(h), scalar2=None, op0=ALU.add), r=[kc], w=[kc])

    def load_small(name, src, shape, dt=F32):
        t = sb(name, shape, dt)
        k = Trk()
        sem = S.newsem("d_" + name)
        op("sp", lambda e: e.dma_start(out=t[:], in_=src), w=[k], dma=sem)
        return t, k

    cT_s, k_cT = load_small("cT_s", cT, [128, KC])
    pm_s, k_pm = load_small("pm_s", pmv, [128, 2])
    bada_s, k_bada = load_small("bada_s", b_adaT, [128, 96])
    n1g_s, k_n1g = load_small("n1g_s", n1g, [128, KC])
    n2g_s, k_n2g = load_small("n2g_s", n2g, [128, KC])
    convw_s, k_convw = load_small("convw_s", convw, [128, 96])
    alog_s, k_alog = load_small("alog_s", alog, [128, H])
    dtb_s, k_dtb = load_small("dtb_s", dtb, [128, H])
    ong_s, k_ong = load_small("ong_s", ong, [128, 1])
    ffb_s, k_ffb = load_small("ffb_s", ffb, [128, H])
    br_s, k_br = load_small("br_s", b_r, [128, 36])

    LM = sb("LM", [128, 8, 128], BF16); k_lm = Trk(); sem_lm = S.newsem("d_lm")
    op("pool", lambda e: e.dma_start(out=LM[:].rearrange("p a b -> p (a b)"), in_=lvlm), w=[k_lm], dma=sem_lm)
    sem_win = S.newsem("d_win")
    for i in range(4):
        op("pool", lambda e: e.dma_start(out=win_d[i * 512:(i + 1) * 512, :], in_=w_in[i * 512:(i + 1) * 512, :]), w=[k_win], dma=sem_win)
    wr_s = sb("wr_s", [128, KC, 36], BF16); k_wr = Trk(); sem_wr = S.newsem("d_wr")
    op("pool", lambda e: e.dma_start(out=wr_s[:], in_=w_r.rearrange("(c p) f -> p c f", p=128)), w=[k_wr], dma=sem_wr)
    wsm = sb("wsm", [128, KC, 24], BF16); k_wsm = Trk(); sem_wsm = S.newsem("d_wsm")
    op("pool", lambda e: e.dma_start(out=wsm[:, :, 0:16], in_=w_in[:, C_AA:C_AA + 16].rearrange("(c p) f -> p c f", p=128)), w=[k_wsm], dma=sem_wsm)
    op("pool", lambda e: e.dma_start(out=wsm[:, :, 16:24], in_=w_in[:, C_FB:C_FB + 8].rearrange("(c p) f -> p c f", p=128)), w=[k_wsm], dma=sem_wsm)

    chk(0)
    sc_b = sb("sc_b", [128, KC], BF16); k_sc = Trk()
    tA = sb("tA", [128, KC]); k_tA = Trk()
    mod = sb("mod", [128, 96]); k_mod = Trk()
    vec = sb("vec", [128, 6 * KC]); k_vec = Trk()
    G1B = sb("G1B", [128, D]); G2B = sb("G2B", [128, D]); k_G = Trk()
    dg = Ring(S, nc, "dg", [128, 128], F32, 2)
    nalog = sb("nalog", [128, H]); k_nalog = Trk()
    stat = Ring(S, nc, "stat", [128, 4], F32, 4)
    CW = sb("CW", [128, NTO, 32]); k_cw = [Trk() for _ in range(NTO)]
    _STK[0] = stack_B
    UW = 256
    WR = Ring(S, nc, "wring", [128, KC, UW], BF16, 3, dma=True)
    op("act", lambda e: e.activation(out=tA[:], in_=cT_s[:], func=AF.Exp, scale=-1.0), r=[k_cT], w=[k_tA])
    op("dve", lambda e: e.tensor_scalar(out=tA[:], in0=tA[:], scalar1=1.0, scalar2=None, op0=ALU.add), r=[k_tA], w=[k_tA])
    op("dve", lambda e: e.reciprocal(out=tA[:], in_=tA[:]), r=[k_tA], w=[k_tA])
    op("dve", lambda e: e.tensor_tensor(out=sc_b[:], in0=tA[:], in1=cT_s[:], op=ALU.mult), r=[k_tA, k_cT], w=[k_sc])
    pmod = PS.get()
    for u in range(48):
        wt, wk, wd = WR.next()
        op("pool", lambda e: e.dma_start(out=wt[:], in_=w_ada[:, u * 256:(u + 1) * 256].rearrange("(c p) f -> p c f", p=128)), w=[wk], dma=wd)
        for j in range(2):
            oc = u * 2 + j
            for k in range(KC):
                op("pe", lambda e: e.matmul(PS.t[pmod][:, oc:oc + 1], lhsT=wt[:, k, j * 128:(j + 1) * 128], rhs=sc_b[:, k:k + 1], start=(k == 0), stop=(k == KC - 1)),
                   r=[wk, k_sc], w=[PS.k[pmod]], inc=(k == KC - 1))
    op("dve", lambda e: e.tensor_tensor(out=mod[:], in0=PS.t[pmod][:, 0:96], in1=bada_s[:], op=ALU.add), r=[PS.k[pmod], k_bada], w=[k_mod])
    PS.rel(pmod)
    A1, B1, A1p, B1p, A2, B2 = [vec[:, i * KC:(i + 1) * KC] for i in range(6)]
    op("dve", lambda e: e.scalar_tensor_tensor(out=A1, in0=mod[:, 16:32], scalar=1.0, in1=n1g_s[:], op0=ALU.add, op1=ALU.mult), r=[k_mod, k_n1g], w=[k_vec])
    op("dve", lambda e: e.tensor_copy(out=B1, in_=mod[:, 0:16]), r=[k_mod], w=[k_vec])
    op("dve", lambda e: e.tensor_scalar(out=A1p, in0=A1, scalar1=pm_s[:, 0:1], scalar2=None, op0=ALU.mult), r=[k_vec, k_pm], w=[k_vec])
    op("dve", lambda e: e.tensor_scalar(out=B1p, in0=B1, scalar1=pm_s[:, 0:1], scalar2=None, op0=ALU.mult), r=[k_vec, k_pm], w=[k_vec])
    op("dve", lambda e: e.scalar_tensor_tensor(out=A2, in0=mod[:, 64:80], scalar=1.0, in1=n2g_s[:], op0=ALU.add, op1=ALU.mult), r=[k_mod, k_n2g], w=[k_vec])
    op("dve", lambda e: e.tensor_copy(out=B2, in_=mod[:, 48:64]), r=[k_mod], w=[k_vec])
    for gi, (GB, off) in enumerate(((G1B, 32), (G2B, 80))):
        for q4 in range(4):
            pb = PS.get()
            for j in range(4):
                c = q4 * 4 + j
                dt_, dk_, _ = dg.next()
                op("dve", lambda e: e.tensor_scalar(out=dt_[:], in0=ident_f[:], scalar1=mod[:, off + c:off + c + 1], scalar2=None, op0=ALU.mult), r=[kc, k_mod], w=[dk_])
                op("pe", lambda e: e.matmul(PS.t[pb][:, j * 128:(j + 1) * 128], lhsT=ones_f[:], rhs=dt_[:], start=True, stop=True), r=[kc, dk_], w=[PS.k[pb]])
            op("act", lambda e: e.copy(out=GB[:, q4 * 512:(q4 + 1) * 512], in_=PS.t[pb][:]), r=[PS.k[pb]], w=[k_G])
            PS.rel(pb)
    op("act", lambda e: e.activation(out=nalog[:], in_=alog_s[:], func=AF.Exp), r=[k_alog], w=[k_nalog])
    op("dve", lambda e: e.tensor_scalar(out=nalog[:], in0=nalog[:], scalar1=-1.0, scalar2=None, op0=ALU.mult), r=[k_nalog], w=[k_nalog])

    chk(1)
    XT = Ring(S, nc, "xtile", [128, D], F32, 2, dma=True)
    XN = Ring(S, nc, "xn", [128, D], BF16, 1)
    hT = sb("hT", [128, KC, BLK], BF16); k_hT = Trk()
    mixT = sb("mixT", [128, KC, BLK], BF16); k_mix = [Trk() for _ in range(KC)]
    h2T = sb("h2T", [128, KC, BLK], BF16); k_h2T = Trk(); sem_h2 = S.newsem("d_h2T")
    UB = Ring(S, nc, "ub", [128, BLK + 3], F32, 2)
    CA = Ring(S, nc, "ca", [128, BLK], F32, 2)
    hist = sb("hist", [128, 24, 3]); k_hist = [Trk() for _ in range(24)]
    op("pool", lambda e: e.memset(hist[:], 0.0), w=k_hist)
    QKV = sb("QKV", [128, 24, BLK], BF16); k_qkv = [Trk() for _ in range(24)]
    ZS = sb("ZS", [128, 8, BLK], BF16); k_zs = [Trk() for _ in range(8)]
    QbT = sb("QbT", [128, 8, BLK], BF16); k_qb = [Trk() for _ in range(8)]
    KbT = sb("KbT", [128, 8, BLK], BF16); k_kb = Trk(); sem_kb = S.newsem("d_kb")
    VbT = Ring(S, nc, "vbt", [128, 1024], BF16, 2, dma=True)
    FK = sb("FK", [128, NKT, H]); k_fk = [Trk() for _ in range(NKT)]
    Fcar = sb("Fcar", [128, H]); k_fcar = Trk()
    op("pool", lambda e: e.memset(Fcar[:], 0.0), w=[k_fcar])
    FqT = sb("FqT", [24, BLK], BF16); k_fq = Trk()
    Sst = sb("Sst", [128, H, 128]); Sbf = sb("Sbf", [128, H, 128], BF16); k_S = [Trk() for _ in range(H)]; k_Sb = [Trk() for _ in range(H)]
    op("pool", lambda e: e.memset(Sst[:], 0.0), w=k_S)
    op("pool", lambda e: e.memset(Sbf[:], 0.0), w=k_Sb)
    GT = Ring(S, nc, "gt", [128, 64], F32, 2)
    GC = Ring(S, nc, "gc", [128, 8 * H], F32, 4)
    T32 = Ring(S, nc, "t32", [128, 128], F32, 6)
    NSLOT = 8
    BFN = ["X", "Y", "qkT", "qdT", "TmA", "TmB", "TtA", "TtB", "Ul", "W1s", "ke", "kd", "vtk", "wT", "vn"]
    SL = []
    for hs in range(NSLOT):
        d_ = {nm: (sb(f"sl{hs}_{nm}", [128, 128], BF16), Trk()) for nm in BFN}
        d_["u2"] = (sb(f"sl{hs}_u2", [128, 128], F32), Trk())
        SL.append(d_)
    f3t = sb("f3t", [128, 24], BF16); k_f3 = Trk()
    r3t = sb("r3t", [128, 16], F32); k_r3 = Trk()
    OTB = sb("OTB", [128, H, BLK], F32); k_ot = [Trk() for _ in range(H)]
    W32 = Ring(S, nc, "w32", [128, BLK], F32, 4)
    W16 = Ring(S, nc, "w16", [128, BLK], BF16, 4)
    KR = Ring(S, nc, "kr", [128, 1024], BF16, 2, dma=True)
    VR = Ring(S, nc, "vr", [128, 8, 128], BF16, 2, dma=True)
    XP = Ring(S, nc, "xpc", [128, 256], F32, 2, dma=True)
    X1P = Ring(S, nc, "x1p", [128, 256], F32, 2, dma=True)
    RT = Ring(S, nc, "rt", [128, 64], F32, 2)

    def dbg_dump(name, src_ap, trk):
        if dbg_t is None or name not in dbg_t:
            return
        sem = S.newsem("d_dbg_" + name + str(S.nops))
        op("pool" if src_ap.dtype != F32 else "sp", lambda e: e.dma_start(out=dbg_t[name], in_=src_ap), r=trk, dma=sem)
        S.eng["sp"].wait_ge(S.sems[sem], S.cnt[sem])

    def rstd_from_ssq(st, sk, col_in, col_out, n):
        op("act", lambda e: e.activation(out=st[:, col_out:col_out + 1], in_=st[:, col_in:col_in + 1], func=AF.Ln, scale=1.0 / n, bias=EPS), r=[sk], w=[sk])
        op("act", lambda e: e.activation(out=st[:, col_out:col_out + 1], in_=st[:, col_out:col_out + 1], func=AF.Exp, scale=-0.5), r=[sk], w=[sk])

    def norm_to_T(src_rows, dst, dst_k, Av, Bv, x_from=None):
        for t in range(BLK // 128):
            xt, xk, xd = XT.next()
            op("sp", lambda e: e.dma_start(out=xt[:], in_=src_rows[t * 128:(t + 1) * 128, :]), r=(x_from or ()), w=[xk], dma=xd)
            st, sk, _ = stat.next()
            xn, nk, _ = XN.next()
            op("act", lambda e: e.activation(out=xn[:], in_=xt[:], func=AF.Square, accum_out=st[:, 0:1]), r=[xk], w=[nk, sk])
            rstd_from_ssq(st, sk, 0, 1, D)
            op("dve", lambda e: e.tensor_scalar(out=xn[:], in0=xt[:], scalar1=st[:, 1:2], scalar2=None, op0=ALU.mult), r=[xk, sk], w=[nk])
            for c4 in range(4):
                pb = PS.get()
                pv = PS.t[pb][:].bitcast(BF16)
                for j in range(4):
                    c = c4 * 4 + j
                    op("pe", lambda e: e.transpose(out=pv[:, j * 128:(j + 1) * 128], in_=xn[:, c * 128:(c + 1) * 128], identity=ident_b[:]), r=[nk, kc], w=[PS.k[pb]])
                for j in range(4):
                    c = c4 * 4 + j
                    eng = "act" if c4 % 2 == 0 else "dve"
                    if eng == "act":
                        op("act", lambda e: e.activation(out=dst[:, c, t * 128:(t + 1) * 128], in_=pv[:, j * 128:(j + 1) * 128], func=AF.Identity, scale=Av[:, c:c + 1], bias=Bv[:, c:c + 1]),
                           r=[PS.k[pb], k_vec], w=[dst_k])
                    else:
                        op("dve", lambda e: e.tensor_scalar(out=dst[:, c, t * 128:(t + 1) * 128], in0=pv[:, j * 128:(j + 1) * 128], scalar1=Av[:, c:c + 1], scalar2=Bv[:, c:c + 1], op0=ALU.mult, op1=ALU.add),
                           r=[PS.k[pb], k_vec], w=[dst_k])
                PS.rel(pb)

    wr_sp_sem = {}

    def load_unit(col0, ncols=256, src=None):
        wt, wk, wd = WR.next()
        if src is None:
            if wd not in wr_sp_sem:
                wr_sp_sem[wd] = S.newsem(wd + "_sp")
            wd = wr_sp_sem[wd]
            op("sp", lambda e: e.dma_start(out=wt[:, :, 0:ncols], in_=win_d[:, col0:col0 + ncols].rearrange("(c p) f -> p c f", p=128)), r=[k_win], w=[wk], dma=wd)
        else:
            op("pool", lambda e: e.dma_start(out=wt[:, :, 0:ncols], in_=src[:, col0:col0 + ncols].rearrange("(c p) f -> p c f", p=128)), w=[wk], dma=wd)
        return wt, wk

    def proj_fm(wt, wk, j):
        pb = PS.get()
        for k in range(KC):
            op("pe", lambda e: e.matmul(PS.t[pb][:, 0:BLK], lhsT=wt[:, k, j * 128:(j + 1) * 128], rhs=hT[:, k, :], start=(k == 0), stop=(k == KC - 1)),
               r=[wk, k_hT], w=[PS.k[pb]], inc=(k == KC - 1))
        return pb

    def silu_from(src_ap, src_k, dst_ap, dst_k):
        op("act", lambda e: e.activation(out=dst_ap, in_=src_ap, func=AF.Silu), r=src_k, w=dst_k)

    for blk in range(NB):
        own = blk >= NBP
        tok0 = blk * BLK
        src = (xo[(blk - NBP) * BLK:(blk - NBP + 1) * BLK, :] if own else xp[blk * BLK:(blk + 1) * BLK, :])
        norm_to_T(src, hT, k_hT, A1 if own else A1p, B1 if own else B1p)

        chk(2)
        gres = []
        for t in range(BLK // 128):
            kt = blk * 2 + t
            pb = PS.get()
            for k in range(KC):
                op("pe", lambda e: e.matmul(PS.t[pb][:, 0:24], lhsT=hT[:, k, t * 128:(t + 1) * 128], rhs=wsm[:, k, :], start=(k == 0), stop=(k == KC - 1)),
                   r=[k_hT, k_wsm], w=[PS.k[pb]], inc=(k == KC - 1))
            g, gk, _ = GT.next()
            R, Rk, _ = GC.next()
            P = PS.t[pb]
            op("dve", lambda e: e.tensor_tensor(out=g[:, 0:8], in0=P[:, 0:8], in1=dtb_s[:], op=ALU.add), r=[PS.k[pb], k_dtb], w=[gk])
            op("act", lambda e: e.activation(out=g[:, 0:8], in_=g[:, 0:8], func=AF.Exp), r=[gk], w=[gk])
            op("act", lambda e: e.activation(out=g[:, 0:8], in_=g[:, 0:8], func=AF.Ln, bias=1.0), r=[gk], w=[gk])
            op("dve", lambda e: e.tensor_tensor(out=R[:, 0:8], in0=g[:, 0:8], in1=nalog[:], op=ALU.mult), r=[gk, k_nalog], w=[Rk])
            op("act", lambda e: e.activation(out=g[:, 8:16], in_=P[:, 8:16], func=AF.Exp, scale=-1.0), r=[PS.k[pb]], w=[gk])
            op("act", lambda e: e.activation(out=g[:, 8:16], in_=g[:, 8:16], func=AF.Ln, bias=1.0), r=[gk], w=[gk])
            op("dve", lambda e: e.tensor_scalar(out=R[:, 8:16], in0=g[:, 8:16], scalar1=-1.0, scalar2=None, op0=ALU.mult), r=[gk], w=[Rk])
            op("act", lambda e: e.activation(out=R[:, 16:24], in_=g[:, 8:16], func=AF.Exp, scale=-1.0), r=[gk], w=[Rk])
            op("dve", lambda e: e.tensor_scalar(out=R[:, 24:32], in0=R[:, 16:24], scalar1=-1.0, scalar2=None, op0=ALU.mult), r=[Rk], w=[Rk])
            op("dve", lambda e: e.tensor_tensor(out=g[:, 16:24], in0=P[:, 16:24], in1=ffb_s[:], op=ALU.add), r=[PS.k[pb], k_ffb], w=[gk])
            op("act", lambda e: e.activation(out=g[:, 16:24], in_=g[:, 16:24], func=AF.Exp, scale=-1.0), r=[gk], w=[gk])
            op("act", lambda e: e.activation(out=g[:, 16:24], in_=g[:, 16:24], func=AF.Ln, bias=1.0), r=[gk], w=[gk])
            op("dve", lambda e: e.tensor_scalar(out=g[:, 16:24], in0=g[:, 16:24], scalar1=-1.0, scalar2=None, op0=ALU.mult), r=[gk], w=[gk])
            PS.rel(pb)
            pc = PS.get()
            Pc = PS.t[pc]
            op("pe", lambda e: e.matmul(Pc[:, 0:8], lhsT=utri[:], rhs=R[:, 0:8], start=True, stop=True), r=[kc, Rk], w=[PS.k[pc]])
            op("pe", lambda e: e.matmul(Pc[:, 8:16], lhsT=utri[:], rhs=g[:, 16:24], start=True, stop=True), r=[kc, gk], w=[PS.k[pc]])
            op("pe", lambda e: e.matmul(Pc[:, 16:24], lhsT=ones_f[:], rhs=R[:, 0:8], start=True, stop=True), r=[kc, Rk], w=[PS.k[pc]])
            op("pe", lambda e: e.matmul(Pc[:, 24:32], lhsT=ones_f[:], rhs=g[:, 16:24], start=True, stop=True), r=[kc, gk], w=[PS.k[pc]])
            op("dve", lambda e: e.tensor_copy(out=R[:, 32:40], in_=Pc[:, 0:8]), r=[PS.k[pc]], w=[Rk])
            op("dve", lambda e: e.tensor_tensor(out=R[:, 40:48], in0=Pc[:, 0:8], in1=R[:, 8:16], op=ALU.subtract), r=[PS.k[pc], Rk], w=[Rk])
            op("act", lambda e: e.activation(out=R[:, 48:56], in_=Pc[:, 0:8], func=AF.Exp), r=[PS.k[pc]], w=[Rk])
            op("dve", lambda e: e.tensor_tensor(out=g[:, 24:32], in0=Pc[:, 16:24], in1=R[:, 32:40], op=ALU.subtract), r=[PS.k[pc], Rk], w=[gk])
            op("act", lambda e: e.activation(out=R[:, 56:64], in_=g[:, 24:32], func=AF.Exp), r=[gk], w=[Rk])
            op("act", lambda e: e.activation(out=g[:, 32:40], in_=Pc[:, 16:24], func=AF.Exp), r=[PS.k[pc]], w=[gk])
            op("dve", lambda e: e.tensor_tensor(out=g[:, 40:48], in0=Pc[:, 8:16], in1=Fcar[:], op=ALU.add), r=[PS.k[pc], k_fcar], w=[gk])
            if own:
                op("dve", lambda e: e.tensor_scalar(out=FK[:, kt, :], in0=g[:, 40:48], scalar1=-1.0, scalar2=None, op0=ALU.mult), r=[gk], w=[k_fk[kt]])
            else:
                op("dve", lambda e: e.tensor_scalar(out=FK[:, kt, :], in0=g[:, 40:48], scalar1=-1.0, scalar2=pm_s[:, 1:2], op0=ALU.mult, op1=ALU.add), r=[gk, k_pm], w=[k_fk[kt]])
            op("dve", lambda e: e.tensor_tensor(out=Fcar[:], in0=Fcar[:], in1=Pc[:, 24:32], op=ALU.add), r=[PS.k[pc], k_fcar], w=[k_fcar])
            PS.rel(pc)
            if own:
                f3, f3k = f3t, k_f3
                r3, r3k = r3t, k_r3
                op("dve", lambda e: e.tensor_copy(out=f3[:, 0:8], in_=g[:, 40:48]), r=[gk], w=[f3k])
                op("dve", lambda e: e.tensor_tensor(out=r3[:, 0:8], in0=g[:, 40:48], in1=f3[:, 0:8], op=ALU.subtract), r=[gk, f3k], w=[r3k])
                op("dve", lambda e: e.tensor_copy(out=f3[:, 8:16], in_=r3[:, 0:8]), r=[r3k], w=[f3k])
                op("dve", lambda e: e.tensor_tensor(out=r3[:, 8:16], in0=r3[:, 0:8], in1=f3[:, 8:16], op=ALU.subtract), r=[r3k, f3k], w=[r3k])
                op("dve", lambda e: e.tensor_copy(out=f3[:, 16:24], in_=r3[:, 8:16]), r=[r3k], w=[f3k])
                pt = PS.get()
                ptv = PS.t[pt][:].bitcast(BF16)
                op("pe", lambda e: e.transpose(out=ptv[0:24, 0:128], in_=f3[:, 0:24], identity=ident_b[:]), r=[f3k, kc], w=[PS.k[pt]])
                op("act", lambda e: e.copy(out=FqT[:, t * 128:(t + 1) * 128], in_=ptv[0:24, 0:128]), r=[PS.k[pt]], w=[k_fq])
                PS.rel(pt)
            gres.append((g, gk, R, Rk))

        chk(3)
        def conv_unit(col0, ch0):
            for j4 in range(4):
                if j4 % 2 == 0:
                    wt, wk = load_unit(col0 + (j4 // 2) * 256)
                j = j4 % 2
                ch = ch0 + j4
                pb = proj_fm(wt, wk, j)
                u, uk, _ = UB.next()
                op("act", lambda e: e.copy(out=u[:, 3:3 + BLK], in_=PS.t[pb][:, 0:BLK]), r=[PS.k[pb]], w=[uk])
                PS.rel(pb)
                op("pool", lambda e: e.tensor_copy(out=u[:, 0:3], in_=hist[:, ch, :]), r=[k_hist[ch]], w=[uk])
                op("pool", lambda e: e.tensor_copy(out=hist[:, ch, :], in_=u[:, BLK:BLK + 3]), r=[uk], w=[k_hist[ch]])
                a, ak, _ = CA.next()
                op("dve", lambda e: e.tensor_scalar(out=a[:], in0=u[:, 0:BLK], scalar1=convw_s[:, ch * 4:ch * 4 + 1], scalar2=None, op0=ALU.mult), r=[uk, k_convw], w=[ak])
                for tp in range(1, 4):
                    op("dve", lambda e: e.scalar_tensor_tensor(out=a[:], in0=u[:, tp:tp + BLK], scalar=convw_s[:, ch * 4 + tp:ch * 4 + tp + 1], in1=a[:], op0=ALU.mult, op1=ALU.add), r=[uk, k_convw, ak], w=[ak])
                silu_from(a[:], [ak], QKV[:, ch, :], [k_qkv[ch]])

        if own:
            conv_unit(C_QA, 0); conv_unit(C_QA + 512, 4)
        conv_unit(C_KA, 8); conv_unit(C_KA + 512, 12)
        conv_unit(C_VA, 16); conv_unit(C_VA + 512, 20)
        if own:
            for q4 in range(4):
                wt, wk = load_unit(C_ZA + q4 * 256)
                for j in range(2):
                    pb = proj_fm(wt, wk, j)
                    silu_from(PS.t[pb][:, 0:BLK], [PS.k[pb]], ZS[:, q4 * 2 + j, :], [k_zs[q4 * 2 + j]])
                    PS.rel(pb)
            for q4 in range(4):
                wt, wk = load_unit(C_QB + q4 * 256)
                for j in range(2):
                    pb = proj_fm(wt, wk, j)
                    hh = q4 * 2 + j
                    op("act", lambda e: e.activation(out=QbT[:, hh, :], in_=PS.t[pb][:, 0:BLK], func=AF.Copy, scale=128.0 ** -0.5), r=[PS.k[pb]], w=[k_qb[hh]])
                    PS.rel(pb)
        for q4 in range(4):
            wt, wk = load_unit(C_KB + q4 * 256)
            for j in range(2):
                pb = proj_fm(wt, wk, j)
                hh = q4 * 2 + j
                op("dve", lambda e: e.tensor_copy(out=KbT[:, hh, :], in_=PS.t[pb][:, 0:BLK]), r=[PS.k[pb]], w=[k_kb])
                PS.rel(pb)
        op("sp", lambda e: e.dma_start(out=kt_d[:, :, tok0:tok0 + BLK].rearrange("h p t -> p h t"), in_=KbT[:]), r=[k_kb], w=[k_ktd[blk]], dma=sem_kb)
        vts = [VbT.next() for _ in range(BLK // 128)]
        for q4 in range(4):
            wt, wk = load_unit(C_VB + q4 * 256)
            for t in range(BLK // 128):
                vt, vk, vd = vts[t]
                pb = PS.get()
                for k in range(KC):
                    op("pe", lambda e: e.matmul(PS.t[pb][:, 0:256], lhsT=hT[:, k, t * 128:(t + 1) * 128], rhs=wt[:, k, :], start=(k == 0), stop=(k == KC - 1)),
                       r=[wk, k_hT], w=[PS.k[pb]], inc=(k == KC - 1))
                if (q4 + t) % 2 == 0:
                    op("act", lambda e: e.copy(out=vt[:, q4 * 256:(q4 + 1) * 256], in_=PS.t[pb][:, 0:256]), r=[PS.k[pb]], w=[vk])
                else:
                    op("dve", lambda e: e.tensor_copy(out=vt[:, q4 * 256:(q4 + 1) * 256], in_=PS.t[pb][:, 0:256]), r=[PS.k[pb]], w=[vk])
                PS.rel(pb)
        for t in range(BLK // 128):
            vt, vk, vd = vts[t]
            op("sp", lambda e: e.dma_start(out=v_d[tok0 + t * 128:tok0 + (t + 1) * 128, :], in_=vt[:]), r=[vk], w=[k_vd[blk]], dma=vd)

        chk(4)
        def l2n(ch, qscale):
            sq, sqk, _ = W16.next()
            op("act", lambda e: e.activation(out=sq[:], in_=QKV[:, ch, :], func=AF.Square), r=[k_qkv[ch]], w=[sqk])
            pb = PS.get()
            op("pe", lambda e: e.matmul(PS.t[pb][:, 0:BLK], lhsT=ones_b[:], rhs=sq[:], start=True, stop=True), r=[kc, sqk], w=[PS.k[pb]])
            rn, rnk, _ = W32.next()
            op("act", lambda e: e.activation(out=rn[:], in_=PS.t[pb][:, 0:BLK], func=AF.Ln, bias=EPS), r=[PS.k[pb]], w=[rnk])
            PS.rel(pb)
            op("act", lambda e: e.activation(out=rn[:], in_=rn[:], func=AF.Exp, scale=-0.5), r=[rnk], w=[rnk])
            op("dve", lambda e: e.scalar_tensor_tensor(out=QKV[:, ch, :], in0=QKV[:, ch, :], scalar=qscale, in1=rn[:], op0=ALU.mult, op1=ALU.mult), r=[k_qkv[ch], rnk], w=[k_qkv[ch]])

        for h in range(H):
            if own:
                l2n(h, 128.0 ** -0.5)
            l2n(8 + h, 1.0)

        def fox_head(h):
            nkt = (blk + 1) * 2
            pO = PS.get(); pR = PS.get()
            pend = []

            def emit_pv(it):
                pT_, pTk, vr_, vrk_, kk_, kt_ = it
                first, last = (kt_ == 0), (kt_ == nkt - 1)
                op("pe", lambda e: e.matmul(PS.t[pO][:, 0:BLK], lhsT=vr_[:, kk_, :], rhs=pT_[:], start=first, stop=last), r=[vrk_, pTk], w=[PS.k[pO]], inc=last)
                op("pe", lambda e: e.matmul(PS.t[pR][:, 0:BLK], lhsT=ones_b[:], rhs=pT_[:], start=first, stop=last), r=[kc, pTk], w=[PS.k[pR]], inc=last)

            for g8 in range(0, nkt, 8):
                n8 = min(8, nkt - g8)
                kr, krk, krd = KR.next()
                vr, vrk, vrd = VR.next()
                bl = list(range(g8 * 128 // BLK, (g8 + n8) * 128 // BLK))
                op("sp", lambda e: e.dma_start(out=kr[:, 0:n8 * 128], in_=kt_d[h, :, g8 * 128:(g8 + n8) * 128]), r=[k_ktd[b] for b in bl], w=[krk], dma=krd)
                op("sp", lambda e: e.dma_start(out=vr[:, 0:n8, :], in_=v_d[g8 * 128:(g8 + n8) * 128, h * 128:(h + 1) * 128].rearrange("(k p) d -> p k d", p=128)), r=[k_vd[b] for b in bl], w=[vrk], dma=vrd)
                for kk in range(n8):
                    kt = g8 + kk
                    pS = PS.get()
                    op("pe", lambda e: e.matmul(PS.t[pS][:, 0:BLK], lhsT=kr[:, kk * 128:(kk + 1) * 128], rhs=QbT[:, h, :], start=True, stop=False), r=[krk, k_qb[h]], w=[PS.k[pS]], inc=False)
                    op("pe", lambda e: e.matmul(PS.t[pS][:, 0:BLK], lhsT=sel[:, h * 128:(h + 1) * 128], rhs=FqT[:], start=False, stop=True), r=[kc, k_fq], w=[PS.k[pS]])
                    pT_, pTk, _ = W16.next()
                    op("act", lambda e: e.activation(out=pT_[:], in_=PS.t[pS][:, 0:BLK], func=AF.Exp, bias=FK[:, kt, h:h + 1]), r=[PS.k[pS], k_fk[kt]], w=[pTk])
                    PS.rel(pS)
                    if kt >= blk * 2:
                        op("pool", lambda e: e.affine_select(out=pT_[:], in_=pT_[:], pattern=[[1, BLK]], compare_op=ALU.is_ge, fill=0.0, base=tok0 - kt * 128, channel_multiplier=-1), r=[pTk], w=[pTk])
                    pend.append((pT_, pTk, vr, vrk, kk, kt))
                    if len(pend) > 2:
                        emit_pv(pend.pop(0))
            while pend:
                emit_pv(pend.pop(0))
            ri, rik, _ = W32.next()
            op("dve", lambda e: e.reciprocal(out=ri[:], in_=PS.t[pR][:, 0:BLK]), r=[PS.k[pR]], w=[rik])
            op("dve", lambda e: e.tensor_tensor(out=mixT[:, 8 + h, :], in0=PS.t[pO][:, 0:BLK], in1=ri[:], op=ALU.mult), r=[PS.k[pO], rik, k_hT], w=[k_mix[8 + h]])
            PS.rel(pO); PS.rel(pR)


        fox_q = list(range(H)) if own else []

        def pump():
            if fox_q:
                fox_head(fox_q.pop(0))

        chk(41)
        for t in range(BLK // 128):
            g, gk, R, Rk = gres[t]
            ts_ = slice(t * 128, (t + 1) * 128)
            for hg in range(H // NSLOT):
                heads = [hg * NSLOT + i for i in range(NSLOT)]
                for hs, h in enumerate(heads):
                    sl = SL[hs]
                    knT = QKV[:, 8 + h, ts_]; kkn = k_qkv[8 + h]
                    gc_col = R[:, 32 + h:33 + h]; gcb_col = R[:, 40 + h:41 + h]
                    d1, d1k, _ = T32.next(); d2, d2k, _ = T32.next()
                    op("dve", lambda e: e.tensor_scalar(out=d1[:], in0=utri[:], scalar1=R[:, h:h + 1], scalar2=None, op0=ALU.mult), r=[kc, Rk], w=[d1k])
                    op("dve", lambda e: e.scalar_tensor_tensor(out=d2[:], in0=ident_f[:], scalar=R[:, 8 + h:9 + h], in1=d1[:], op0=ALU.mult, op1=ALU.subtract), r=[kc, Rk, d1k], w=[d2k])
                    pG = PS.get(); PG = PS.t[pG]
                    op("pe", lambda e: e.matmul(PG[:, 0:128], lhsT=ones_f[:], rhs=d1[:], start=True, stop=True), r=[kc, d1k], w=[PS.k[pG]])
                    op("pe", lambda e: e.matmul(PG[:, 128:256], lhsT=ones_f[:], rhs=d2[:], start=True, stop=True), r=[kc, d2k], w=[PS.k[pG]])
                    pK = PS.get(); PK = PS.t[pK]
                    op("pe", lambda e: e.matmul(PK[:, 0:128], lhsT=knT, rhs=knT, start=True, stop=True), r=[kkn], w=[PS.k[pK]])
                    if own:
                        op("pe", lambda e: e.matmul(PK[:, 128:256], lhsT=knT, rhs=QKV[:, h, ts_], start=True, stop=True), r=[kkn, k_qkv[h]], w=[PS.k[pK]])
                    e1, e1k, _ = T32.next()
                    op("dve", lambda e: e.scalar_tensor_tensor(out=e1[:], in0=PG[:, 128:256], scalar=gc_col, in1=m_posL[:], op0=ALU.add, op1=ALU.subtract), r=[PS.k[pG], Rk, kc], w=[e1k])
                    op("dve", lambda e: e.tensor_scalar(out=e1[:], in0=e1[:], scalar1=0.0, scalar2=None, op0=ALU.min), r=[e1k], w=[e1k])
                    op("act", lambda e: e.activation(out=e1[:], in_=e1[:], func=AF.Exp), r=[e1k], w=[e1k])
                    X, Xk = sl["X"]
                    op("dve", lambda e: e.scalar_tensor_tensor(out=X[:], in0=e1[:], scalar=-1.0, in1=PK[:, 0:128], op0=ALU.mult, op1=ALU.mult), r=[e1k, PS.k[pK]], w=[Xk])
                    e2, e2k, _ = T32.next()
                    op("dve", lambda e: e.scalar_tensor_tensor(out=e2[:], in0=PG[:, 0:128], scalar=gcb_col, in1=m_negUs[:], op0=ALU.subtract, op1=ALU.add), r=[PS.k[pG], Rk, kc], w=[e2k])
                    op("dve", lambda e: e.tensor_scalar(out=e2[:], in0=e2[:], scalar1=0.0, scalar2=None, op0=ALU.min), r=[e2k], w=[e2k])
                    op("act", lambda e: e.activation(out=e2[:], in_=e2[:], func=AF.Exp), r=[e2k], w=[e2k])
                    Y, Yk = sl["Y"]
                    op("dve", lambda e: e.scalar_tensor_tensor(out=Y[:], in0=e2[:], scalar=-1.0, in1=PK[:, 0:128], op0=ALU.mult, op1=ALU.mult), r=[e2k, PS.k[pK]], w=[Yk])
                    if own:
                        e3, e3k, _ = T32.next()
                        op("dve", lambda e: e.scalar_tensor_tensor(out=e3[:], in0=PG[:, 0:128], scalar=gc_col, in1=m_negUi[:], op0=ALU.subtract, op1=ALU.add), r=[PS.k[pG], Rk, kc], w=[e3k])
                        op("dve", lambda e: e.tensor_scalar(out=e3[:], in0=e3[:], scalar1=0.0, scalar2=None, op0=ALU.min), r=[e3k], w=[e3k])
                        op("act", lambda e: e.activation(out=e3[:], in_=e3[:], func=AF.Exp), r=[e3k], w=[e3k])
                        qkT, qkk = sl["qkT"]
                        op("dve", lambda e: e.tensor_tensor(out=qkT[:], in0=e3[:], in1=PK[:, 128:256], op=ALU.mult), r=[e3k, PS.k[pK]], w=[qkk])
                        e4, e4k, _ = T32.next()
                        op("act", lambda e: e.activation(out=e4[:], in_=PG[:, 0:128], func=AF.Exp), r=[PS.k[pG]], w=[e4k])
                        qdT, qdk = sl["qdT"]
                        op("dve", lambda e: e.tensor_tensor(out=qdT[:], in0=e4[:], in1=QKV[:, h, ts_], op=ALU.mult), r=[e4k, k_qkv[h]], w=[qdk])
                    PS.rel(pG); PS.rel(pK)
                chk(42)
                for hs, h in enumerate(heads):
                    sl = SL[hs]
                    X, Xk = sl["X"]; Y, Yk = sl["Y"]
                    m0, m0k = sl["Ul"]; m1, m1k = sl["W1s"]
                    Tm, Tmk = sl["TmA"]; Tt, Ttk = sl["TtA"]
                    op("pool", lambda e: e.tensor_tensor(out=m0[:], in0=X[:], in1=LM[:, 0, :], op=ALU.mult), r=[Xk, k_lm], w=[m0k])
                    op("dve", lambda e: e.tensor_tensor(out=Tm[:], in0=m0[:], in1=ident_f[:], op=ALU.add), r=[m0k, kc], w=[Tmk])
                    op("pool", lambda e: e.tensor_tensor(out=m1[:], in0=Y[:], in1=LM[:, 1, :], op=ALU.mult), r=[Yk, k_lm], w=[m1k])
                    op("dve", lambda e: e.tensor_tensor(out=Tt[:], in0=m1[:], in1=ident_f[:], op=ALU.add), r=[m1k, kc], w=[Ttk])
                cur = "A"
                for lvl in range(1, 7):
                    nxt = "B" if cur == "A" else "A"
                    for hs, h in enumerate(heads):
                        sl = SL[hs]
                        Y, Yk = sl["Y"]; Ul, Ulk = sl["Ul"]; W1s, W1k = sl["W1s"]; Tm, Tmk = sl["Tm" + cur]
                        op("pool", lambda e: e.tensor_tensor(out=Ul[:], in0=Y[:], in1=LM[:, 1 + lvl, :], op=ALU.mult), r=[Yk, k_lm], w=[Ulk])
                        pI = PS.get()
                        op("pe", lambda e: e.matmul(PS.t[pI][:, 0:128], lhsT=Ul[:], rhs=Tm[:], start=True, stop=True), r=[Ulk, Tmk], w=[PS.k[pI]])
                        op("act", lambda e: e.copy(out=W1s[:], in_=PS.t[pI][:, 0:128]), r=[PS.k[pI]], w=[W1k])
                        PS.rel(pI)
                    for hs, h in enumerate(heads):
                        sl = SL[hs]
                        W1s, W1k = sl["W1s"]; Tm, Tmk = sl["Tm" + cur]; Tt, Ttk = sl["Tt" + cur]; Tn, Tnk = sl["Tm" + nxt]
                        pJ = PS.get()
                        op("pe", lambda e: e.matmul(PS.t[pJ][:, 0:128], lhsT=Tt[:], rhs=W1s[:], start=True, stop=True), r=[Ttk, W1k], w=[PS.k[pJ]])
                        op("dve", lambda e: e.tensor_tensor(out=Tn[:], in0=PS.t[pJ][:, 0:128], in1=Tm[:], op=ALU.add), r=[PS.k[pJ], Tmk], w=[Tnk])
                        PS.rel(pJ)
                    for hs, h in enumerate(heads):
                        sl = SL[hs]
                        Tn, Tnk = sl["Tm" + nxt]; Ttn, Ttnk = sl["Tt" + nxt]
                        pL = PS.get()
                        PLb = PS.t[pL][:].bitcast(BF16)
                        op("pe", lambda e: e.transpose(out=PLb[:, 0:128], in_=Tn[:], identity=ident_b[:]), r=[Tnk, kc], w=[PS.k[pL]])
                        op("act", lambda e: e.copy(out=Ttn[:], in_=PLb[:, 0:128]), r=[PS.k[pL]], w=[Ttnk])
                        PS.rel(pL)
                    cur = nxt
                    pump()
                chk(43)
                for hs, h in enumerate(heads):
                    sl = SL[hs]
                    knT = QKV[:, 8 + h, ts_]; kkn = k_qkv[8 + h]
                    vT = QKV[:, 16 + h, ts_]; kvn = k_qkv[16 + h]
                    ke, kek = sl["ke"]; kd, kdk = sl["kd"]; vtk, vtkk = sl["vtk"]
                    pT = PS.get()
                    PTb = PS.t[pT][:].bitcast(BF16)
                    op("pe", lambda e: e.transpose(out=PTb[:, 0:128], in_=knT, identity=ident_b[:]), r=[kkn, kc], w=[PS.k[pT]])
                    op("dve", lambda e: e.tensor_scalar(out=ke[:], in0=PTb[:, 0:128], scalar1=R[:, 48 + h:49 + h], scalar2=None, op0=ALU.mult), r=[PS.k[pT], Rk], w=[kek])
                    op("dve", lambda e: e.tensor_scalar(out=kd[:], in0=PTb[:, 0:128], scalar1=R[:, 56 + h:57 + h], scalar2=None, op0=ALU.mult), r=[PS.k[pT], Rk], w=[kdk])
                    PS.rel(pT)
                    pT2 = PS.get()
                    PTb2 = PS.t[pT2][:].bitcast(BF16)
                    op("pe", lambda e: e.transpose(out=PTb2[:, 0:128], in_=vT, identity=ident_b[:]), r=[kvn, kc], w=[PS.k[pT2]])
                    op("act", lambda e: e.copy(out=vtk[:], in_=PTb2[:, 0:128]), r=[PS.k[pT2]], w=[vtkk])
                    PS.rel(pT2)
                chk(44)
                for hs, h in enumerate(heads):
                    sl = SL[hs]
                    Rm, Rmk = sl["Tt" + cur]
                    ke, kek = sl["ke"]; vtk, vtkk = sl["vtk"]; u2, u2k = sl["u2"]; wT, wTk = sl["wT"]
                    pU = PS.get()
                    op("pe", lambda e: e.matmul(PS.t[pU][:, 0:128], lhsT=Rm[:], rhs=vtk[:], start=True, stop=True), r=[Rmk, vtkk], w=[PS.k[pU]])
                    op("act", lambda e: e.activation(out=u2[:], in_=PS.t[pU][:, 0:128], func=AF.Identity, scale=R[:, 16 + h:17 + h]), r=[PS.k[pU], Rk], w=[u2k])
                    PS.rel(pU)
                    pU2 = PS.get()
                    op("pe", lambda e: e.matmul(PS.t[pU2][:, 0:128], lhsT=ke[:], rhs=Rm[:], start=True, stop=True), r=[Rmk, kek], w=[PS.k[pU2]])
                    op("dve", lambda e: e.tensor_copy(out=wT[:], in_=PS.t[pU2][:, 0:128]), r=[PS.k[pU2]], w=[wTk])
                    PS.rel(pU2)
                chk(45)
                for hs, h in enumerate(heads):
                    sl = SL[hs]
                    wT, wTk = sl["wT"]; vn, vnk = sl["vn"]; u2, u2k = sl["u2"]
                    pS1 = PS.get()
                    op("pe", lambda e: e.matmul(PS.t[pS1][:, 0:128], lhsT=wT[:], rhs=Sbf[:, h, :], start=True, stop=True), r=[wTk, k_Sb[h]], w=[PS.k[pS1]])
                    op("dve", lambda e: e.scalar_tensor_tensor(out=vn[:], in0=PS.t[pS1][:, 0:128], scalar=R[:, 24 + h:25 + h], in1=u2[:], op0=ALU.mult, op1=ALU.add), r=[PS.k[pS1], Rk, u2k], w=[vnk])
                    PS.rel(pS1)
                for hs, h in enumerate(heads):
                    sl = SL[hs]
                    vn, vnk = sl["vn"]; kd, kdk = sl["kd"]
                    if own:
                        qdT, qdk = sl["qdT"]; qkT, qkk = sl["qkT"]
                        pS2 = PS.get()
                        op("pe", lambda e: e.matmul(PS.t[pS2][:, 0:128], lhsT=Sbf[:, h, :], rhs=qdT[:], start=True, stop=False), r=[k_Sb[h], qdk], w=[PS.k[pS2]], inc=False)
                        op("pe", lambda e: e.matmul(PS.t[pS2][:, 0:128], lhsT=vn[:], rhs=qkT[:], start=False, stop=True), r=[vnk, qkk], w=[PS.k[pS2]])
                        op("act", lambda e: e.copy(out=OTB[:, h, ts_], in_=PS.t[pS2][:, 0:128]), r=[PS.k[pS2]], w=[k_ot[h]])
                        PS.rel(pS2)
                    pS3 = PS.get()
                    op("pe", lambda e: e.matmul(PS.t[pS3][:, 0:128], lhsT=kd[:], rhs=vn[:], start=True, stop=True), r=[kdk, vnk], w=[PS.k[pS3]])
                    op("dve", lambda e: e.scalar_tensor_tensor(out=Sst[:, h, :], in0=Sst[:, h, :], scalar=g[:, 32 + h:33 + h], in1=PS.t[pS3][:, 0:128], op0=ALU.mult, op1=ALU.add), r=[k_S[h], gk, PS.k[pS3]], w=[k_S[h]])
                    op("act", lambda e: e.copy(out=Sbf[:, h, :], in_=Sst[:, h, :]), r=[k_S[h]], w=[k_Sb[h]])
                    PS.rel(pS3)
                chk(46)
        if own:
            for h in range(H):
                sq, sqk, _ = W16.next()
                op("act", lambda e: e.activation(out=sq[:], in_=OTB[:, h, :], func=AF.Square), r=[k_ot[h]], w=[sqk])
                pb = PS.get()
                op("pe", lambda e: e.matmul(PS.t[pb][:, 0:BLK], lhsT=ones_b[:], rhs=sq[:], start=True, stop=True), r=[kc, sqk], w=[PS.k[pb]])
                rn, rnk, _ = W32.next()
                op("act", lambda e: e.activation(out=rn[:], in_=PS.t[pb][:, 0:BLK], func=AF.Ln, scale=1.0 / 128, bias=EPS), r=[PS.k[pb]], w=[rnk])
                PS.rel(pb)
                op("act", lambda e: e.activation(out=rn[:], in_=rn[:], func=AF.Exp, scale=-0.5), r=[rnk], w=[rnk])
                op("dve", lambda e: e.scalar_tensor_tensor(out=rn[:], in0=OTB[:, h, :], scalar=ong_s[:, 0:1], in1=rn[:], op0=ALU.mult, op1=ALU.mult), r=[k_ot[h], k_ong, rnk], w=[rnk])
                op("dve", lambda e: e.tensor_tensor(out=mixT[:, h, :], in0=rn[:], in1=ZS[:, h, :], op=ALU.mult), r=[rnk, k_zs[h]], w=[k_mix[h]])

        if dbg and blk == NB - 1:
            dbg_dump("qkv", QKV[:, :, :], k_qkv)

        chk(5)
        if own:
            chk(6)
        if own:
            while fox_q:
                pump()

            if dbg and blk == NBP:
                dbg_dump("mixT", mixT[:, :, :].rearrange("p c t -> p (c t)"), k_mix)

            chk(7)
            ob = blk - NBP
            for dblk in range(8):
                wt, wk = load_unit(dblk * 256, src=w_out)
                c0_, c1_ = dblk * 256, (dblk + 1) * 256
                for t in range(BLK // 128):
                    r0 = ob * BLK + t * 128
                    xpi, xpk, xpd = XP.next()
                    op("sp", lambda e: e.dma_start(out=xpi[:, 0:256], in_=xo[r0:r0 + 128, c0_:c1_]), w=[xpk], dma=xpd)
                    pb = PS.get()
                    for m in range(KC):
                        op("pe", lambda e: e.matmul(PS.t[pb][:, 0:256], lhsT=mixT[:, m, t * 128:(t + 1) * 128], rhs=wt[:, m, :], start=(m == 0), stop=(m == KC - 1)),
                           r=[k_mix[m], wk], w=[PS.k[pb]], inc=(m == KC - 1))
                    x1p, x1k, x1d = X1P.next()
                    op("dve", lambda e: e.tensor_tensor(out=x1p[:, 0:256], in0=PS.t[pb][:, 0:256], in1=G1B[:, c0_:c1_], op=ALU.mult), r=[PS.k[pb], k_G], w=[x1k])
                    PS.rel(pb)
                    op("pool", lambda e: e.tensor_tensor(out=x1p[:, 0:256], in0=x1p[:, 0:256], in1=xpi[:, 0:256], op=ALU.add), r=[x1k, xpk], w=[x1k])
                    op("sp", lambda e: e.dma_start(out=x1_d[r0:r0 + 128, c0_:c1_], in_=x1p[:, 0:256]), r=[x1k], w=[k_x1d[ob * 2 + t]], dma=x1d)
            chk(8)
            norm_to_T(x1_d[ob * BLK:(ob + 1) * BLK, :], h2T, k_h2T, A2, B2, x_from=[k_x1d[ob * 2], k_x1d[ob * 2 + 1]])
            op("sp", lambda e: e.dma_start(out=h2_d[:, :, ob * BLK:(ob + 1) * BLK].rearrange("c p t -> p c t"), in_=h2T[:]), r=[k_h2T], w=[k_h2d[ob]], dma=sem_h2)
            for t in range(BLK // 128):
                ti = ob * 2 + t
                pb = PS.get()
                for k in range(KC):
                    op("pe", lambda e: e.matmul(PS.t[pb][:, 0:36], lhsT=h2T[:, k, t * 128:(t + 1) * 128], rhs=wr_s[:, k, :], start=(k == 0), stop=(k == KC - 1)),
                       r=[k_h2T, k_wr], w=[PS.k[pb]], inc=(k == KC - 1))
                rt, rtk, _ = RT.next()
                op("dve", lambda e: e.tensor_tensor(out=rt[:, 0:36], in0=PS.t[pb][:, 0:36], in1=br_s[:], op=ALU.add), r=[PS.k[pb], k_br], w=[rtk])
                PS.rel(pb)
                op("dve", lambda e: e.tensor_reduce(out=rt[:, 40:41], in_=rt[:, 0:4], axis=AX.X, op=ALU.max), r=[rtk], w=[rtk])
                op("dve", lambda e: e.tensor_scalar(out=rt[:, 36:40], in0=rt[:, 0:4], scalar1=rt[:, 40:41], scalar2=None, op0=ALU.is_ge), r=[rtk], w=[rtk])
                op("dve", lambda e: e.tensor_scalar(out=rt[:, 41:42], in0=rt[:, 40:41], scalar1=-1.0, scalar2=None, op0=ALU.mult), r=[rtk], w=[rtk])
                op("act", lambda e: e.activation(out=rt[:, 44:48], in_=rt[:, 0:4], func=AF.Exp, bias=rt[:, 41:42], accum_out=rt[:, 42:43]), r=[rtk], w=[rtk])
                op("dve", lambda e: e.reciprocal(out=rt[:, 43:44], in_=rt[:, 42:43]), r=[rtk], w=[rtk])
                op("dve", lambda e: e.tensor_scalar(out=rt[:, 48:56], in0=rt[:, 4:12], scalar1=rt[:, 36:37], scalar2=None, op0=ALU.mult), r=[rtk], w=[rtk])
                for gi in range(1, 4):
                    op("dve", lambda e: e.scalar_tensor_tensor(out=rt[:, 48:56], in0=rt[:, 4 + gi * 8:12 + gi * 8], scalar=rt[:, 36 + gi:37 + gi], in1=rt[:, 48:56], op0=ALU.mult, op1=ALU.add), r=[rtk], w=[rtk])
                op("dve", lambda e: e.max(out=rt[:, 56:64], in_=rt[:, 48:56]), r=[rtk], w=[rtk])
                rt2, rt2k, _ = RT.next()
                op("dve", lambda e: e.tensor_scalar(out=rt2[:, 0:8], in0=rt[:, 48:56], scalar1=rt[:, 57:58], scalar2=None, op0=ALU.is_ge), r=[rtk], w=[rt2k])
                op("dve", lambda e: e.tensor_scalar(out=rt2[:, 8:9], in0=rt[:, 56:57], scalar1=-1.0, scalar2=None, op0=ALU.mult), r=[rtk], w=[rt2k])
                op("act", lambda e: e.activation(out=rt2[:, 16:24], in_=rt[:, 48:56], func=AF.Exp, bias=rt2[:, 8:9]), r=[rtk, rt2k], w=[rt2k])
                op("dve", lambda e: e.tensor_tensor(out=rt2[:, 16:24], in0=rt2[:, 16:24], in1=rt2[:, 0:8], op=ALU.mult), r=[rt2k], w=[rt2k])
                op("dve", lambda e: e.tensor_reduce(out=rt2[:, 9:10], in_=rt2[:, 16:24], axis=AX.X, op=ALU.add), r=[rt2k], w=[rt2k])
                op("dve", lambda e: e.reciprocal(out=rt2[:, 10:11], in_=rt2[:, 9:10]), r=[rt2k], w=[rt2k])
                op("dve", lambda e: e.tensor_tensor(out=rt2[:, 10:11], in0=rt2[:, 10:11], in1=rt[:, 43:44], op=ALU.mult), r=[rt2k, rtk], w=[rt2k])
                op("dve", lambda e: e.tensor_scalar(out=rt2[:, 16:24], in0=rt2[:, 16:24], scalar1=rt2[:, 10:11], scalar2=None, op0=ALU.mult), r=[rt2k], w=[rt2k])
                for gi in range(4):
                    op("dve", lambda e: e.tensor_scalar(out=CW[:, ti, gi * 8:(gi + 1) * 8], in0=rt2[:, 16:24], scalar1=rt[:, 36 + gi:37 + gi], scalar2=None, op0=ALU.mult), r=[rt2k, rtk], w=[k_cw[ti]])

    if dbg:
        dbg_dump("cw", CW[:, :, :], k_cw)
        dbg_dump("x1", x1_d, k_x1d)

    chk(9)
    S.barrier()
    stack_B.close()
    _STK[0] = stack_main
    fgb_s, k_fgb = load_small("fgb_s", fgb, [128, D])
    passes = []
    PT_MAX = min(4, NTO)
    passes = [(t0, min(t0 + PT_MAX, NTO)) for t0 in range(0, NTO, PT_MAX)]
    h2p = sb("h2p", [128, KC, PT_MAX * 128], BF16); k_h2p = Trk(); sem_h2p = S.newsem("d_h2p")
    acc = sb("acc", [128, PT_MAX, D]); k_acc = [Trk() for _ in range(PT_MAX)]
    HID = Ring(S, nc, "hid", [128, 4, PT_MAX * 128], BF16, 2)
    SG = Ring(S, nc, "sg", [128, 512], F32, 3)
    W2R = Ring(S, nc, "w2r", [128, 4, D], BF16, 2, dma=True)
    WRC = Ring(S, nc, "wrc", [128, KC, 512], BF16, 3, dma=True)
    X1L = Ring(S, nc, "x1l", [128, D], F32, 1, dma=True)
    OUTR = Ring(S, nc, "outr", [128, D], F32, 1, dma=True)
    for (t0, t1) in passes:
        ntl = t1 - t0
        ntok = ntl * 128
        op("sp", lambda e: e.dma_start(out=h2p[:, :, 0:ntok], in_=h2_d[:, :, t0 * 128:t1 * 128].rearrange("c p t -> p c t")), r=k_h2d, w=[k_h2p], dma=sem_h2p)
        for tl in range(ntl):
            op("pool", lambda e: e.memset(acc[:, tl, :], 0.0), w=[k_acc[tl]])
        cbs = [(c0, min(c0 + 512, ntok)) for c0 in range(0, ntok, 512)]
        for ex in range(NE):
            w1t, w1k, w1d = WRC.next()
            op("pool", lambda e: e.dma_start(out=w1t[:], in_=w1[ex].rearrange("(c p) f -> p c f", p=128)), w=[w1k], dma=w1d)
            w3t, w3k, w3d = WRC.next()
            op("pool", lambda e: e.dma_start(out=w3t[:], in_=w3[ex].rearrange("(c p) f -> p c f", p=128)), w=[w3k], dma=w3d)
            w2t, w2k, w2d = W2R.next()
            op("pool", lambda e: e.dma_start(out=w2t[:], in_=w2[ex].rearrange("(c p) f -> p c f", p=128)), w=[w2k], dma=w2d)
            hid, hidk, _ = HID.next()
            for fc in range(4):
                for (c0, c1) in cbs:
                    n = c1 - c0
                    pa = PS.get(); pb = PS.get()
                    for k in range(KC):
                        op("pe", lambda e: e.matmul(PS.t[pa][:, 0:n], lhsT=w1t[:, k, fc * 128:(fc + 1) * 128], rhs=h2p[:, k, c0:c1], start=(k == 0), stop=(k == KC - 1)),
                           r=[w1k, k_h2p], w=[PS.k[pa]], inc=(k == KC - 1))
                    for k in range(KC):
                        op("pe", lambda e: e.matmul(PS.t[pb][:, 0:n], lhsT=w3t[:, k, fc * 128:(fc + 1) * 128], rhs=h2p[:, k, c0:c1], start=(k == 0), stop=(k == KC - 1)),
                           r=[w3k, k_h2p], w=[PS.k[pb]], inc=(k == KC - 1))
                    sg, sgk, _ = SG.next()
                    op("act", lambda e: e.activation(out=sg[:, 0:n], in_=PS.t[pa][:, 0:n], func=AF.Silu), r=[PS.k[pa]], w=[sgk])
                    PS.rel(pa)
                    op("dve", lambda e: e.tensor_tensor(out=hid[:, fc, c0:c1], in0=sg[:, 0:n], in1=PS.t[pb][:, 0:n], op=ALU.mult), r=[sgk, PS.k[pb]], w=[hidk])
                    PS.rel(pb)
            for tl in range(ntl):
                ti = t0 + tl
                for dblk in range(4):
                    pb = PS.get()
                    for fc in range(4):
                        op("pe", lambda e: e.matmul(PS.t[pb][:], lhsT=hid[:, fc, tl * 128:(tl + 1) * 128], rhs=w2t[:, fc, dblk * 512:(dblk + 1) * 512], start=(fc == 0), stop=(fc == 3)),
                           r=[hidk, w2k], w=[PS.k[pb]], inc=(fc == 3))
                    op("dve", lambda e: e.scalar_tensor_tensor(out=acc[:, tl, dblk * 512:(dblk + 1) * 512], in0=PS.t[pb][:], scalar=CW[:, ti, ex:ex + 1], in1=acc[:, tl, dblk * 512:(dblk + 1) * 512], op0=ALU.mult, op1=ALU.add),
                       r=[PS.k[pb], k_cw[ti], k_acc[tl]], w=[k_acc[tl]])
                    PS.rel(pb)
        for tl in range(ntl):
            ti = t0 + tl
            x1t, x1k, x1dd = X1L.next()
            op("sp", lambda e: e.dma_start(out=x1t[:], in_=x1_d[ti * 128:(ti + 1) * 128, :]), r=[k_x1d[ti]], w=[x1k], dma=x1dd)
            op("pool", lambda e: e.tensor_tensor(out=acc[:, tl, :], in0=acc[:, tl, :], in1=G2B[:], op=ALU.mult), r=[k_acc[tl], k_G], w=[k_acc[tl]])
            op("dve", lambda e: e.tensor_tensor(out=x1t[:], in0=x1t[:], in1=acc[:, tl, :], op=ALU.add), r=[x1k, k_acc[tl]], w=[x1k])
            st, sk, _ = stat.next()
            ot_, otk_, otd = OUTR.next()
            op("act", lambda e: e.activation(out=ot_[:], in_=x1t[:], func=AF.Square, accum_out=st[:, 0:1]), r=[x1k], w=[otk_, sk])
            rstd_from_ssq(st, sk, 0, 1, D)
            op("dve", lambda e: e.scalar_tensor_tensor(out=ot_[:], in0=x1t[:], scalar=st[:, 1:2], in1=fgb_s[:], op0=ALU.mult, op1=ALU.mult), r=[x1k, sk, k_fgb], w=[otk_])
            op("sp", lambda e: e.dma_start(out=out[ti * 128:(ti + 1) * 128, :], in_=ot_[:]), r=[otk_], dma=otd)
    finalize()
    return nc


def _fm(v):
    v = np.asarray(v, np.float32)
    return np.ascontiguousarray(v.reshape(-1, 128).T)


def _rep(v):
    v = np.asarray(v, np.float32).reshape(1, -1)
    return np.ascontiguousarray(np.broadcast_to(v, (128, v.shape[1])))


def _level_masks():
    i = np.arange(128)[:, None]
    j = np.arange(128)[None, :]
    ms = []
    for l in range(7):
        s_ = 1 << l
        lmx = ((i // (2 * s_) == j // (2 * s_)) & (i % (2 * s_) >= s_) & (j % (2 * s_) < s_)).astype(np.float32)
        if l == 0:
            ms.append(lmx)
        ms.append(np.ascontiguousarray(lmx.T))
    return np.ascontiguousarray(np.concatenate(ms, axis=1))


def make_in_maps(inputs, NP, NO):
    x = np.asarray(inputs["x"], np.float32)
    B = x.shape[0]
    f = lambda k: np.asarray(inputs[k], np.float32)
    shared = {
        "w_ada": np.ascontiguousarray(f("w_ada")[0]),
        "b_adaT": _fm(f("b_ada")[0]),
        "n1g": _fm(f("norm1_g")[0]),
        "w_in": np.ascontiguousarray(f("w_in")[0]),
        "convw": np.ascontiguousarray(f("conv_w")[0].T.reshape(24, 128, 4).transpose(1, 0, 2).reshape(128, 96)),
        "alog": _rep(f("a_log")[0]), "dtb": _rep(f("dt_bias")[0]), "ong": _fm(f("dn_onorm_g")[0]),
        "ffb": _rep(f("fox_f_bias")[0]),
        "w_out": np.ascontiguousarray(f("w_out")[0]),
        "n2g": _fm(f("norm2_g")[0]),
        "w_r": np.ascontiguousarray(np.concatenate([f("w_router_group")[0], f("w_router_expert")[0]], axis=1)),
        "b_r": _rep(np.concatenate([f("b_router_group")[0], f("b_router_expert")[0]])),
        "w1": np.ascontiguousarray(f("w1")[0].reshape(NE, D, 512)),
        "w3": np.ascontiguousarray(f("w3")[0].reshape(NE, D, 512)),
        "w2": np.ascontiguousarray(f("w2")[0].reshape(NE, 512, D)),
        "fgb": _rep(f("final_g")),
        "lvlm": _level_masks(),
    }
    maps = []
    for b in range(B):
        for s in range(2):
            m = dict(shared)
            m["xp"] = np.ascontiguousarray(x[b, 0:NP])
            m["xo"] = np.ascontiguousarray(x[b, s * NP:s * NP + NO])
            m["cT"] = _fm(f("c")[b])
            pm = np.zeros((128, 2), np.float32)
            pm[:, 0] = float(s)
            pm[:, 1] = 0.0 if s == 1 else -30000.0
            m["pmv"] = pm
            maps.append(m)
    return maps


_NC_CACHE = {}


def kernel(**inputs):
    NP = NO = 2048
    if "nc" not in _NC_CACHE:
        _NC_CACHE["nc"] = build(NP, NO)
    nc = _NC_CACHE["nc"]
    maps = make_in_maps(inputs, NP, NO)
    res = run_bass_kernel_spmd(nc, maps, core_ids=list(range(8)))
    B = 4
    outp = np.zeros((B, 2 * NO, D), np.float32)
    for b in range(B):
        for s in range(2):
            outp[b, s * NO:(s + 1) * NO] = res.results[b * 2 + s]["out"]
    return outp
```

```python
from contextlib import ExitStack
import numpy as np
import concourse.bass as bass
import concourse.mybir as mybir
from concourse.bass_utils import run_bass_kernel_spmd

F32 = mybir.dt.float32
BF16 = mybir.dt.bfloat16
I32 = mybir.dt.int32
AF = mybir.ActivationFunctionType
ALU = mybir.AluOpType
AX = mybir.AxisListType

D = 2048
KC = 16
H = 8
BLK = 256
NE = 32
EPS = 1e-6
IN_W = 7192
C_QA, C_KA, C_VA, C_ZA, C_AA, C_BA, C_QB, C_KB, C_VB, C_FB = 0, 1024, 2048, 3072, 4096, 4104, 4112, 5136, 6160, 7184


class Trk:
    __slots__ = ("w", "r", "ps")

    def __init__(self, ps=False):
        self.w = None
        self.r = {}
        self.ps = ps


class Sched:
    def __init__(self, nc):
        self.nc = nc
        self.eng = {"pe": nc.tensor, "act": nc.scalar, "dve": nc.vector, "pool": nc.gpsimd, "sp": nc.sync}
        self.sems, self.cnt, self.isdma = {}, {}, {}
        self.seen = {e: {} for e in self.eng}
        for e in self.eng:
            self.newsem(e, False)
        self.nops = 0

    def newsem(self, key, isdma=True):
        self.sems[key] = self.nc.alloc_semaphore(name=f"s_{key}")
        self.cnt[key] = 0
        self.isdma[key] = isdma
        return key

    def _wait(self, e, key, val):
        if self.isdma[key]:
            val = self.cnt[key]
        if self.seen[e].get(key, 0) >= val:
            return
        self.eng[e].wait_ge(self.sems[key], val)
        self.seen[e][key] = val

    def op(self, e, fn, reads=(), writes=(), inc=True, dma=None):
        deps = {}

        def add(k, v):
            if deps.get(k, 0) < v:
                deps[k] = v
        for t in reads:
            if t.w is not None:
                k, v = t.w
                if not (k == e and e == "pe"):
                    add(k, v)
            if t.ps:
                for k, v in t.r.items():
                    if k != e:
                        add(k, v)
        for t in writes:
            if t.w is not None and not (t.w[0] == e and dma is None):
                add(*t.w)
            for k, v in t.r.items():
                if k == e and dma is None:
                    continue
                add(k, v)
        for k, v in deps.items():
            self._wait(e, k, v)
        ins = fn(self.eng[e])
        self.nops += 1
        if dma is not None:
            self.cnt[dma] += 16
            ins.then_inc(self.sems[dma], 16)
            tk = (dma, self.cnt[dma])
        elif inc:
            self.cnt[e] += 1
            ins.then_inc(self.sems[e], 1)
            tk = (e, self.cnt[e])
        else:
            tk = (e, self.cnt[e] + 1)
        for t in reads:
            if t.r.get(tk[0], 0) < tk[1]:
                t.r[tk[0]] = tk[1]
        for t in writes:
            t.w = tk
            t.r = {}
        return tk

    def barrier(self):
        for e in self.eng:
            for k in self.sems:
                if k != e and self.cnt[k] > 0:
                    v = self.cnt[k]
                    if self.seen[e].get(k, 0) < v:
                        self.eng[e].wait_ge(self.sems[k], v)
                        self.seen[e][k] = v


_STK = [None]


def _alloc_sb(nc, name, shape, dtype):
    return _STK[0].enter_context(nc.sbuf_tensor(name, list(shape), dtype))


class Ring:
    def __init__(self, S, nc, name, shape, dtype, n, dma=False):
        self.t = [_alloc_sb(nc, f"{name}{i}", shape, dtype) for i in range(n)]
        self.k = [Trk() for _ in range(n)]
        self.d = [S.newsem(f"d_{name}{i}") for i in range(n)] if dma else [None] * n
        self.i = 0
        self.n = n

    def next(self):
        j = self.i % self.n
        self.i += 1
        return self.t[j], self.k[j], self.d[j]


class PsPool:
    def __init__(self, nc, n):
        self.t = [nc.alloc_psum_tensor(f"ps{i}", [128, 512], F32) for i in range(n)]
        self.k = [Trk(ps=True) for _ in range(n)]
        self.busy = [False] * n
        self.i = 0
        self.n = n

    def get(self):
        for _ in range(self.n):
            j = self.i % self.n
            self.i += 1
            if not self.busy[j]:
                self.busy[j] = True
                return j
        raise RuntimeError("out of PSUM banks")

    def rel(self, j):
        self.busy[j] = False


class StopBuild(Exception):
    pass


_LAST = {}


def build(NP, NO, dbg=None, stop=None):
    def chk(n):
        if stop == n:
            finalize()
            _LAST["nc"] = nc
            raise StopBuild()
    nc = bass.Bass("TRN2", target_bir_lowering=False)
    S = Sched(nc)

    def finalize():
        for k in S.sems:
            if S.isdma[k] and S.cnt[k] > 0:
                S.eng["sp"].wait_ge(S.sems[k], S.cnt[k])
        S.barrier()
    T = NP + NO
    NBP, NBO = NP // BLK, NO // BLK
    NB = NBP + NBO
    NKT = T // 128
    NTO = NO // 128

    def din(name, shape, dt=F32):
        return nc.dram_tensor(name, list(shape), dt, kind="ExternalInput").ap()

    xp = din("xp", [NP, D]); xo = din("xo", [NO, D]); cT = din("cT", [128, KC])
    pmv = din("pmv", [128, 2])
    w_ada = din("w_ada", [D, 6 * D]); b_adaT = din("b_adaT", [128, 96]); n1g = din("n1g", [128, KC])
    w_in = din("w_in", [D, IN_W]); convw = din("convw", [128, 24 * 4])
    alog = din("alog", [128, H]); dtb = din("dtb", [128, H]); ong = din("ong", [128, 1]); ffb = din("ffb", [128, H])
    w_out = din("w_out", [D, D]); n2g = din("n2g", [128, KC])
    w_r = din("w_r", [D, 36]); b_r = din("b_r", [128, 36])
    w1 = din("w1", [NE, D, 512]); w3 = din("w3", [NE, D, 512]); w2 = din("w2", [NE, 512, D])
    fgb = din("fgb", [128, D])
    lvlm = din("lvlm", [128, 8 * 128])
    out = nc.dram_tensor("out", [NO, D], F32, kind="ExternalOutput").ap()
    dbg_t = None
    if dbg:
        dbg_t = {k: nc.dram_tensor("dbg_" + k, list(shp), F32, kind="ExternalOutput").ap() for k, shp in dbg.items()}

    win_d = nc.dram_tensor("win_d", [D, IN_W], BF16, kind="Internal").ap()
    kt_d = nc.dram_tensor("kt_d", [H, 128, T], BF16, kind="Internal").ap()
    v_d = nc.dram_tensor("v_d", [T, H * 128], BF16, kind="Internal").ap()
    x1_d = nc.dram_tensor("x1_d", [NO, D], F32, kind="Internal").ap()
    h2_d = nc.dram_tensor("h2_d", [KC, 128, NO], BF16, kind="Internal").ap()
    k_win = Trk(); k_ktd = [Trk() for _ in range(NB)]; k_vd = [Trk() for _ in range(NB)]
    k_x1d = [Trk() for _ in range(NTO)]; k_h2d = [Trk() for _ in range(NBO)]

    stack_main = ExitStack()
    stack_B = ExitStack()
    _STK[0] = stack_main

    def sb(name, shape, dt=F32):
        return _alloc_sb(nc, name, shape, dt)

    def op(e, fn, r=(), w=(), inc=True, dma=None):
        return S.op(e, fn, r, w, inc, dma)

    PS = PsPool(nc, 8)

    ident_f = sb("ident_f", [128, 128]); ident_b = sb("ident_b", [128, 128], BF16)
    ones_f = sb("ones_f", [128, 128]); ones_b = sb("ones_b", [128, 128], BF16)
    utri = sb("utri", [128, 128])
    m_posL = sb("m_posL", [128, 128])
    m_negUs = sb("m_negUs", [128, 128])
    m_negUi = sb("m_negUi", [128, 128])
    sel = sb("sel", [24, H * 128], BF16)
    iot = sb("iot", [24, 128], I32); iotf = sb("iotf", [24, 128])
    kc = Trk()
    op("pool", lambda e: e.memset(ident_f[:], 0.0), w=[kc])
    op("pool", lambda e: e.affine_select(out=ident_f[:], in_=ident_f[:], pattern=[[-1, 128]], compare_op=ALU.not_equal, fill=1.0, base=0, channel_multiplier=1), r=[kc], w=[kc])
    op("pool", lambda e: e.tensor_copy(out=ident_b[:], in_=ident_f[:]), r=[kc], w=[kc])
    op("pool", lambda e: e.memset(ones_f[:], 1.0), w=[kc])
    op("pool", lambda e: e.memset(ones_b[:], 1.0), w=[kc])
    op("pool", lambda e: e.affine_select(out=utri[:], in_=ones_f[:], pattern=[[1, 128]], compare_op=ALU.is_ge, fill=0.0, base=0, channel_multiplier=-1), r=[kc], w=[kc])
    op("pool", lambda e: e.memset(m_posL[:], 0.0), w=[kc])
    op("pool", lambda e: e.affine_select(out=m_posL[:], in_=m_posL[:], pattern=[[-1, 128]], compare_op=ALU.is_ge, fill=1.0e4, base=-1, channel_multiplier=1), r=[kc], w=[kc])
    op("pool", lambda e: e.memset(m_negUs[:], 0.0), w=[kc])
    op("pool", lambda e: e.affine_select(out=m_negUs[:], in_=m_negUs[:], pattern=[[1, 128]], compare_op=ALU.is_ge, fill=-1.0e4, base=-1, channel_multiplier=-1), r=[kc], w=[kc])
    op("pool", lambda e: e.memset(m_negUi[:], 0.0), w=[kc])
    op("pool", lambda e: e.affine_select(out=m_negUi[:], in_=m_negUi[:], pattern=[[1, 128]], compare_op=ALU.is_ge, fill=-1.0e4, base=0, channel_multiplier=-1), r=[kc], w=[kc])
    op("pool", lambda e: e.iota(iot[:], pattern=[[0, 128]], base=0, channel_multiplier=1), w=[kc])
    op("pool", lambda e: e.tensor_copy(out=iotf[:], in_=iot[:]), r=[kc], w=[kc])
    for h in range(H):
        op("dve", lambda e: e.tensor_scalar(out=iot[:].bitcast(F32), in0=iotf[:], scalar1=float(-h), scalar2=None, op0=ALU.add), r=[kc], w=[kc])
        tmpf = iot[:].bitcast(F32)
        op("dve", lambda e: e.scalar_tensor_tensor(out=iotf[:], in0=tmpf, scalar=-8.0, in1=tmpf, op0=ALU.add, op1=ALU.mult), r=[kc], w=[kc])
        op("dve", lambda e: e.scalar_tensor_tensor(out=iotf[:], in0=tmpf, scalar=-16.0, in1=iotf[:], op0=ALU.add, op1=ALU.mult), r=[kc], w=[kc])
        op("dve", lambda e: e.tensor_scalar(out=sel[:, h * 128:(h + 1) * 128], in0=iotf[:], scalar1=0.0, scalar2=None, op0=ALU.is_equal), r=[kc], w=[kc])
        op("dve", lambda e: e.tensor_scalar(out=iotf[:], in0=tmpf, scalar1=float(h), scalar2=None, op0=ALU.add), r=[kc], w=[kc])

    def load_small(name, src, shape, dt=F32):
        t = sb(name, shape, dt)
        k = Trk()
        sem = S.newsem("d_" + name)
        op("sp", lambda e: e.dma_start(out=t[:], in_=src), w=[k], dma=sem)
        return t, k

    cT_s, k_cT = load_small("cT_s", cT, [128, KC])
    pm_s, k_pm = load_small("pm_s", pmv, [128, 2])
    bada_s, k_bada = load_small("bada_s", b_adaT, [128, 96])
    n1g_s, k_n1g = load_small("n1g_s", n1g, [128, KC])
    n2g_s, k_n2g = load_small("n2g_s", n2g, [128, KC])
    convw_s, k_convw = load_small("convw_s", convw, [128, 96])
    alog_s, k_alog = load_small("alog_s", alog, [128, H])
    dtb_s, k_dtb = load_small("dtb_s", dtb, [128, H])
    ong_s, k_ong = load_small("ong_s", ong, [128, 1])
    ffb_s, k_ffb = load_small("ffb_s", ffb, [128, H])
    br_s, k_br = load_small("br_s", b_r, [128, 36])

    LM = sb("LM", [128, 8, 128], BF16); k_lm = Trk(); sem_lm = S.newsem("d_lm")
    op("pool", lambda e: e.dma_start(out=LM[:].rearrange("p a b -> p (a b)"), in_=lvlm), w=[k_lm], dma=sem_lm)
    sem_win = S.newsem("d_win")
    for i in range(4):
        op("pool", lambda e: e.dma_start(out=win_d[i * 512:(i + 1) * 512, :], in_=w_in[i * 512:(i + 1) * 512, :]), w=[k_win], dma=sem_win)
    wr_s = sb("wr_s", [128, KC, 36], BF16); k_wr = Trk(); sem_wr = S.newsem("d_wr")
    op("pool", lambda e: e.dma_start(out=wr_s[:], in_=w_r.rearrange("(c p) f -> p c f", p=128)), w=[k_wr], dma=sem_wr)
    wsm = sb("wsm", [128, KC, 24], BF16); k_wsm = Trk(); sem_wsm = S.newsem("d_wsm")
    op("pool", lambda e: e.dma_start(out=wsm[:, :, 0:16], in_=w_in[:, C_AA:C_AA + 16].rearrange("(c p) f -> p c f", p=128)), w=[k_wsm], dma=sem_wsm)
    op("pool", lambda e: e.dma_start(out=wsm[:, :, 16:24], in_=w_in[:, C_FB:C_FB + 8].rearrange("(c p) f -> p c f", p=128)), w=[k_wsm], dma=sem_wsm)

    chk(0)
    sc_b = sb("sc_b", [128, KC], BF16); k_sc = Trk()
    tA = sb("tA", [128, KC]); k_tA = Trk()
    mod = sb("mod", [128, 96]); k_mod = Trk()
    vec = sb("vec", [128, 6 * KC]); k_vec = Trk()
    G1B = sb("G1B", [128, D]); G2B = sb("G2B", [128, D]); k_G = Trk()
    dg = Ring(S, nc, "dg", [128, 128], F32, 2)
    nalog = sb("nalog", [128, H]); k_nalog = Trk()
    stat = Ring(S, nc, "stat", [128, 4], F32, 4)
    CW = sb("CW", [128, NTO, 32]); k_cw = [Trk() for _ in range(NTO)]
    _STK[0] = stack_B
    UW = 256
    WR = Ring(S, nc, "wring", [128, KC, UW], BF16, 3, dma=True)
    op("act", lambda e: e.activation(out=tA[:], in_=cT_s[:], func=AF.Exp, scale=-1.0), r=[k_cT], w=[k_tA])
    op("dve", lambda e: e.tensor_scalar(out=tA[:], in0=tA[:], scalar1=1.0, scalar2=None, op0=ALU.add), r=[k_tA], w=[k_tA])
    op("dve", lambda e: e.reciprocal(out=tA[:], in_=tA[:]), r=[k_tA], w=[k_tA])
    op("dve", lambda e: e.tensor_tensor(out=sc_b[:], in0=tA[:], in1=cT_s[:], op=ALU.mult), r=[k_tA, k_cT], w=[k_sc])
    pmod = PS.get()
    for u in range(48):
        wt, wk, wd = WR.next()
        op("pool", lambda e: e.dma_start(out=wt[:], in_=w_ada[:, u * 256:(u + 1) * 256].rearrange("(c p) f -> p c f", p=128)), w=[wk], dma=wd)
        for j in range(2):
            oc = u * 2 + j
            for k in range(KC):
                op("pe", lambda e: e.matmul(PS.t[pmod][:, oc:oc + 1], lhsT=wt[:, k, j * 128:(j + 1) * 128], rhs=sc_b[:, k:k + 1], start=(k == 0), stop=(k == KC - 1)),
                   r=[wk, k_sc], w=[PS.k[pmod]], inc=(k == KC - 1))
    op("dve", lambda e: e.tensor_tensor(out=mod[:], in0=PS.t[pmod][:, 0:96], in1=bada_s[:], op=ALU.add), r=[PS.k[pmod], k_bada], w=[k_mod])
    PS.rel(pmod)
    A1, B1, A1p, B1p, A2, B2 = [vec[:, i * KC:(i + 1) * KC] for i in range(6)]
    op("dve", lambda e: e.scalar_tensor_tensor(out=A1, in0=mod[:, 16:32], scalar=1.0, in1=n1g_s[:], op0=ALU.add, op1=ALU.mult), r=[k_mod, k_n1g], w=[k_vec])
    op("dve", lambda e: e.tensor_copy(out=B1, in_=mod[:, 0:16]), r=[k_mod], w=[k_vec])
    op("dve", lambda e: e.tensor_scalar(out=A1p, in0=A1, scalar1=pm_s[:, 0:1], scalar2=None, op0=ALU.mult), r=[k_vec, k_pm], w=[k_vec])
    op("dve", lambda e: e.tensor_scalar(out=B1p, in0=B1, scalar1=pm_s[:, 0:1], scalar2=None, op0=ALU.mult), r=[k_vec, k_pm], w=[k_vec])
    op("dve", lambda e: e.scalar_tensor_tensor(out=A2, in0=mod[:, 64:80], scalar=1.0, in1=n2g_s[:], op0=ALU.add, op1=ALU.mult), r=[k_mod, k_n2g], w=[k_vec])
    op("dve", lambda e: e.tensor_copy(out=B2, in_=mod[:, 48:64]), r=[k_mod], w=[k_vec])
    for gi, (GB, off) in enumerate(((G1B, 32), (G2B, 80))):
        for q4 in range(4):
            pb = PS.get()
            for j in range(4):
                c = q4 * 4 + j
                dt_, dk_, _ = dg.next()
                op("dve", lambda e: e.tensor_scalar(out=dt_[:], in0=ident_f[:], scalar1=mod[:, off + c:off + c + 1], scalar2=None, op0=ALU.mult), r=[kc, k_mod], w=[dk_])
                op("pe", lambda e: e.matmul(PS.t[pb][:, j * 128:(j + 1) * 128], lhsT=ones_f[:], rhs=dt_[:], start=True, stop=True), r=[kc, dk_], w=[PS.k[pb]])
            op("act", lambda e: e.copy(out=GB[:, q4 * 512:(q4 + 1) * 512], in_=PS.t[pb][:]), r=[PS.k[pb]], w=[k_G])
            PS.rel(pb)
    op("act", lambda e: e.activation(out=nalog[:], in_=alog_s[:], func=AF.Exp), r=[k_alog], w=[k_nalog])
    op("dve", lambda e: e.tensor_scalar(out=nalog[:], in0=nalog[:], scalar1=-1.0, scalar2=None, op0=ALU.mult), r=[k_nalog], w=[k_nalog])

    chk(1)
    XT = Ring(S, nc, "xtile", [128, D], F32, 2, dma=True)
    XN = Ring(S, nc, "xn", [128, D], BF16, 1)
    hT = sb("hT", [128, KC, BLK], BF16); k_hT = Trk()
    mixT = sb("mixT", [128, KC, BLK], BF16); k_mix = [Trk() for _ in range(KC)]
    h2T = sb("h2T", [128, KC, BLK], BF16); k_h2T = Trk(); sem_h2 = S.newsem("d_h2T")
    UB = Ring(S, nc, "ub", [128, BLK + 3], F32, 2)
    CA = Ring(S, nc, "ca", [128, BLK], F32, 2)
    hist = sb("hist", [128, 24, 3]); k_hist = [Trk() for _ in range(24)]
    op("pool", lambda e: e.memset(hist[:], 0.0), w=k_hist)
    QKV = sb("QKV", [128, 24, BLK], BF16); k_qkv = [Trk() for _ in range(24)]
    ZS = sb("ZS", [128, 8, BLK], BF16); k_zs = [Trk() for _ in range(8)]
    QbT = sb("QbT", [128, 8, BLK], BF16); k_qb = [Trk() for _ in range(8)]
    KbT = sb("KbT", [128, 8, BLK], BF16); k_kb = Trk(); sem_kb = S.newsem("d_kb")
    VbT = Ring(S, nc, "vbt", [128, 1024], BF16, 2, dma=True)
    FK = sb("FK", [128, NKT, H]); k_fk = [Trk() for _ in range(NKT)]
    Fcar = sb("Fcar", [128, H]); k_fcar = Trk()
    op("pool", lambda e: e.memset(Fcar[:], 0.0), w=[k_fcar])
    FqT = sb("FqT", [24, BLK], BF16); k_fq = Trk()
    Sst = sb("Sst", [128, H, 128]); Sbf = sb("Sbf", [128, H, 128], BF16); k_S = [Trk() for _ in range(H)]; k_Sb = [Trk() for _ in range(H)]
    op("pool", lambda e: e.memset(Sst[:], 0.0), w=k_S)
    op("pool", lambda e: e.memset(Sbf[:], 0.0), w=k_Sb)
    GT = Ring(S, nc, "gt", [128, 64], F32, 2)
    GC = Ring(S, nc, "gc", [128, 8 * H], F32, 4)
    T32 = Ring(S, nc, "t32", [128, 128], F32, 6)
    NSLOT = 8
    BFN = ["X", "Y", "qkT", "qdT", "TmA", "TmB", "TtA", "TtB", "Ul", "W1s", "ke", "kd", "vtk", "wT", "vn"]
    SL = []
    for hs in range(NSLOT):
        d_ = {nm: (sb(f"sl{hs}_{nm}", [128, 128], BF16), Trk()) for nm in BFN}
        d_["u2"] = (sb(f"sl{hs}_u2", [128, 128], F32), Trk())
        SL.append(d_)
    f3t = sb("f3t", [128, 24], BF16); k_f3 = Trk()
    r3t = sb("r3t", [128, 16], F32); k_r3 = Trk()
    OTB = sb("OTB", [128, H, BLK], F32); k_ot = [Trk() for _ in range(H)]
    W32 = Ring(S, nc, "w32", [128, BLK], F32, 4)
    W16 = Ring(S, nc, "w16", [128, BLK], BF16, 4)
    KR = Ring(S, nc, "kr", [128, 1024], BF16, 2, dma=True)
    VR = Ring(S, nc, "vr", [128, 8, 128], BF16, 2, dma=True)
    XP = Ring(S, nc, "xpc", [128, 256], F32, 2, dma=True)
    X1P = Ring(S, nc, "x1p", [128, 256], F32, 2, dma=True)
    RT = Ring(S, nc, "rt", [128, 64], F32, 2)

    def dbg_dump(name, src_ap, trk):
        if dbg_t is None or name not in dbg_t:
            return
        sem = S.newsem("d_dbg_" + name + str(S.nops))
        op("pool" if src_ap.dtype != F32 else "sp", lambda e: e.dma_start(out=dbg_t[name], in_=src_ap), r=trk, dma=sem)
        S.eng["sp"].wait_ge(S.sems[sem], S.cnt[sem])

    def rstd_from_ssq(st, sk, col_in, col_out, n):
        op("act", lambda e: e.activation(out=st[:, col_out:col_out + 1], in_=st[:, col_in:col_in + 1], func=AF.Ln, scale=1.0 / n, bias=EPS), r=[sk], w=[sk])
        op("act", lambda e: e.activation(out=st[:, col_out:col_out + 1], in_=st[:, col_out:col_out + 1], func=AF.Exp, scale=-0.5), r=[sk], w=[sk])

    def norm_to_T(src_rows, dst, dst_k, Av, Bv, x_from=None):
        for t in range(BLK // 128):
            xt, xk, xd = XT.next()
            op("sp", lambda e: e.dma_start(out=xt[:], in_=src_rows[t * 128:(t + 1) * 128, :]), r=(x_from or ()), w=[xk], dma=xd)
            st, sk, _ = stat.next()
            xn, nk, _ = XN.next()
            op("act", lambda e: e.activation(out=xn[:], in_=xt[:], func=AF.Square, accum_out=st[:, 0:1]), r=[xk], w=[nk, sk])
            rstd_from_ssq(st, sk, 0, 1, D)
            op("dve", lambda e: e.tensor_scalar(out=xn[:], in0=xt[:], scalar1=st[:, 1:2], scalar2=None, op0=ALU.mult), r=[xk, sk], w=[nk])
            for c4 in range(4):
                pb = PS.get()
                pv = PS.t[pb][:].bitcast(BF16)
                for j in range(4):
                    c = c4 * 4 + j
                    op("pe", lambda e: e.transpose(out=pv[:, j * 128:(j + 1) * 128], in_=xn[:, c * 128:(c + 1) * 128], identity=ident_b[:]), r=[nk, kc], w=[PS.k[pb]])
                for j in range(4):
                    c = c4 * 4 + j
                    eng = "act" if c4 % 2 == 0 else "dve"
                    if eng == "act":
                        op("act", lambda e: e.activation(out=dst[:, c, t * 128:(t + 1) * 128], in_=pv[:, j * 128:(j + 1) * 128], func=AF.Identity, scale=Av[:, c:c + 1], bias=Bv[:, c:c + 1]),
                           r=[PS.k[pb], k_vec], w=[dst_k])
                    else:
                        op("dve", lambda e: e.tensor_scalar(out=dst[:, c, t * 128:(t + 1) * 128], in0=pv[:, j * 128:(j + 1) * 128], scalar1=Av[:, c:c + 1], scalar2=Bv[:, c:c + 1], op0=ALU.mult, op1=ALU.add),
                           r=[PS.k[pb], k_vec], w=[dst_k])
                PS.rel(pb)

    wr_sp_sem = {}

    def load_unit(col0, ncols=256, src=None):
        wt, wk, wd = WR.next()
        if src is None:
            if wd not in wr_sp_sem:
                wr_sp_sem[wd] = S.newsem(wd + "_sp")
            wd = wr_sp_sem[wd]
            op("sp", lambda e: e.dma_start(out=wt[:, :, 0:ncols], in_=win_d[:, col0:col0 + ncols].rearrange("(c p) f -> p c f", p=128)), r=[k_win], w=[wk], dma=wd)
        else:
            op("pool", lambda e: e.dma_start(out=wt[:, :, 0:ncols], in_=src[:, col0:col0 + ncols].rearrange("(c p) f -> p c f", p=128)), w=[wk], dma=wd)
        return wt, wk

    def proj_fm(wt, wk, j):
        pb = PS.get()
        for k in range(KC):
            op("pe", lambda e: e.matmul(PS.t[pb][:, 0:BLK], lhsT=wt[:, k, j * 128:(j + 1) * 128], rhs=hT[:, k, :], start=(k == 0), stop=(k == KC - 1)),
               r=[wk, k_hT], w=[PS.k[pb]], inc=(k == KC - 1))
        return pb

    def silu_from(src_ap, src_k, dst_ap, dst_k):
        op("act", lambda e: e.activation(out=dst_ap, in_=src_ap, func=AF.Silu), r=src_k, w=dst_k)

    for blk in range(NB):
        own = blk >= NBP
        tok0 = blk * BLK
        src = (xo[(blk - NBP) * BLK:(blk - NBP + 1) * BLK, :] if own else xp[blk * BLK:(blk + 1) * BLK, :])
        norm_to_T(src, hT, k_hT, A1 if own else A1p, B1 if own else B1p)

        chk(2)
        gres = []
        for t in range(BLK // 128):
            kt = blk * 2 + t
            pb = PS.get()
            for k in range(KC):
                op("pe", lambda e: e.matmul(PS.t[pb][:, 0:24], lhsT=hT[:, k, t * 128:(t + 1) * 128], rhs=wsm[:, k, :], start=(k == 0), stop=(k == KC - 1)),
                   r=[k_hT, k_wsm], w=[PS.k[pb]], inc=(k == KC - 1))
            g, gk, _ = GT.next()
            R, Rk, _ = GC.next()
            P = PS.t[pb]
            op("dve", lambda e: e.tensor_tensor(out=g[:, 0:8], in0=P[:, 0:8], in1=dtb_s[:], op=ALU.add), r=[PS.k[pb], k_dtb], w=[gk])
            op("act", lambda e: e.activation(out=g[:, 0:8], in_=g[:, 0:8], func=AF.Exp), r=[gk], w=[gk])
            op("act", lambda e: e.activation(out=g[:, 0:8], in_=g[:, 0:8], func=AF.Ln, bias=1.0), r=[gk], w=[gk])
            op("dve", lambda e: e.tensor_tensor(out=R[:, 0:8], in0=g[:, 0:8], in1=nalog[:], op=ALU.mult), r=[gk, k_nalog], w=[Rk])
            op("act", lambda e: e.activation(out=g[:, 8:16], in_=P[:, 8:16], func=AF.Exp, scale=-1.0), r=[PS.k[pb]], w=[gk])
            op("act", lambda e: e.activation(out=g[:, 8:16], in_=g[:, 8:16], func=AF.Ln, bias=1.0), r=[gk], w=[gk])
            op("dve", lambda e: e.tensor_scalar(out=R[:, 8:16], in0=g[:, 8:16], scalar1=-1.0, scalar2=None, op0=ALU.mult), r=[gk], w=[Rk])
            op("act", lambda e: e.activation(out=R[:, 16:24], in_=g[:, 8:16], func=AF.Exp, scale=-1.0), r=[gk], w=[Rk])
            op("dve", lambda e: e.tensor_scalar(out=R[:, 24:32], in0=R[:, 16:24], scalar1=-1.0, scalar2=None, op0=ALU.mult), r=[Rk], w=[Rk])
            op("dve", lambda e: e.tensor_tensor(out=g[:, 16:24], in0=P[:, 16:24], in1=ffb_s[:], op=ALU.add), r=[PS.k[pb], k_ffb], w=[gk])
            op("act", lambda e: e.activation(out=g[:, 16:24], in_=g[:, 16:24], func=AF.Exp, scale=-1.0), r=[gk], w=[gk])
            op("act", lambda e: e.activation(out=g[:, 16:24], in_=g[:, 16:24], func=AF.Ln, bias=1.0), r=[gk], w=[gk])
            op("dve", lambda e: e.tensor_scalar(out=g[:, 16:24], in0=g[:, 16:24], scalar1=-1.0, scalar2=None, op0=ALU.mult), r=[gk], w=[gk])
            PS.rel(pb)
            pc = PS.get()
            Pc = PS.t[pc]
            op("pe", lambda e: e.matmul(Pc[:, 0:8], lhsT=utri[:], rhs=R[:, 0:8], start=True, stop=True), r=[kc, Rk], w=[PS.k[pc]])
            op("pe", lambda e: e.matmul(Pc[:, 8:16], lhsT=utri[:], rhs=g[:, 16:24], start=True, stop=True), r=[kc, gk], w=[PS.k[pc]])
            op("pe", lambda e: e.matmul(Pc[:, 16:24], lhsT=ones_f[:], rhs=R[:, 0:8], start=True, stop=True), r=[kc, Rk], w=[PS.k[pc]])
            op("pe", lambda e: e.matmul(Pc[:, 24:32], lhsT=ones_f[:], rhs=g[:, 16:24], start=True, stop=True), r=[kc, gk], w=[PS.k[pc]])
            op("dve", lambda e: e.tensor_copy(out=R[:, 32:40], in_=Pc[:, 0:8]), r=[PS.k[pc]], w=[Rk])
            op("dve", lambda e: e.tensor_tensor(out=R[:, 40:48], in0=Pc[:, 0:8], in1=R[:, 8:16], op=ALU.subtract), r=[PS.k[pc], Rk], w=[Rk])
            op("act", lambda e: e.activation(out=R[:, 48:56], in_=Pc[:, 0:8], func=AF.Exp), r=[PS.k[pc]], w=[Rk])
            op("dve", lambda e: e.tensor_tensor(out=g[:, 24:32], in0=Pc[:, 16:24], in1=R[:, 32:40], op=ALU.subtract), r=[PS.k[pc], Rk], w=[gk])
            op("act", lambda e: e.activation(out=R[:, 56:64], in_=g[:, 24:32], func=AF.Exp), r=[gk], w=[Rk])
            op("act", lambda e: e.activation(out=g[:, 32:40], in_=Pc[:, 16:24], func=AF.Exp), r=[PS.k[pc]], w=[gk])
            op("dve", lambda e: e.tensor_tensor(out=g[:, 40:48], in0=Pc[:, 8:16], in1=Fcar[:], op=ALU.add), r=[PS.k[pc], k_fcar], w=[gk])
            if own:
                op("dve", lambda e: e.tensor_scalar(out=FK[:, kt, :], in0=g[:, 40:48], scalar1=-1.0, scalar2=None, op0=ALU.mult), r=[gk], w=[k_fk[kt]])
            else:
                op("dve", lambda e: e.tensor_scalar(out=FK[:, kt, :], in0=g[:, 40:48], scalar1=-1.0, scalar2=pm_s[:, 1:2], op0=ALU.mult, op1=ALU.add), r=[gk, k_pm], w=[k_fk[kt]])
            op("dve", lambda e: e.tensor_tensor(out=Fcar[:], in0=Fcar[:], in1=Pc[:, 24:32], op=ALU.add), r=[PS.k[pc], k_fcar], w=[k_fcar])
            PS.rel(pc)
            if own:
                f3, f3k = f3t, k_f3
                r3, r3k = r3t, k_r3
                op("dve", lambda e: e.tensor_copy(out=f3[:, 0:8], in_=g[:, 40:48]), r=[gk], w=[f3k])
                op("dve", lambda e: e.tensor_tensor(out=r3[:, 0:8], in0=g[:, 40:48], in1=f3[:, 0:8], op=ALU.subtract), r=[gk, f3k], w=[r3k])
                op("dve", lambda e: e.tensor_copy(out=f3[:, 8:16], in_=r3[:, 0:8]), r=[r3k], w=[f3k])
                op("dve", lambda e: e.tensor_tensor(out=r3[:, 8:16], in0=r3[:, 0:8], in1=f3[:, 8:16], op=ALU.subtract), r=[r3k, f3k], w=[r3k])
                op("dve", lambda e: e.tensor_copy(out=f3[:, 16:24], in_=r3[:, 8:16]), r=[r3k], w=[f3k])
                pt = PS.get()
                ptv = PS.t[pt][:].bitcast(BF16)
                op("pe", lambda e: e.transpose(out=ptv[0:24, 0:128], in_=f3[:, 0:24], identity=ident_b[:]), r=[f3k, kc], w=[PS.k[pt]])
                op("act", lambda e: e.copy(out=FqT[:, t * 128:(t + 1) * 128], in_=ptv[0:24, 0:128]), r=[PS.k[pt]], w=[k_fq])
                PS.rel(pt)
            gres.append((g, gk, R, Rk))

        chk(3)
        def conv_unit(col0, ch0):
            for j4 in range(4):
                if j4 % 2 == 0:
                    wt, wk = load_unit(col0 + (j4 // 2) * 256)
                j = j4 % 2
                ch = ch0 + j4
                pb = proj_fm(wt, wk, j)
                u, uk, _ = UB.next()
                op("act", lambda e: e.copy(out=u[:, 3:3 + BLK], in_=PS.t[pb][:, 0:BLK]), r=[PS.k[pb]], w=[uk])
                PS.rel(pb)
                op("pool", lambda e: e.tensor_copy(out=u[:, 0:3], in_=hist[:, ch, :]), r=[k_hist[ch]], w=[uk])
                op("pool", lambda e: e.tensor_copy(out=hist[:, ch, :], in_=u[:, BLK:BLK + 3]), r=[uk], w=[k_hist[ch]])
                a, ak, _ = CA.next()
                op("dve", lambda e: e.tensor_scalar(out=a[:], in0=u[:, 0:BLK], scalar1=convw_s[:, ch * 4:ch * 4 + 1], scalar2=None, op0=ALU.mult), r=[uk, k_convw], w=[ak])
                for tp in range(1, 4):
                    op("dve", lambda e: e.scalar_tensor_tensor(out=a[:], in0=u[:, tp:tp + BLK], scalar=convw_s[:, ch * 4 + tp:ch * 4 + tp + 1], in1=a[:], op0=ALU.mult, op1=ALU.add), r=[uk, k_convw, ak], w=[ak])
                silu_from(a[:], [ak], QKV[:, ch, :], [k_qkv[ch]])

        if own:
            conv_unit(C_QA, 0); conv_unit(C_QA + 512, 4)
        conv_unit(C_KA, 8); conv_unit(C_KA + 512, 12)
        conv_unit(C_VA, 16); conv_unit(C_VA + 512, 20)
        if own:
            for q4 in range(4):
                wt, wk = load_unit(C_ZA + q4 * 256)
                for j in range(2):
                    pb = proj_fm(wt, wk, j)
                    silu_from(PS.t[pb][:, 0:BLK], [PS.k[pb]], ZS[:, q4 * 2 + j, :], [k_zs[q4 * 2 + j]])
                    PS.rel(pb)
            for q4 in range(4):
                wt, wk = load_unit(C_QB + q4 * 256)
                for j in range(2):
                    pb = proj_fm(wt, wk, j)
                    hh = q4 * 2 + j
                    op("act", lambda e: e.activation(out=QbT[:, hh, :], in_=PS.t[pb][:, 0:BLK], func=AF.Copy, scale=128.0 ** -0.5), r=[PS.k[pb]], w=[k_qb[hh]])
                    PS.rel(pb)
        for q4 in range(4):
            wt, wk = load_unit(C_KB + q4 * 256)
            for j in range(2):
                pb = proj_fm(wt, wk, j)
                hh = q4 * 2 + j
                op("dve", lambda e: e.tensor_copy(out=KbT[:, hh, :], in_=PS.t[pb][:, 0:BLK]), r=[PS.k[pb]], w=[k_kb])
                PS.rel(pb)
        op("sp", lambda e: e.dma_start(out=kt_d[:, :, tok0:tok0 + BLK].rearrange("h p t -> p h t"), in_=KbT[:]), r=[k_kb], w=[k_ktd[blk]], dma=sem_kb)
        vts = [VbT.next() for _ in range(BLK // 128)]
        for q4 in range(4):
            wt, wk = load_unit(C_VB + q4 * 256)
            for t in range(BLK // 128):
                vt, vk, vd = vts[t]
                pb = PS.get()
                for k in range(KC):
                    op("pe", lambda e: e.matmul(PS.t[pb][:, 0:256], lhsT=hT[:, k, t * 128:(t + 1) * 128], rhs=wt[:, k, :], start=(k == 0), stop=(k == KC - 1)),
                       r=[wk, k_hT], w=[PS.k[pb]], inc=(k == KC - 1))
                if (q4 + t) % 2 == 0:
                    op("act", lambda e: e.copy(out=vt[:, q4 * 256:(q4 + 1) * 256], in_=PS.t[pb][:, 0:256]), r=[PS.k[pb]], w=[vk])
                else:
                    op("dve", lambda e: e.tensor_copy(out=vt[:, q4 * 256:(q4 + 1) * 256], in_=PS.t[pb][:, 0:256]), r=[PS.k[pb]], w=[vk])
                PS.rel(pb)
        for t in range(BLK // 128):
            vt, vk, vd = vts[t]
            op("sp", lambda e: e.dma_start(out=v_d[tok0 + t * 128:tok0 + (t + 1) * 128, :], in_=vt[:]), r=[vk], w=[k_vd[blk]], dma=vd)

        chk(4)
        def l2n(ch, qscale):
            sq, sqk, _ = W16.next()
            op("act", lambda e: e.activation(out=sq[:], in_=QKV[:, ch, :], func=AF.Square), r=[k_qkv[ch]], w=[sqk])
            pb = PS.get()
            op("pe", lambda e: e.matmul(PS.t[pb][:, 0:BLK], lhsT=ones_b[:], rhs=sq[:], start=True, stop=True), r=[kc, sqk], w=[PS.k[pb]])
            rn, rnk, _ = W32.next()
            op("act", lambda e: e.activation(out=rn[:], in_=PS.t[pb][:, 0:BLK], func=AF.Ln, bias=EPS), r=[PS.k[pb]], w=[rnk])
            PS.rel(pb)
            op("act", lambda e: e.activation(out=rn[:], in_=rn[:], func=AF.Exp, scale=-0.5), r=[rnk], w=[rnk])
            op("dve", lambda e: e.scalar_tensor_tensor(out=QKV[:, ch, :], in0=QKV[:, ch, :], scalar=qscale, in1=rn[:], op0=ALU.mult, op1=ALU.mult), r=[k_qkv[ch], rnk], w=[k_qkv[ch]])

        for h in range(H):
            if own:
                l2n(h, 128.0 ** -0.5)
            l2n(8 + h, 1.0)

        def fox_head(h):
            nkt = (blk + 1) * 2
            pO = PS.get(); pR = PS.get()
            pend = []

            def emit_pv(it):
                pT_, pTk, vr_, vrk_, kk_, kt_ = it
                first, last = (kt_ == 0), (kt_ == nkt - 1)
                op("pe", lambda e: e.matmul(PS.t[pO][:, 0:BLK], lhsT=vr_[:, kk_, :], rhs=pT_[:], start=first, stop=last), r=[vrk_, pTk], w=[PS.k[pO]], inc=last)
                op("pe", lambda e: e.matmul(PS.t[pR][:, 0:BLK], lhsT=ones_b[:], rhs=pT_[:], start=first, stop=last), r=[kc, pTk], w=[PS.k[pR]], inc=last)

            for g8 in range(0, nkt, 8):
                n8 = min(8, nkt - g8)
                kr, krk, krd = KR.next()
                vr, vrk, vrd = VR.next()
                bl = list(range(g8 * 128 // BLK, (g8 + n8) * 128 // BLK))
                op("sp", lambda e: e.dma_start(out=kr[:, 0:n8 * 128], in_=kt_d[h, :, g8 * 128:(g8 + n8) * 128]), r=[k_ktd[b] for b in bl], w=[krk], dma=krd)
                op("sp", lambda e: e.dma_start(out=vr[:, 0:n8, :], in_=v_d[g8 * 128:(g8 + n8) * 128, h * 128:(h + 1) * 128].rearrange("(k p) d -> p k d", p=128)), r=[k_vd[b] for b in bl], w=[vrk], dma=vrd)
                for kk in range(n8):
                    kt = g8 + kk
                    pS = PS.get()
                    op("pe", lambda e: e.matmul(PS.t[pS][:, 0:BLK], lhsT=kr[:, kk * 128:(kk + 1) * 128], rhs=QbT[:, h, :], start=True, stop=False), r=[krk, k_qb[h]], w=[PS.k[pS]], inc=False)
                    op("pe", lambda e: e.matmul(PS.t[pS][:, 0:BLK], lhsT=sel[:, h * 128:(h + 1) * 128], rhs=FqT[:], start=False, stop=True), r=[kc, k_fq], w=[PS.k[pS]])
                    pT_, pTk, _ = W16.next()
                    op("act", lambda e: e.activation(out=pT_[:], in_=PS.t[pS][:, 0:BLK], func=AF.Exp, bias=FK[:, kt, h:h + 1]), r=[PS.k[pS], k_fk[kt]], w=[pTk])
                    PS.rel(pS)
                    if kt >= blk * 2:
                        op("pool", lambda e: e.affine_select(out=pT_[:], in_=pT_[:], pattern=[[1, BLK]], compare_op=ALU.is_ge, fill=0.0, base=tok0 - kt * 128, channel_multiplier=-1), r=[pTk], w=[pTk])
                    pend.append((pT_, pTk, vr, vrk, kk, kt))
                    if len(pend) > 2:
                        emit_pv(pend.pop(0))
            while pend:
                emit_pv(pend.pop(0))
            ri, rik, _ = W32.next()
            op("dve", lambda e: e.reciprocal(out=ri[:], in_=PS.t[pR][:, 0:BLK]), r=[PS.k[pR]], w=[rik])
            op("dve", lambda e: e.tensor_tensor(out=mixT[:, 8 + h, :], in0=PS.t[pO][:, 0:BLK], in1=ri[:], op=ALU.mult), r=[PS.k[pO], rik, k_hT], w=[k_mix[8 + h]])
            PS.rel(pO); PS.rel(pR)


        fox_q = list(range(H)) if own else []

        def pump():
            if fox_q:
                fox_head(fox_q.pop(0))

        chk(41)
        for t in range(BLK // 128):
            g, gk, R, Rk = gres[t]
            ts_ = slice(t * 128, (t + 1) * 128)
            for hg in range(H // NSLOT):
                heads = [hg * NSLOT + i for i in range(NSLOT)]
                for hs, h in enumerate(heads):
                    sl = SL[hs]
                    knT = QKV[:, 8 + h, ts_]; kkn = k_qkv[8 + h]
                    gc_col = R[:, 32 + h:33 + h]; gcb_col = R[:, 40 + h:41 + h]
                    d1, d1k, _ = T32.next(); d2, d2k, _ = T32.next()
                    op("dve", lambda e: e.tensor_scalar(out=d1[:], in0=utri[:], scalar1=R[:, h:h + 1], scalar2=None, op0=ALU.mult), r=[kc, Rk], w=[d1k])
                    op("dve", lambda e: e.scalar_tensor_tensor(out=d2[:], in0=ident_f[:], scalar=R[:, 8 + h:9 + h], in1=d1[:], op0=ALU.mult, op1=ALU.subtract), r=[kc, Rk, d1k], w=[d2k])
                    pG = PS.get(); PG = PS.t[pG]
                    op("pe", lambda e: e.matmul(PG[:, 0:128], lhsT=ones_f[:], rhs=d1[:], start=True, stop=True), r=[kc, d1k], w=[PS.k[pG]])
                    op("pe", lambda e: e.matmul(PG[:, 128:256], lhsT=ones_f[:], rhs=d2[:], start=True, stop=True), r=[kc, d2k], w=[PS.k[pG]])
                    pK = PS.get(); PK = PS.t[pK]
                    op("pe", lambda e: e.matmul(PK[:, 0:128], lhsT=knT, rhs=knT, start=True, stop=True), r=[kkn], w=[PS.k[pK]])
                    if own:
                        op("pe", lambda e: e.matmul(PK[:, 128:256], lhsT=knT, rhs=QKV[:, h, ts_], start=True, stop=True), r=[kkn, k_qkv[h]], w=[PS.k[pK]])
                    e1, e1k, _ = T32.next()
                    op("dve", lambda e: e.scalar_tensor_tensor(out=e1[:], in0=PG[:, 128:256], scalar=gc_col, in1=m_posL[:], op0=ALU.add, op1=ALU.subtract), r=[PS.k[pG], Rk, kc], w=[e1k])
                    op("dve", lambda e: e.tensor_scalar(out=e1[:], in0=e1[:], scalar1=0.0, scalar2=None, op0=ALU.min), r=[e1k], w=[e1k])
                    op("act", lambda e: e.activation(out=e1[:], in_=e1[:], func=AF.Exp), r=[e1k], w=[e1k])
                    X, Xk = sl["X"]
                    op("dve", lambda e: e.scalar_tensor_tensor(out=X[:], in0=e1[:], scalar=-1.0, in1=PK[:, 0:128], op0=ALU.mult, op1=ALU.mult), r=[e1k, PS.k[pK]], w=[Xk])
                    e2, e2k, _ = T32.next()
                    op("dve", lambda e: e.scalar_tensor_tensor(out=e2[:], in0=PG[:, 0:128], scalar=gcb_col, in1=m_negUs[:], op0=ALU.subtract, op1=ALU.add), r=[PS.k[pG], Rk, kc], w=[e2k])
                    op("dve", lambda e: e.tensor_scalar(out=e2[:], in0=e2[:], scalar1=0.0, scalar2=None, op0=ALU.min), r=[e2k], w=[e2k])
                    op("act", lambda e: e.activation(out=e2[:], in_=e2[:], func=AF.Exp), r=[e2k], w=[e2k])
                    Y, Yk = sl["Y"]
                    op("dve", lambda e: e.scalar_tensor_tensor(out=Y[:], in0=e2[:], scalar=-1.0, in1=PK[:, 0:128], op0=ALU.mult, op1=ALU.mult), r=[e2k, PS.k[pK]], w=[Yk])
                    if own:
                        e3, e3k, _ = T32.next()
                        op("dve", lambda e: e.scalar_tensor_tensor(out=e3[:], in0=PG[:, 0:128], scalar=gc_col, in1=m_negUi[:], op0=ALU.subtract, op1=ALU.add), r=[PS.k[pG], Rk, kc], w=[e3k])
                        op("dve", lambda e: e.tensor_scalar(out=e3[:], in0=e3[:], scalar1=0.0, scalar2=None, op0=ALU.min), r=[e3k], w=[e3k])
                        op("act", lambda e: e.activation(out=e3[:], in_=e3[:], func=AF.Exp), r=[e3k], w=[e3k])
                        qkT, qkk = sl["qkT"]
                        op("dve", lambda e: e.tensor_tensor(out=qkT[:], in0=e3[:], in1=PK[:, 128:256], op=ALU.mult), r=[e3k, PS.k[pK]], w=[qkk])
                        e4, e4k, _ = T32.next()
                        op("act", lambda e: e.activation(out=e4[:], in_=PG[:, 0:128], func=AF.Exp), r=[PS.k[pG]], w=[e4k])
                        qdT, qdk = sl["qdT"]
                        op("dve", lambda e: e.tensor_tensor(out=qdT[:], in0=e4[:], in1=QKV[:, h, ts_], op=ALU.mult), r=[e4k, k_qkv[h]], w=[qdk])
                    PS.rel(pG); PS.rel(pK)
                    pump()
                chk(42)
                for hs, h in enumerate(heads):
                    sl = SL[hs]
                    X, Xk = sl["X"]; Y, Yk = sl["Y"]
                    m0, m0k = sl["Ul"]; m1, m1k = sl["W1s"]
                    Tm, Tmk = sl["TmA"]; Tt, Ttk = sl["TtA"]
                    op("pool", lambda e: e.tensor_tensor(out=m0[:], in0=X[:], in1=LM[:, 0, :], op=ALU.mult), r=[Xk, k_lm], w=[m0k])
                    op("dve", lambda e: e.tensor_tensor(out=Tm[:], in0=m0[:], in1=ident_f[:], op=ALU.add), r=[m0k, kc], w=[Tmk])
                    op("pool", lambda e: e.tensor_tensor(out=m1[:], in0=Y[:], in1=LM[:, 1, :], op=ALU.mult), r=[Yk, k_lm], w=[m1k])
                    op("dve", lambda e: e.tensor_tensor(out=Tt[:], in0=m1[:], in1=ident_f[:], op=ALU.add), r=[m1k, kc], w=[Ttk])
                cur = "A"
                for lvl in range(1, 7):
                    nxt = "B" if cur == "A" else "A"
                    for hs, h in enumerate(heads):
                        sl = SL[hs]
                        Y, Yk = sl["Y"]; Ul, Ulk = sl["Ul"]; W1s, W1k = sl["W1s"]; Tm, Tmk = sl["Tm" + cur]
                        op("pool", lambda e: e.tensor_tensor(out=Ul[:], in0=Y[:], in1=LM[:, 1 + lvl, :], op=ALU.mult), r=[Yk, k_lm], w=[Ulk])
                        pI = PS.get()
                        op("pe", lambda e: e.matmul(PS.t[pI][:, 0:128], lhsT=Ul[:], rhs=Tm[:], start=True, stop=True), r=[Ulk, Tmk], w=[PS.k[pI]])
                        op("act", lambda e: e.copy(out=W1s[:], in_=PS.t[pI][:, 0:128]), r=[PS.k[pI]], w=[W1k])
                        PS.rel(pI)
                    for hs, h in enumerate(heads):
                        sl = SL[hs]
                        W1s, W1k = sl["W1s"]; Tm, Tmk = sl["Tm" + cur]; Tt, Ttk = sl["Tt" + cur]; Tn, Tnk = sl["Tm" + nxt]
                        pJ = PS.get()
                        op("pe", lambda e: e.matmul(PS.t[pJ][:, 0:128], lhsT=Tt[:], rhs=W1s[:], start=True, stop=True), r=[Ttk, W1k], w=[PS.k[pJ]])
                        op("dve", lambda e: e.tensor_tensor(out=Tn[:], in0=PS.t[pJ][:, 0:128], in1=Tm[:], op=ALU.add), r=[PS.k[pJ], Tmk], w=[Tnk])
                        PS.rel(pJ)
                    for hs, h in enumerate(heads):
                        sl = SL[hs]
                        Tn, Tnk = sl["Tm" + nxt]; Ttn, Ttnk = sl["Tt" + nxt]
                        pL = PS.get()
                        PLb = PS.t[pL][:].bitcast(BF16)
                        op("pe", lambda e: e.transpose(out=PLb[:, 0:128], in_=Tn[:], identity=ident_b[:]), r=[Tnk, kc], w=[PS.k[pL]])
                        op("act", lambda e: e.copy(out=Ttn[:], in_=PLb[:, 0:128]), r=[PS.k[pL]], w=[Ttnk])
                        PS.rel(pL)
                    cur = nxt
                chk(43)
                for hs, h in enumerate(heads):
                    sl = SL[hs]
                    knT = QKV[:, 8 + h, ts_]; kkn = k_qkv[8 + h]
                    vT = QKV[:, 16 + h, ts_]; kvn = k_qkv[16 + h]
                    ke, kek = sl["ke"]; kd, kdk = sl["kd"]; vtk, vtkk = sl["vtk"]
                    pT = PS.get()
                    PTb = PS.t[pT][:].bitcast(BF16)
                    op("pe", lambda e: e.transpose(out=PTb[:, 0:128], in_=knT, identity=ident_b[:]), r=[kkn, kc], w=[PS.k[pT]])
                    op("dve", lambda e: e.tensor_scalar(out=ke[:], in0=PTb[:, 0:128], scalar1=R[:, 48 + h:49 + h], scalar2=None, op0=ALU.mult), r=[PS.k[pT], Rk], w=[kek])
                    op("dve", lambda e: e.tensor_scalar(out=kd[:], in0=PTb[:, 0:128], scalar1=R[:, 56 + h:57 + h], scalar2=None, op0=ALU.mult), r=[PS.k[pT], Rk], w=[kdk])
                    PS.rel(pT)
                    pT2 = PS.get()
                    PTb2 = PS.t[pT2][:].bitcast(BF16)
                    op("pe", lambda e: e.transpose(out=PTb2[:, 0:128], in_=vT, identity=ident_b[:]), r=[kvn, kc], w=[PS.k[pT2]])
                    op("act", lambda e: e.copy(out=vtk[:], in_=PTb2[:, 0:128]), r=[PS.k[pT2]], w=[vtkk])
                    PS.rel(pT2)
                chk(44)
                for hs, h in enumerate(heads):
                    sl = SL[hs]
                    Rm, Rmk = sl["Tt" + cur]
                    ke, kek = sl["ke"]; vtk, vtkk = sl["vtk"]; u2, u2k = sl["u2"]; wT, wTk = sl["wT"]
                    pU = PS.get()
                    op("pe", lambda e: e.matmul(PS.t[pU][:, 0:128], lhsT=Rm[:], rhs=vtk[:], start=True, stop=True), r=[Rmk, vtkk], w=[PS.k[pU]])
                    op("act", lambda e: e.activation(out=u2[:], in_=PS.t[pU][:, 0:128], func=AF.Identity, scale=R[:, 16 + h:17 + h]), r=[PS.k[pU], Rk], w=[u2k])
                    PS.rel(pU)
                    pU2 = PS.get()
                    op("pe", lambda e: e.matmul(PS.t[pU2][:, 0:128], lhsT=ke[:], rhs=Rm[:], start=True, stop=True), r=[Rmk, kek], w=[PS.k[pU2]])
                    op("dve", lambda e: e.tensor_copy(out=wT[:], in_=PS.t[pU2][:, 0:128]), r=[PS.k[pU2]], w=[wTk])
                    PS.rel(pU2)
                chk(45)
                for hs, h in enumerate(heads):
                    sl = SL[hs]
                    wT, wTk = sl["wT"]; vn, vnk = sl["vn"]; u2, u2k = sl["u2"]
                    pS1 = PS.get()
                    op("pe", lambda e: e.matmul(PS.t[pS1][:, 0:128], lhsT=wT[:], rhs=Sbf[:, h, :], start=True, stop=True), r=[wTk, k_Sb[h]], w=[PS.k[pS1]])
                    op("dve", lambda e: e.scalar_tensor_tensor(out=vn[:], in0=PS.t[pS1][:, 0:128], scalar=R[:, 24 + h:25 + h], in1=u2[:], op0=ALU.mult, op1=ALU.add), r=[PS.k[pS1], Rk, u2k], w=[vnk])
                    PS.rel(pS1)
                for hs, h in enumerate(heads):
                    sl = SL[hs]
                    vn, vnk = sl["vn"]; kd, kdk = sl["kd"]
                    if own:
                        qdT, qdk = sl["qdT"]; qkT, qkk = sl["qkT"]
                        pS2 = PS.get()
                        op("pe", lambda e: e.matmul(PS.t[pS2][:, 0:128], lhsT=Sbf[:, h, :], rhs=qdT[:], start=True, stop=False), r=[k_Sb[h], qdk], w=[PS.k[pS2]], inc=False)
                        op("pe", lambda e: e.matmul(PS.t[pS2][:, 0:128], lhsT=vn[:], rhs=qkT[:], start=False, stop=True), r=[vnk, qkk], w=[PS.k[pS2]])
                        op("act", lambda e: e.copy(out=OTB[:, h, ts_], in_=PS.t[pS2][:, 0:128]), r=[PS.k[pS2]], w=[k_ot[h]])
                        PS.rel(pS2)
                    pS3 = PS.get()
                    op("pe", lambda e: e.matmul(PS.t[pS3][:, 0:128], lhsT=kd[:], rhs=vn[:], start=True, stop=True), r=[kdk, vnk], w=[PS.k[pS3]])
                    op("dve", lambda e: e.scalar_tensor_tensor(out=Sst[:, h, :], in0=Sst[:, h, :], scalar=g[:, 32 + h:33 + h], in1=PS.t[pS3][:, 0:128], op0=ALU.mult, op1=ALU.add), r=[k_S[h], gk, PS.k[pS3]], w=[k_S[h]])
                    op("act", lambda e: e.copy(out=Sbf[:, h, :], in_=Sst[:, h, :]), r=[k_S[h]], w=[k_Sb[h]])
                    PS.rel(pS3)
                chk(46)
        if own:
            for h in range(H):
                sq, sqk, _ = W16.next()
                op("act", lambda e: e.activation(out=sq[:], in_=OTB[:, h, :], func=AF.Square), r=[k_ot[h]], w=[sqk])
                pb = PS.get()
                op("pe", lambda e: e.matmul(PS.t[pb][:, 0:BLK], lhsT=ones_b[:], rhs=sq[:], start=True, stop=True), r=[kc, sqk], w=[PS.k[pb]])
                rn, rnk, _ = W32.next()
                op("act", lambda e: e.activation(out=rn[:], in_=PS.t[pb][:, 0:BLK], func=AF.Ln, scale=1.0 / 128, bias=EPS), r=[PS.k[pb]], w=[rnk])
                PS.rel(pb)
                op("act", lambda e: e.activation(out=rn[:], in_=rn[:], func=AF.Exp, scale=-0.5), r=[rnk], w=[rnk])
                op("dve", lambda e: e.scalar_tensor_tensor(out=rn[:], in0=OTB[:, h, :], scalar=ong_s[:, 0:1], in1=rn[:], op0=ALU.mult, op1=ALU.mult), r=[k_ot[h], k_ong, rnk], w=[rnk])
                op("dve", lambda e: e.tensor_tensor(out=mixT[:, h, :], in0=rn[:], in1=ZS[:, h, :], op=ALU.mult), r=[rnk, k_zs[h]], w=[k_mix[h]])

        if dbg and blk == NB - 1:
            dbg_dump("qkv", QKV[:, :, :], k_qkv)

        chk(5)
        if own:
            chk(6)
        if own:
            while fox_q:
                pump()

            if dbg and blk == NBP:
                dbg_dump("mixT", mixT[:, :, :].rearrange("p c t -> p (c t)"), k_mix)

            chk(7)
            ob = blk - NBP
            for dblk in range(8):
                wt, wk = load_unit(dblk * 256, src=w_out)
                c0_, c1_ = dblk * 256, (dblk + 1) * 256
                for t in range(BLK // 128):
                    r0 = ob * BLK + t * 128
                    xpi, xpk, xpd = XP.next()
                    op("sp", lambda e: e.dma_start(out=xpi[:, 0:256], in_=xo[r0:r0 + 128, c0_:c1_]), w=[xpk], dma=xpd)
                    pb = PS.get()
                    for m in range(KC):
                        op("pe", lambda e: e.matmul(PS.t[pb][:, 0:256], lhsT=mixT[:, m, t * 128:(t + 1) * 128], rhs=wt[:, m, :], start=(m == 0), stop=(m == KC - 1)),
                           r=[k_mix[m], wk], w=[PS.k[pb]], inc=(m == KC - 1))
                    x1p, x1k, x1d = X1P.next()
                    op("dve", lambda e: e.tensor_tensor(out=x1p[:, 0:256], in0=PS.t[pb][:, 0:256], in1=G1B[:, c0_:c1_], op=ALU.mult), r=[PS.k[pb], k_G], w=[x1k])
                    PS.rel(pb)
                    op("pool", lambda e: e.tensor_tensor(out=x1p[:, 0:256], in0=x1p[:, 0:256], in1=xpi[:, 0:256], op=ALU.add), r=[x1k, xpk], w=[x1k])
                    op("sp", lambda e: e.dma_start(out=x1_d[r0:r0 + 128, c0_:c1_], in_=x1p[:, 0:256]), r=[x1k], w=[k_x1d[ob * 2 + t]], dma=x1d)
            chk(8)
            norm_to_T(x1_d[ob * BLK:(ob + 1) * BLK, :], h2T, k_h2T, A2, B2, x_from=[k_x1d[ob * 2], k_x1d[ob * 2 + 1]])
            op("sp", lambda e: e.dma_start(out=h2_d[:, :, ob * BLK:(ob + 1) * BLK].rearrange("c p t -> p c t"), in_=h2T[:]), r=[k_h2T], w=[k_h2d[ob]], dma=sem_h2)
            for t in range(BLK // 128):
                ti = ob * 2 + t
                pb = PS.get()
                for k in range(KC):
                    op("pe", lambda e: e.matmul(PS.t[pb][:, 0:36], lhsT=h2T[:, k, t * 128:(t + 1) * 128], rhs=wr_s[:, k, :], start=(k == 0), stop=(k == KC - 1)),
                       r=[k_h2T, k_wr], w=[PS.k[pb]], inc=(k == KC - 1))
                rt, rtk, _ = RT.next()
                op("dve", lambda e: e.tensor_tensor(out=rt[:, 0:36], in0=PS.t[pb][:, 0:36], in1=br_s[:], op=ALU.add), r=[PS.k[pb], k_br], w=[rtk])
                PS.rel(pb)
                op("dve", lambda e: e.tensor_reduce(out=rt[:, 40:41], in_=rt[:, 0:4], axis=AX.X, op=ALU.max), r=[rtk], w=[rtk])
                op("dve", lambda e: e.tensor_scalar(out=rt[:, 36:40], in0=rt[:, 0:4], scalar1=rt[:, 40:41], scalar2=None, op0=ALU.is_ge), r=[rtk], w=[rtk])
                op("dve", lambda e: e.tensor_scalar(out=rt[:, 41:42], in0=rt[:, 40:41], scalar1=-1.0, scalar2=None, op0=ALU.mult), r=[rtk], w=[rtk])
                op("act", lambda e: e.activation(out=rt[:, 44:48], in_=rt[:, 0:4], func=AF.Exp, bias=rt[:, 41:42], accum_out=rt[:, 42:43]), r=[rtk], w=[rtk])
                op("dve", lambda e: e.reciprocal(out=rt[:, 43:44], in_=rt[:, 42:43]), r=[rtk], w=[rtk])
                op("dve", lambda e: e.tensor_scalar(out=rt[:, 48:56], in0=rt[:, 4:12], scalar1=rt[:, 36:37], scalar2=None, op0=ALU.mult), r=[rtk], w=[rtk])
                for gi in range(1, 4):
                    op("dve", lambda e: e.scalar_tensor_tensor(out=rt[:, 48:56], in0=rt[:, 4 + gi * 8:12 + gi * 8], scalar=rt[:, 36 + gi:37 + gi], in1=rt[:, 48:56], op0=ALU.mult, op1=ALU.add), r=[rtk], w=[rtk])
                op("dve", lambda e: e.max(out=rt[:, 56:64], in_=rt[:, 48:56]), r=[rtk], w=[rtk])
                rt2, rt2k, _ = RT.next()
                op("dve", lambda e: e.tensor_scalar(out=rt2[:, 0:8], in0=rt[:, 48:56], scalar1=rt[:, 57:58], scalar2=None, op0=ALU.is_ge), r=[rtk], w=[rt2k])
                op("dve", lambda e: e.tensor_scalar(out=rt2[:, 8:9], in0=rt[:, 56:57], scalar1=-1.0, scalar2=None, op0=ALU.mult), r=[rtk], w=[rt2k])
                op("act", lambda e: e.activation(out=rt2[:, 16:24], in_=rt[:, 48:56], func=AF.Exp, bias=rt2[:, 8:9]), r=[rtk, rt2k], w=[rt2k])
                op("dve", lambda e: e.tensor_tensor(out=rt2[:, 16:24], in0=rt2[:, 16:24], in1=rt2[:, 0:8], op=ALU.mult), r=[rt2k], w=[rt2k])
                op("dve", lambda e: e.tensor_reduce(out=rt2[:, 9:10], in_=rt2[:, 16:24], axis=AX.X, op=ALU.add), r=[rt2k], w=[rt2k])
                op("dve", lambda e: e.reciprocal(out=rt2[:, 10:11], in_=rt2[:, 9:10]), r=[rt2k], w=[rt2k])
                op("dve", lambda e: e.tensor_tensor(out=rt2[:, 10:11], in0=rt2[:, 10:11], in1=rt[:, 43:44], op=ALU.mult), r=[rt2k, rtk], w=[rt2k])
                op("dve", lambda e: e.tensor_scalar(out=rt2[:, 16:24], in0=rt2[:, 16:24], scalar1=rt2[:, 10:11], scalar2=None, op0=ALU.mult), r=[rt2k], w=[rt2k])
                for gi in range(4):
                    op("dve", lambda e: e.tensor_scalar(out=CW[:, ti, gi * 8:(gi + 1) * 8], in0=rt2[:, 16:24], scalar1=rt[:, 36 + gi:37 + gi], scalar2=None, op0=ALU.mult), r=[rt2k, rtk], w=[k_cw[ti]])

    if dbg:
        dbg_dump("cw", CW[:, :, :], k_cw)
        dbg_dump("x1", x1_d, k_x1d)

    chk(9)
    S.barrier()
    stack_B.close()
    _STK[0] = stack_main
    fgb_s, k_fgb = load_small("fgb_s", fgb, [128, D])
    passes = []
    PT_MAX = min(4, NTO)
    passes = [(t0, min(t0 + PT_MAX, NTO)) for t0 in range(0, NTO, PT_MAX)]
    h2p = sb("h2p", [128, KC, PT_MAX * 128], BF16); k_h2p = Trk(); sem_h2p = S.newsem("d_h2p")
    acc = sb("acc", [128, PT_MAX, D]); k_acc = [Trk() for _ in range(PT_MAX)]
    HID = Ring(S, nc, "hid", [128, 4, PT_MAX * 128], BF16, 2)
    SG = Ring(S, nc, "sg", [128, 512], F32, 3)
    W2R = Ring(S, nc, "w2r", [128, 4, D], BF16, 2, dma=True)
    WRC = Ring(S, nc, "wrc", [128, KC, 512], BF16, 3, dma=True)
    X1L = Ring(S, nc, "x1l", [128, D], F32, 1, dma=True)
    OUTR = Ring(S, nc, "outr", [128, D], F32, 1, dma=True)
    for (t0, t1) in passes:
        ntl = t1 - t0
        ntok = ntl * 128
        op("sp", lambda e: e.dma_start(out=h2p[:, :, 0:ntok], in_=h2_d[:, :, t0 * 128:t1 * 128].rearrange("c p t -> p c t")), r=k_h2d, w=[k_h2p], dma=sem_h2p)
        for tl in range(ntl):
            op("pool", lambda e: e.memset(acc[:, tl, :], 0.0), w=[k_acc[tl]])
        cbs = [(c0, min(c0 + 512, ntok)) for c0 in range(0, ntok, 512)]
        for ex in range(NE):
            w1t, w1k, w1d = WRC.next()
            op("pool", lambda e: e.dma_start(out=w1t[:], in_=w1[ex].rearrange("(c p) f -> p c f", p=128)), w=[w1k], dma=w1d)
            w3t, w3k, w3d = WRC.next()
            op("pool", lambda e: e.dma_start(out=w3t[:], in_=w3[ex].rearrange("(c p) f -> p c f", p=128)), w=[w3k], dma=w3d)
            w2t, w2k, w2d = W2R.next()
            op("pool", lambda e: e.dma_start(out=w2t[:], in_=w2[ex].rearrange("(c p) f -> p c f", p=128)), w=[w2k], dma=w2d)
            hid, hidk, _ = HID.next()
            for fc in range(4):
                for (c0, c1) in cbs:
                    n = c1 - c0
                    pa = PS.get(); pb = PS.get()
                    for k in range(KC):
                        op("pe", lambda e: e.matmul(PS.t[pa][:, 0:n], lhsT=w1t[:, k, fc * 128:(fc + 1) * 128], rhs=h2p[:, k, c0:c1], start=(k == 0), stop=(k == KC - 1)),
                           r=[w1k, k_h2p], w=[PS.k[pa]], inc=(k == KC - 1))
                    for k in range(KC):
                        op("pe", lambda e: e.matmul(PS.t[pb][:, 0:n], lhsT=w3t[:, k, fc * 128:(fc + 1) * 128], rhs=h2p[:, k, c0:c1], start=(k == 0), stop=(k == KC - 1)),
                           r=[w3k, k_h2p], w=[PS.k[pb]], inc=(k == KC - 1))
                    sg, sgk, _ = SG.next()
                    op("act", lambda e: e.activation(out=sg[:, 0:n], in_=PS.t[pa][:, 0:n], func=AF.Silu), r=[PS.k[pa]], w=[sgk])
                    PS.rel(pa)
                    op("dve", lambda e: e.tensor_tensor(out=hid[:, fc, c0:c1], in0=sg[:, 0:n], in1=PS.t[pb][:, 0:n], op=ALU.mult), r=[sgk, PS.k[pb]], w=[hidk])
                    PS.rel(pb)
            for tl in range(ntl):
                ti = t0 + tl
                for dblk in range(4):
                    pb = PS.get()
                    for fc in range(4):
                        op("pe", lambda e: e.matmul(PS.t[pb][:], lhsT=hid[:, fc, tl * 128:(tl + 1) * 128], rhs=w2t[:, fc, dblk * 512:(dblk + 1) * 512], start=(fc == 0), stop=(fc == 3)),
                           r=[hidk, w2k], w=[PS.k[pb]], inc=(fc == 3))
                    op("dve", lambda e: e.scalar_tensor_tensor(out=acc[:, tl, dblk * 512:(dblk + 1) * 512], in0=PS.t[pb][:], scalar=CW[:, ti, ex:ex + 1], in1=acc[:, tl, dblk * 512:(dblk + 1) * 512], op0=ALU.mult, op1=ALU.add),
                       r=[PS.k[pb], k_cw[ti], k_acc[tl]], w=[k_acc[tl]])
                    PS.rel(pb)
        for tl in range(ntl):
            ti = t0 + tl
            x1t, x1k, x1dd = X1L.next()
            op("sp", lambda e: e.dma_start(out=x1t[:], in_=x1_d[ti * 128:(ti + 1) * 128, :]), r=[k_x1d[ti]], w=[x1k], dma=x1dd)
            op("pool", lambda e: e.tensor_tensor(out=acc[:, tl, :], in0=acc[:, tl, :], in1=G2B[:], op=ALU.mult), r=[k_acc[tl], k_G], w=[k_acc[tl]])
            op("dve", lambda e: e.tensor_tensor(out=x1t[:], in0=x1t[:], in1=acc[:, tl, :], op=ALU.add), r=[x1k, k_acc[tl]], w=[x1k])
            st, sk, _ = stat.next()
            ot_, otk_, otd = OUTR.next()
            op("act", lambda e: e.activation(out=ot_[:], in_=x1t[:], func=AF.Square, accum_out=st[:, 0:1]), r=[x1k], w=[otk_, sk])
            rstd_from_ssq(st, sk, 0, 1, D)
            op("dve", lambda e: e.scalar_tensor_tensor(out=ot_[:], in0=x1t[:], scalar=st[:, 1:2], in1=fgb_s[:], op0=ALU.mult, op1=ALU.mult), r=[x1k, sk, k_fgb], w=[otk_])
            op("sp", lambda e: e.dma_start(out=out[ti * 128:(ti + 1) * 128, :], in_=ot_[:]), r=[otk_], dma=otd)
    finalize()
    return nc


def _fm(v):
    v = np.asarray(v, np.float32)
    return np.ascontiguousarray(v.reshape(-1, 128).T)


def _rep(v):
    v = np.asarray(v, np.float32).reshape(1, -1)
    return np.ascontiguousarray(np.broadcast_to(v, (128, v.shape[1])))


def _level_masks():
    i = np.arange(128)[:, None]
    j = np.arange(128)[None, :]
    ms = []
    for l in range(7):
        s_ = 1 << l
        lmx = ((i // (2 * s_) == j // (2 * s_)) & (i % (2 * s_) >= s_) & (j % (2 * s_) < s_)).astype(np.float32)
        if l == 0:
            ms.append(lmx)
        ms.append(np.ascontiguousarray(lmx.T))
    return np.ascontiguousarray(np.concatenate(ms, axis=1))


def make_in_maps(inputs, NP, NO):
    x = np.asarray(inputs["x"], np.float32)
    B = x.shape[0]
    f = lambda k: np.asarray(inputs[k], np.float32)
    shared = {
        "w_ada": np.ascontiguousarray(f("w_ada")[0]),
        "b_adaT": _fm(f("b_ada")[0]),
        "n1g": _fm(f("norm1_g")[0]),
        "w_in": np.ascontiguousarray(f("w_in")[0]),
        "convw": np.ascontiguousarray(f("conv_w")[0].T.reshape(24, 128, 4).transpose(1, 0, 2).reshape(128, 96)),
        "alog": _rep(f("a_log")[0]), "dtb": _rep(f("dt_bias")[0]), "ong": _fm(f("dn_onorm_g")[0]),
        "ffb": _rep(f("fox_f_bias")[0]),
        "w_out": np.ascontiguousarray(f("w_out")[0]),
        "n2g": _fm(f("norm2_g")[0]),
        "w_r": np.ascontiguousarray(np.concatenate([f("w_router_group")[0], f("w_router_expert")[0]], axis=1)),
        "b_r": _rep(np.concatenate([f("b_router_group")[0], f("b_router_expert")[0]])),
        "w1": np.ascontiguousarray(f("w1")[0].reshape(NE, D, 512)),
        "w3": np.ascontiguousarray(f("w3")[0].reshape(NE, D, 512)),
        "w2": np.ascontiguousarray(f("w2")[0].reshape(NE, 512, D)),
        "fgb": _rep(f("final_g")),
        "lvlm": _level_masks(),
    }
    maps = []
    for b in range(B):
        for s in range(2):
            m = dict(shared)
            m["xp"] = np.ascontiguousarray(x[b, 0:NP])
            m["xo"] = np.ascontiguousarray(x[b, s * NP:s * NP + NO])
            m["cT"] = _fm(f("c")[b])
            pm = np.zeros((128, 2), np.float32)
            pm[:, 0] = float(s)
            pm[:, 1] = 0.0 if s == 1 else -30000.0
            m["pmv"] = pm
            maps.append(m)
    return maps


_NC_CACHE = {}


def kernel(**inputs):
    NP = NO = 2048
    if "nc" not in _NC_CACHE:
        _NC_CACHE["nc"] = build(NP, NO)
    nc = _NC_CACHE["nc"]
    maps = make_in_maps(inputs, NP, NO)
    res = run_bass_kernel_spmd(nc, maps, core_ids=list(range(8)))
    B = 4
    outp = np.zeros((B, 2 * NO, D), np.float32)
    for b in range(B):
        for s in range(2):
            outp[b, s * NO:(s + 1) * NO] = res.results[b * 2 + s]["out"]
    return outp
```

```python
from contextlib import ExitStack
import numpy as np
import concourse.bass as bass
import concourse.mybir as mybir
from concourse.bass_utils import run_bass_kernel_spmd

F32 = mybir.dt.float32
BF16 = mybir.dt.bfloat16
I32 = mybir.dt.int32
AF = mybir.ActivationFunctionType
ALU = mybir.AluOpType
AX = mybir.AxisListType

D = 2048
KC = 16
H = 8
BLK = 256
NE = 32
EPS = 1e-6
IN_W = 7192
C_QA, C_KA, C_VA, C_ZA, C_AA, C_BA, C_QB, C_KB, C_VB, C_FB = 0, 1024, 2048, 3072, 4096, 4104, 4112, 5136, 6160, 7184


class Trk:
    __slots__ = ("w", "r", "ps")

    def __init__(self, ps=False):
        self.w = None
        self.r = {}
        self.ps = ps


class Sched:
    def __init__(self, nc):
        self.nc = nc
        self.eng = {"pe": nc.tensor, "act": nc.scalar, "dve": nc.vector, "pool": nc.gpsimd, "sp": nc.sync}
        self.sems, self.cnt, self.isdma = {}, {}, {}
        self.seen = {e: {} for e in self.eng}
        for e in self.eng:
            self.newsem(e, False)
        self.nops = 0

    def newsem(self, key, isdma=True):
        self.sems[key] = self.nc.alloc_semaphore(name=f"s_{key}")
        self.cnt[key] = 0
        self.isdma[key] = isdma
        return key

    def _wait(self, e, key, val):
        if self.isdma[key]:
            val = self.cnt[key]
        if self.seen[e].get(key, 0) >= val:
            return
        self.eng[e].wait_ge(self.sems[key], val)
        self.seen[e][key] = val

    def op(self, e, fn, reads=(), writes=(), inc=True, dma=None):
        deps = {}

        def add(k, v):
            if deps.get(k, 0) < v:
                deps[k] = v
        for t in reads:
            if t.w is not None:
                k, v = t.w
                if not (k == e and e == "pe"):
                    add(k, v)
            if t.ps:
                for k, v in t.r.items():
                    if k != e:
                        add(k, v)
        for t in writes:
            if t.w is not None and not (t.w[0] == e and dma is None):
                add(*t.w)
            for k, v in t.r.items():
                if k == e and dma is None:
                    continue
                add(k, v)
        for k, v in deps.items():
            self._wait(e, k, v)
        ins = fn(self.eng[e])
        self.nops += 1
        if dma is not None:
            self.cnt[dma] += 16
            ins.then_inc(self.sems[dma], 16)
            tk = (dma, self.cnt[dma])
        elif inc:
            self.cnt[e] += 1
            ins.then_inc(self.sems[e], 1)
            tk = (e, self.cnt[e])
        else:
            tk = (e, self.cnt[e] + 1)
        for t in reads:
            if t.r.get(tk[0], 0) < tk[1]:
                t.r[tk[0]] = tk[1]
        for t in writes:
            t.w = tk
            t.r = {}
        return tk

    def barrier(self):
        for e in self.eng:
            for k in self.sems:
                if k != e and self.cnt[k] > 0:
                    v = self.cnt[k]
                    if self.seen[e].get(k, 0) < v:
                        self.eng[e].wait_ge(self.sems[k], v)
                        self.seen[e][k] = v


_STK = [None]


def _alloc_sb(nc, name, shape, dtype):
    return _STK[0].enter_context(nc.sbuf_tensor(name, list(shape), dtype))


class Ring:
    def __init__(self, S, nc, name, shape, dtype, n, dma=False):
        self.t = [_alloc_sb(nc, f"{name}{i}", shape, dtype) for i in range(n)]
        self.k = [Trk() for _ in range(n)]
        self.d = [S.newsem(f"d_{name}{i}") for i in range(n)] if dma else [None] * n
        self.i = 0
        self.n = n

    def next(self):
        j = self.i % self.n
        self.i += 1
        return self.t[j], self.k[j], self.d[j]


class PsPool:
    def __init__(self, nc, n):
        self.t = [nc.alloc_psum_tensor(f"ps{i}", [128, 512], F32) for i in range(n)]
        self.k = [Trk(ps=True) for _ in range(n)]
        self.busy = [False] * n
        self.i = 0
        self.n = n

    def get(self):
        for _ in range(self.n):
            j = self.i % self.n
            self.i += 1
            if not self.busy[j]:
                self.busy[j] = True
                return j
        raise RuntimeError("out of PSUM banks")

    def rel(self, j):
        self.busy[j] = False


class StopBuild(Exception):
    pass


_LAST = {}


def build(NP, NO, dbg=None, stop=None):
    def chk(n):
        if stop == n:
            finalize()
            _LAST["nc"] = nc
            raise StopBuild()
    nc = bass.Bass("TRN2", target_bir_lowering=False)
    S = Sched(nc)

    def finalize():
        for k in S.sems:
            if S.isdma[k] and S.cnt[k] > 0:
                S.eng["sp"].wait_ge(S.sems[k], S.cnt[k])
        S.barrier()
    T = NP + NO
    NBP, NBO = NP // BLK, NO // BLK
    NB = NBP + NBO
    NKT = T // 128
    NTO = NO // 128

    def din(name, shape, dt=F32):
        return nc.dram_tensor(name, list(shape), dt, kind="ExternalInput").ap()

    xp = din("xp", [NP, D]); xo = din("xo", [NO, D]); cT = din("cT", [128, KC])
    pmv = din("pmv", [128, 2])
    w_ada = din("w_ada", [D, 6 * D]); b_adaT = din("b_adaT", [128, 96]); n1g = din("n1g", [128, KC])
    w_in = din("w_in", [D, IN_W]); convw = din("convw", [128, 24 * 4])
    alog = din("alog", [128, H]); dtb = din("dtb", [128, H]); ong = din("ong", [128, 1]); ffb = din("ffb", [128, H])
    w_out = din("w_out", [D, D]); n2g = din("n2g", [128, KC])
    w_r = din("w_r", [D, 36]); b_r = din("b_r", [128, 36])
    w1 = din("w1", [NE, D, 512]); w3 = din("w3", [NE, D, 512]); w2 = din("w2", [NE, 512, D])
    fgb = din("fgb", [128, D])
    lvlm = din("lvlm", [128, 8 * 128])
    out = nc.dram_tensor("out", [NO, D], F32, kind="ExternalOutput").ap()
    dbg_t = None
    if dbg:
        dbg_t = {k: nc.dram_tensor("dbg_" + k, list(shp), F32, kind="ExternalOutput").ap() for k, shp in dbg.items()}

    win_d = nc.dram_tensor("win_d", [D, IN_W], BF16, kind="Internal").ap()
    kt_d = nc.dram_tensor("kt_d", [H, 128, T], BF16, kind="Internal").ap()
    v_d = nc.dram_tensor("v_d", [T, H * 128], BF16, kind="Internal").ap()
    x1_d = nc.dram_tensor("x1_d", [NO, D], F32, kind="Internal").ap()
    h2_d = nc.dram_tensor("h2_d", [KC, 128, NO], BF16, kind="Internal").ap()
    k_win = Trk(); k_ktd = [Trk() for _ in range(NB)]; k_vd = [Trk() for _ in range(NB)]
    k_x1d = [Trk() for _ in range(NTO)]; k_h2d = [Trk() for _ in range(NBO)]

    stack_main = ExitStack()
    stack_B = ExitStack()
    _STK[0] = stack_main

    def sb(name, shape, dt=F32):
        return _alloc_sb(nc, name, shape, dt)

    def op(e, fn, r=(), w=(), inc=True, dma=None):
        return S.op(e, fn, r, w, inc, dma)

    PS = PsPool(nc, 8)

    ident_f = sb("ident_f", [128, 128]); ident_b = sb("ident_b", [128, 128], BF16)
    ones_f = sb("ones_f", [128, 128]); ones_b = sb("ones_b", [128, 128], BF16)
    utri = sb("utri", [128, 128])
    m_posL = sb("m_posL", [128, 128])
    m_negUs = sb("m_negUs", [128, 128])
    m_negUi = sb("m_negUi", [128, 128])
    sel = sb("sel", [24, H * 128], BF16)
    iot = sb("iot", [24, 128], I32); iotf = sb("iotf", [24, 128])
    kc = Trk()
    op("pool", lambda e: e.memset(ident_f[:], 0.0), w=[kc])
    op("pool", lambda e: e.affine_select(out=ident_f[:], in_=ident_f[:], pattern=[[-1, 128]], compare_op=ALU.not_equal, fill=1.0, base=0, channel_multiplier=1), r=[kc], w=[kc])
    op("pool", lambda e: e.tensor_copy(out=ident_b[:], in_=ident_f[:]), r=[kc], w=[kc])
    op("pool", lambda e: e.memset(ones_f[:], 1.0), w=[kc])
    op("pool", lambda e: e.memset(ones_b[:], 1.0), w=[kc])
    op("pool", lambda e: e.affine_select(out=utri[:], in_=ones_f[:], pattern=[[1, 128]], compare_op=ALU.is_ge, fill=0.0, base=0, channel_multiplier=-1), r=[kc], w=[kc])
    op("pool", lambda e: e.memset(m_posL[:], -1.0), w=[kc])
    op("pool", lambda e: e.affine_select(out=m_posL[:], in_=m_posL[:], pattern=[[-1, 128]], compare_op=ALU.is_ge, fill=0.0, base=-1, channel_multiplier=1), r=[kc], w=[kc])
    op("pool", lambda e: e.memset(m_negUs[:], -1.0), w=[kc])
    op("pool", lambda e: e.affine_select(out=m_negUs[:], in_=m_negUs[:], pattern=[[1, 128]], compare_op=ALU.is_ge, fill=0.0, base=-1, channel_multiplier=-1), r=[kc], w=[kc])
    op("pool", lambda e: e.memset(m_negUi[:], 1.0), w=[kc])
    op("pool", lambda e: e.affine_select(out=m_negUi[:], in_=m_negUi[:], pattern=[[1, 128]], compare_op=ALU.is_ge, fill=0.0, base=0, channel_multiplier=-1), r=[kc], w=[kc])
    op("pool", lambda e: e.iota(iot[:], pattern=[[0, 128]], base=0, channel_multiplier=1), w=[kc])
    op("pool", lambda e: e.tensor_copy(out=iotf[:], in_=iot[:]), r=[kc], w=[kc])
    for h in range(H):
        op("dve", lambda e: e.tensor_scalar(out=iot[:].bitcast(F32), in0=iotf[:], scalar1=float(-h), scalar2=None, op0=ALU.add), r=[kc], w=[kc])
        tmpf = iot[:].bitcast(F32)
        op("dve", lambda e: e.scalar_tensor_tensor(out=iotf[:], in0=tmpf, scalar=-8.0, in1=tmpf, op0=ALU.add, op1=ALU.mult), r=[kc], w=[kc])
        op("dve", lambda e: e.scalar_tensor_tensor(out=iotf[:], in0=tmpf, scalar=-16.0, in1=iotf[:], op0=ALU.add, op1=ALU.mult), r=[kc], w=[kc])
        op("dve", lambda e: e.tensor_scalar(out=sel[:, h * 128:(h + 1) * 128], in0=iotf[:], scalar1=0.0, scalar2=None, op0=ALU.is_equal), r=[kc], w=[kc])
        op("dve", lambda e: e.tensor_scalar(out=iotf[:], in0=tmpf, scalar1=float(h), scalar2=None, op0=ALU.add), r=[kc], w=[kc])

    def load_small(name, src, shape, dt=F32):
        t = sb(name, shape, dt)
        k = Trk()
        sem = S.newsem("d_" + name)
        op("sp", lambda e: e.dma_start(out=t[:], in_=src), w=[k], dma=sem)
        return t, k

    cT_s, k_cT = load_small("cT_s", cT, [128, KC])
    pm_s, k_pm = load_small("pm_s", pmv, [128, 2])
    bada_s, k_bada = load_small("bada_s", b_adaT, [128, 96])
    n1g_s, k_n1g = load_small("n1g_s", n1g, [128, KC])
    n2g_s, k_n2g = load_small("n2g_s", n2g, [128, KC])
    convw_s, k_convw = load_small("convw_s", convw, [128, 96])
    alog_s, k_alog = load_small("alog_s", alog, [128, H])
    dtb_s, k_dtb = load_small("dtb_s", dtb, [128, H])
    ong_s, k_ong = load_small("ong_s", ong, [128, 1])
    ffb_s, k_ffb = load_small("ffb_s", ffb, [128, H])
    br_s, k_br = load_small("br_s", b_r, [128, 36])

    LM = sb("LM", [128, 8, 128], BF16); k_lm = Trk(); sem_lm = S.newsem("d_lm")
    op("pool", lambda e: e.dma_start(out=LM[:].rearrange("p a b -> p (a b)"), in_=lvlm), w=[k_lm], dma=sem_lm)
    sem_win = S.newsem("d_win")
    for i in range(4):
        op("pool", lambda e: e.dma_start(out=win_d[i * 512:(i + 1) * 512, :], in_=w_in[i * 512:(i + 1) * 512, :]), w=[k_win], dma=sem_win)
    wr_s = sb("wr_s", [128, KC, 36], BF16); k_wr = Trk(); sem_wr = S.newsem("d_wr")
    op("pool", lambda e: e.dma_start(out=wr_s[:], in_=w_r.rearrange("(c p) f -> p c f", p=128)), w=[k_wr], dma=sem_wr)
    wsm = sb("wsm", [128, KC, 24], BF16); k_wsm = Trk(); sem_wsm = S.newsem("d_wsm")
    op("pool", lambda e: e.dma_start(out=wsm[:, :, 0:16], in_=w_in[:, C_AA:C_AA + 16].rearrange("(c p) f -> p c f", p=128)), w=[k_wsm], dma=sem_wsm)
    op("pool", lambda e: e.dma_start(out=wsm[:, :, 16:24], in_=w_in[:, C_FB:C_FB + 8].rearrange("(c p) f -> p c f", p=128)), w=[k_wsm], dma=sem_wsm)

    chk(0)
    sc_b = sb("sc_b", [128, KC], BF16); k_sc = Trk()
    tA = sb("tA", [128, KC]); k_tA = Trk()
    mod = sb("mod", [128, 96]); k_mod = Trk()
    vec = sb("vec", [128, 6 * KC]); k_vec = Trk()
    G1B = sb("G1B", [128, D]); G2B = sb("G2B", [128, D]); k_G = Trk()
    dg = Ring(S, nc, "dg", [128, 128], F32, 2)
    nalog = sb("nalog", [128, H]); k_nalog = Trk()
    stat = Ring(S, nc, "stat", [128, 4], F32, 4)
    CW = sb("CW", [128, NTO, 32]); k_cw = [Trk() for _ in range(NTO)]
    _STK[0] = stack_B
    UW = 256
    WR = Ring(S, nc, "wring", [128, KC, UW], BF16, 3, dma=True)
    op("act", lambda e: e.activation(out=tA[:], in_=cT_s[:], func=AF.Exp, scale=-1.0), r=[k_cT], w=[k_tA])
    op("dve", lambda e: e.tensor_scalar(out=tA[:], in0=tA[:], scalar1=1.0, scalar2=None, op0=ALU.add), r=[k_tA], w=[k_tA])
    op("dve", lambda e: e.reciprocal(out=tA[:], in_=tA[:]), r=[k_tA], w=[k_tA])
    op("dve", lambda e: e.tensor_tensor(out=sc_b[:], in0=tA[:], in1=cT_s[:], op=ALU.mult), r=[k_tA, k_cT], w=[k_sc])
    pmod = PS.get()
    for u in range(48):
        wt, wk, wd = WR.next()
        op("pool", lambda e: e.dma_start(out=wt[:], in_=w_ada[:, u * 256:(u + 1) * 256].rearrange("(c p) f -> p c f", p=128)), w=[wk], dma=wd)
        for j in range(2):
            oc = u * 2 + j
            for k in range(KC):
                op("pe", lambda e: e.matmul(PS.t[pmod][:, oc:oc + 1], lhsT=wt[:, k, j * 128:(j + 1) * 128], rhs=sc_b[:, k:k + 1], start=(k == 0), stop=(k == KC - 1)),
                   r=[wk, k_sc], w=[PS.k[pmod]], inc=(k == KC - 1))
    op("dve", lambda e: e.tensor_tensor(out=mod[:], in0=PS.t[pmod][:, 0:96], in1=bada_s[:], op=ALU.add), r=[PS.k[pmod], k_bada], w=[k_mod])
    PS.rel(pmod)
    A1, B1, A1p, B1p, A2, B2 = [vec[:, i * KC:(i + 1) * KC] for i in range(6)]
    op("dve", lambda e: e.scalar_tensor_tensor(out=A1, in0=mod[:, 16:32], scalar=1.0, in1=n1g_s[:], op0=ALU.add, op1=ALU.mult), r=[k_mod, k_n1g], w=[k_vec])
    op("dve", lambda e: e.tensor_copy(out=B1, in_=mod[:, 0:16]), r=[k_mod], w=[k_vec])
    op("dve", lambda e: e.tensor_scalar(out=A1p, in0=A1, scalar1=pm_s[:, 0:1], scalar2=None, op0=ALU.mult), r=[k_vec, k_pm], w=[k_vec])
    op("dve", lambda e: e.tensor_scalar(out=B1p, in0=B1, scalar1=pm_s[:, 0:1], scalar2=None, op0=ALU.mult), r=[k_vec, k_pm], w=[k_vec])
    op("dve", lambda e: e.scalar_tensor_tensor(out=A2, in0=mod[:, 64:80], scalar=1.0, in1=n2g_s[:], op0=ALU.add, op1=ALU.mult), r=[k_mod, k_n2g], w=[k_vec])
    op("dve", lambda e: e.tensor_copy(out=B2, in_=mod[:, 48:64]), r=[k_mod], w=[k_vec])
    for gi, (GB, off) in enumerate(((G1B, 32), (G2B, 80))):
        for q4 in range(4):
            pb = PS.get()
            for j in range(4):
                c = q4 * 4 + j
                dt_, dk_, _ = dg.next()
                op("dve", lambda e: e.tensor_scalar(out=dt_[:], in0=ident_f[:], scalar1=mod[:, off + c:off + c + 1], scalar2=None, op0=ALU.mult), r=[kc, k_mod], w=[dk_])
                op("pe", lambda e: e.matmul(PS.t[pb][:, j * 128:(j + 1) * 128], lhsT=ones_f[:], rhs=dt_[:], start=True, stop=True), r=[kc, dk_], w=[PS.k[pb]])
            op("act", lambda e: e.copy(out=GB[:, q4 * 512:(q4 + 1) * 512], in_=PS.t[pb][:]), r=[PS.k[pb]], w=[k_G])
            PS.rel(pb)
    op("act", lambda e: e.activation(out=nalog[:], in_=alog_s[:], func=AF.Exp), r=[k_alog], w=[k_nalog])
    op("dve", lambda e: e.tensor_scalar(out=nalog[:], in0=nalog[:], scalar1=-1.0, scalar2=None, op0=ALU.mult), r=[k_nalog], w=[k_nalog])

    chk(1)
    XT = Ring(S, nc, "xtile", [128, D], F32, 2, dma=True)
    XN = Ring(S, nc, "xn", [128, D], BF16, 1)
    hT = sb("hT", [128, KC, BLK], BF16); k_hT = Trk()
    mixT = sb("mixT", [128, KC, BLK], BF16); k_mix = [Trk() for _ in range(KC)]
    h2T = sb("h2T", [128, KC, BLK], BF16); k_h2T = Trk(); sem_h2 = S.newsem("d_h2T")
    UB = Ring(S, nc, "ub", [128, BLK + 3], F32, 2)
    CA = Ring(S, nc, "ca", [128, BLK], F32, 2)
    hist = sb("hist", [128, 24, 3]); k_hist = [Trk() for _ in range(24)]
    op("pool", lambda e: e.memset(hist[:], 0.0), w=k_hist)
    QKV = sb("QKV", [128, 24, BLK], BF16); k_qkv = [Trk() for _ in range(24)]
    ZS = sb("ZS", [128, 8, BLK], BF16); k_zs = [Trk() for _ in range(8)]
    QbT = sb("QbT", [128, 8, BLK], BF16); k_qb = [Trk() for _ in range(8)]
    KbT = sb("KbT", [128, 8, BLK], BF16); k_kb = Trk(); sem_kb = S.newsem("d_kb")
    VbT = Ring(S, nc, "vbt", [128, 1024], BF16, 2, dma=True)
    FK = sb("FK", [128, NKT, H]); k_fk = [Trk() for _ in range(NKT)]
    Fcar = sb("Fcar", [128, H]); k_fcar = Trk()
    op("pool", lambda e: e.memset(Fcar[:], 0.0), w=[k_fcar])
    FqT = sb("FqT", [24, BLK], BF16); k_fq = Trk()
    Sst = sb("Sst", [128, H, 128]); Sbf = sb("Sbf", [128, H, 128], BF16); k_S = [Trk() for _ in range(H)]; k_Sb = [Trk() for _ in range(H)]
    op("pool", lambda e: e.memset(Sst[:], 0.0), w=k_S)
    op("pool", lambda e: e.memset(Sbf[:], 0.0), w=k_Sb)
    GT = Ring(S, nc, "gt", [128, 64], F32, 2)
    GC = Ring(S, nc, "gc", [128, 8 * H], F32, 4)
    T32 = Ring(S, nc, "t32", [128, 128], F32, 6)
    NSLOT = 8
    BFN = ["X", "Y", "qkT", "qdT", "TmA", "TmB", "TtA", "TtB", "Ul", "W1s", "ke", "kd", "vtk", "wT", "vn"]
    SL = []
    for hs in range(NSLOT):
        d_ = {nm: (sb(f"sl{hs}_{nm}", [128, 128], BF16), Trk()) for nm in BFN}
        d_["u2"] = (sb(f"sl{hs}_u2", [128, 128], F32), Trk())
        SL.append(d_)
    f3t = sb("f3t", [128, 24], BF16); k_f3 = Trk()
    r3t = sb("r3t", [128, 16], F32); k_r3 = Trk()
    OTB = sb("OTB", [128, H, BLK], F32); k_ot = [Trk() for _ in range(H)]
    W32 = Ring(S, nc, "w32", [128, BLK], F32, 4)
    W16 = Ring(S, nc, "w16", [128, BLK], BF16, 4)
    KR = Ring(S, nc, "kr", [128, 1024], BF16, 2, dma=True)
    VR = Ring(S, nc, "vr", [128, 8, 128], BF16, 2, dma=True)
    XP = Ring(S, nc, "xpc", [128, 256], F32, 2, dma=True)
    X1P = Ring(S, nc, "x1p", [128, 256], F32, 2, dma=True)
    RT = Ring(S, nc, "rt", [128, 64], F32, 2)

    def dbg_dump(name, src_ap, trk):
        if dbg_t is None or name not in dbg_t:
            return
        sem = S.newsem("d_dbg_" + name + str(S.nops))
        op("pool" if src_ap.dtype != F32 else "sp", lambda e: e.dma_start(out=dbg_t[name], in_=src_ap), r=trk, dma=sem)
        S.eng["sp"].wait_ge(S.sems[sem], S.cnt[sem])

    def rstd_from_ssq(st, sk, col_in, col_out, n):
        op("act", lambda e: e.activation(out=st[:, col_out:col_out + 1], in_=st[:, col_in:col_in + 1], func=AF.Ln, scale=1.0 / n, bias=EPS), r=[sk], w=[sk])
        op("act", lambda e: e.activation(out=st[:, col_out:col_out + 1], in_=st[:, col_out:col_out + 1], func=AF.Exp, scale=-0.5), r=[sk], w=[sk])

    def norm_to_T(src_rows, dst, dst_k, Av, Bv, x_from=None):
        for t in range(BLK // 128):
            xt, xk, xd = XT.next()
            op("sp", lambda e: e.dma_start(out=xt[:], in_=src_rows[t * 128:(t + 1) * 128, :]), r=(x_from or ()), w=[xk], dma=xd)
            st, sk, _ = stat.next()
            xn, nk, _ = XN.next()
            op("act", lambda e: e.activation(out=xn[:], in_=xt[:], func=AF.Square, accum_out=st[:, 0:1]), r=[xk], w=[nk, sk])
            rstd_from_ssq(st, sk, 0, 1, D)
            op("dve", lambda e: e.tensor_scalar(out=xn[:], in0=xt[:], scalar1=st[:, 1:2], scalar2=None, op0=ALU.mult), r=[xk, sk], w=[nk])
            for c4 in range(4):
                pb = PS.get()
                pv = PS.t[pb][:].bitcast(BF16)
                for j in range(4):
                    c = c4 * 4 + j
                    op("pe", lambda e: e.transpose(out=pv[:, j * 128:(j + 1) * 128], in_=xn[:, c * 128:(c + 1) * 128], identity=ident_b[:]), r=[nk, kc], w=[PS.k[pb]])
                for j in range(4):
                    c = c4 * 4 + j
                    eng = "act" if c4 % 2 == 0 else "dve"
                    if eng == "act":
                        op("act", lambda e: e.activation(out=dst[:, c, t * 128:(t + 1) * 128], in_=pv[:, j * 128:(j + 1) * 128], func=AF.Identity, scale=Av[:, c:c + 1], bias=Bv[:, c:c + 1]),
                           r=[PS.k[pb], k_vec], w=[dst_k])
                    else:
                        op("dve", lambda e: e.tensor_scalar(out=dst[:, c, t * 128:(t + 1) * 128], in0=pv[:, j * 128:(j + 1) * 128], scalar1=Av[:, c:c + 1], scalar2=Bv[:, c:c + 1], op0=ALU.mult, op1=ALU.add),
                           r=[PS.k[pb], k_vec], w=[dst_k])
                PS.rel(pb)

    wr_sp_sem = {}

    def load_unit(col0, ncols=256, src=None):
        wt, wk, wd = WR.next()
        if src is None:
            if wd not in wr_sp_sem:
                wr_sp_sem[wd] = S.newsem(wd + "_sp")
            wd = wr_sp_sem[wd]
            op("sp", lambda e: e.dma_start(out=wt[:, :, 0:ncols], in_=win_d[:, col0:col0 + ncols].rearrange("(c p) f -> p c f", p=128)), r=[k_win], w=[wk], dma=wd)
        else:
            op("pool", lambda e: e.dma_start(out=wt[:, :, 0:ncols], in_=src[:, col0:col0 + ncols].rearrange("(c p) f -> p c f", p=128)), w=[wk], dma=wd)
        return wt, wk

    def proj_fm(wt, wk, j):
        pb = PS.get()
        for k in range(KC):
            op("pe", lambda e: e.matmul(PS.t[pb][:, 0:BLK], lhsT=wt[:, k, j * 128:(j + 1) * 128], rhs=hT[:, k, :], start=(k == 0), stop=(k == KC - 1)),
               r=[wk, k_hT], w=[PS.k[pb]], inc=(k == KC - 1))
        return pb

    def silu_from(src_ap, src_k, dst_ap, dst_k):
        op("act", lambda e: e.activation(out=dst_ap, in_=src_ap, func=AF.Silu), r=src_k, w=dst_k)

    for blk in range(NB):
        own = blk >= NBP
        tok0 = blk * BLK
        src = (xo[(blk - NBP) * BLK:(blk - NBP + 1) * BLK, :] if own else xp[blk * BLK:(blk + 1) * BLK, :])
        norm_to_T(src, hT, k_hT, A1 if own else A1p, B1 if own else B1p)

        chk(2)
        gres = []
        for t in range(BLK // 128):
            kt = blk * 2 + t
            pb = PS.get()
            for k in range(KC):
                op("pe", lambda e: e.matmul(PS.t[pb][:, 0:24], lhsT=hT[:, k, t * 128:(t + 1) * 128], rhs=wsm[:, k, :], start=(k == 0), stop=(k == KC - 1)),
                   r=[k_hT, k_wsm], w=[PS.k[pb]], inc=(k == KC - 1))
            g, gk, _ = GT.next()
            R, Rk, _ = GC.next()
            P = PS.t[pb]
            op("dve", lambda e: e.tensor_tensor(out=g[:, 0:8], in0=P[:, 0:8], in1=dtb_s[:], op=ALU.add), r=[PS.k[pb], k_dtb], w=[gk])
            op("act", lambda e: e.activation(out=g[:, 0:8], in_=g[:, 0:8], func=AF.Exp), r=[gk], w=[gk])
            op("act", lambda e: e.activation(out=g[:, 0:8], in_=g[:, 0:8], func=AF.Ln, bias=1.0), r=[gk], w=[gk])
            op("dve", lambda e: e.tensor_tensor(out=R[:, 0:8], in0=g[:, 0:8], in1=nalog[:], op=ALU.mult), r=[gk, k_nalog], w=[Rk])
            op("act", lambda e: e.activation(out=g[:, 8:16], in_=P[:, 8:16], func=AF.Exp, scale=-1.0), r=[PS.k[pb]], w=[gk])
            op("act", lambda e: e.activation(out=g[:, 8:16], in_=g[:, 8:16], func=AF.Ln, bias=1.0), r=[gk], w=[gk])
            op("dve", lambda e: e.tensor_scalar(out=R[:, 8:16], in0=g[:, 8:16], scalar1=-1.0, scalar2=None, op0=ALU.mult), r=[gk], w=[Rk])
            op("act", lambda e: e.activation(out=R[:, 16:24], in_=g[:, 8:16], func=AF.Exp, scale=-1.0), r=[gk], w=[Rk])
            op("dve", lambda e: e.tensor_scalar(out=R[:, 24:32], in0=R[:, 16:24], scalar1=-1.0, scalar2=None, op0=ALU.mult), r=[Rk], w=[Rk])
            op("dve", lambda e: e.tensor_tensor(out=g[:, 16:24], in0=P[:, 16:24], in1=ffb_s[:], op=ALU.add), r=[PS.k[pb], k_ffb], w=[gk])
            op("act", lambda e: e.activation(out=g[:, 16:24], in_=g[:, 16:24], func=AF.Exp, scale=-1.0), r=[gk], w=[gk])
            op("act", lambda e: e.activation(out=g[:, 16:24], in_=g[:, 16:24], func=AF.Ln, bias=1.0), r=[gk], w=[gk])
            op("dve", lambda e: e.tensor_scalar(out=g[:, 16:24], in0=g[:, 16:24], scalar1=-1.0, scalar2=None, op0=ALU.mult), r=[gk], w=[gk])
            PS.rel(pb)
            pc = PS.get()
            Pc = PS.t[pc]
            op("pe", lambda e: e.matmul(Pc[:, 0:8], lhsT=utri[:], rhs=R[:, 0:8], start=True, stop=True), r=[kc, Rk], w=[PS.k[pc]])
            op("pe", lambda e: e.matmul(Pc[:, 8:16], lhsT=utri[:], rhs=g[:, 16:24], start=True, stop=True), r=[kc, gk], w=[PS.k[pc]])
            op("pe", lambda e: e.matmul(Pc[:, 16:24], lhsT=ones_f[:], rhs=R[:, 0:8], start=True, stop=True), r=[kc, Rk], w=[PS.k[pc]])
            op("pe", lambda e: e.matmul(Pc[:, 24:32], lhsT=ones_f[:], rhs=g[:, 16:24], start=True, stop=True), r=[kc, gk], w=[PS.k[pc]])
            op("dve", lambda e: e.tensor_copy(out=R[:, 32:40], in_=Pc[:, 0:8]), r=[PS.k[pc]], w=[Rk])
            op("dve", lambda e: e.tensor_tensor(out=R[:, 40:48], in0=Pc[:, 0:8], in1=R[:, 8:16], op=ALU.subtract), r=[PS.k[pc], Rk], w=[Rk])
            op("dve", lambda e: e.tensor_scalar(out=g[:, 48:56], in0=R[:, 40:48], scalar1=-1.0, scalar2=None, op0=ALU.mult), r=[Rk], w=[gk])
            op("dve", lambda e: e.tensor_scalar(out=g[:, 56:64], in0=R[:, 32:40], scalar1=-1.0, scalar2=None, op0=ALU.mult), r=[Rk], w=[gk])
            op("act", lambda e: e.activation(out=R[:, 48:56], in_=Pc[:, 0:8], func=AF.Exp), r=[PS.k[pc]], w=[Rk])
            op("dve", lambda e: e.tensor_tensor(out=g[:, 24:32], in0=Pc[:, 16:24], in1=R[:, 32:40], op=ALU.subtract), r=[PS.k[pc], Rk], w=[gk])
            op("act", lambda e: e.activation(out=R[:, 56:64], in_=g[:, 24:32], func=AF.Exp), r=[gk], w=[Rk])
            op("act", lambda e: e.activation(out=g[:, 32:40], in_=Pc[:, 16:24], func=AF.Exp), r=[PS.k[pc]], w=[gk])
            op("dve", lambda e: e.tensor_tensor(out=g[:, 40:48], in0=Pc[:, 8:16], in1=Fcar[:], op=ALU.add), r=[PS.k[pc], k_fcar], w=[gk])
            if own:
                op("dve", lambda e: e.tensor_scalar(out=FK[:, kt, :], in0=g[:, 40:48], scalar1=-1.0, scalar2=None, op0=ALU.mult), r=[gk], w=[k_fk[kt]])
            else:
                op("dve", lambda e: e.tensor_scalar(out=FK[:, kt, :], in0=g[:, 40:48], scalar1=-1.0, scalar2=pm_s[:, 1:2], op0=ALU.mult, op1=ALU.add), r=[gk, k_pm], w=[k_fk[kt]])
            op("dve", lambda e: e.tensor_tensor(out=Fcar[:], in0=Fcar[:], in1=Pc[:, 24:32], op=ALU.add), r=[PS.k[pc], k_fcar], w=[k_fcar])
            PS.rel(pc)
            if own:
                f3, f3k = f3t, k_f3
                r3, r3k = r3t, k_r3
                op("dve", lambda e: e.tensor_copy(out=f3[:, 0:8], in_=g[:, 40:48]), r=[gk], w=[f3k])
                op("dve", lambda e: e.tensor_tensor(out=r3[:, 0:8], in0=g[:, 40:48], in1=f3[:, 0:8], op=ALU.subtract), r=[gk, f3k], w=[r3k])
                op("dve", lambda e: e.tensor_copy(out=f3[:, 8:16], in_=r3[:, 0:8]), r=[r3k], w=[f3k])
                op("dve", lambda e: e.tensor_tensor(out=r3[:, 8:16], in0=r3[:, 0:8], in1=f3[:, 8:16], op=ALU.subtract), r=[r3k, f3k], w=[r3k])
                op("dve", lambda e: e.tensor_copy(out=f3[:, 16:24], in_=r3[:, 8:16]), r=[r3k], w=[f3k])
                pt = PS.get()
                ptv = PS.t[pt][:].bitcast(BF16)
                op("pe", lambda e: e.transpose(out=ptv[0:24, 0:128], in_=f3[:, 0:24], identity=ident_b[:]), r=[f3k, kc], w=[PS.k[pt]])
                op("act", lambda e: e.copy(out=FqT[:, t * 128:(t + 1) * 128], in_=ptv[0:24, 0:128]), r=[PS.k[pt]], w=[k_fq])
                PS.rel(pt)
            gres.append((g, gk, R, Rk))

        chk(3)
        def conv_unit(col0, ch0):
            for j4 in range(4):
                if j4 % 2 == 0:
                    wt, wk = load_unit(col0 + (j4 // 2) * 256)
                j = j4 % 2
                ch = ch0 + j4
                pb = proj_fm(wt, wk, j)
                u, uk, _ = UB.next()
                op("act", lambda e: e.copy(out=u[:, 3:3 + BLK], in_=PS.t[pb][:, 0:BLK]), r=[PS.k[pb]], w=[uk])
                PS.rel(pb)
                op("pool", lambda e: e.tensor_copy(out=u[:, 0:3], in_=hist[:, ch, :]), r=[k_hist[ch]], w=[uk])
                op("pool", lambda e: e.tensor_copy(out=hist[:, ch, :], in_=u[:, BLK:BLK + 3]), r=[uk], w=[k_hist[ch]])
                a, ak, _ = CA.next()
                op("dve", lambda e: e.tensor_scalar(out=a[:], in0=u[:, 0:BLK], scalar1=convw_s[:, ch * 4:ch * 4 + 1], scalar2=None, op0=ALU.mult), r=[uk, k_convw], w=[ak])
                for tp in range(1, 4):
                    op("dve", lambda e: e.scalar_tensor_tensor(out=a[:], in0=u[:, tp:tp + BLK], scalar=convw_s[:, ch * 4 + tp:ch * 4 + tp + 1], in1=a[:], op0=ALU.mult, op1=ALU.add), r=[uk, k_convw, ak], w=[ak])
                silu_from(a[:], [ak], QKV[:, ch, :], [k_qkv[ch]])

        if own:
            conv_unit(C_QA, 0); conv_unit(C_QA + 512, 4)
        conv_unit(C_KA, 8); conv_unit(C_KA + 512, 12)
        conv_unit(C_VA, 16); conv_unit(C_VA + 512, 20)
        if own:
            for q4 in range(4):
                wt, wk = load_unit(C_ZA + q4 * 256)
                for j in range(2):
                    pb = proj_fm(wt, wk, j)
                    silu_from(PS.t[pb][:, 0:BLK], [PS.k[pb]], ZS[:, q4 * 2 + j, :], [k_zs[q4 * 2 + j]])
                    PS.rel(pb)
            for q4 in range(4):
                wt, wk = load_unit(C_QB + q4 * 256)
                for j in range(2):
                    pb = proj_fm(wt, wk, j)
                    hh = q4 * 2 + j
                    op("act", lambda e: e.activation(out=QbT[:, hh, :], in_=PS.t[pb][:, 0:BLK], func=AF.Copy, scale=128.0 ** -0.5), r=[PS.k[pb]], w=[k_qb[hh]])
                    PS.rel(pb)
        for q4 in range(4):
            wt, wk = load_unit(C_KB + q4 * 256)
            for j in range(2):
                pb = proj_fm(wt, wk, j)
                hh = q4 * 2 + j
                op("dve", lambda e: e.tensor_copy(out=KbT[:, hh, :], in_=PS.t[pb][:, 0:BLK]), r=[PS.k[pb]], w=[k_kb])
                PS.rel(pb)
        op("sp", lambda e: e.dma_start(out=kt_d[:, :, tok0:tok0 + BLK].rearrange("h p t -> p h t"), in_=KbT[:]), r=[k_kb], w=[k_ktd[blk]], dma=sem_kb)
        vts = [VbT.next() for _ in range(BLK // 128)]
        for q4 in range(4):
            wt, wk = load_unit(C_VB + q4 * 256)
            for t in range(BLK // 128):
                vt, vk, vd = vts[t]
                pb = PS.get()
                for k in range(KC):
                    op("pe", lambda e: e.matmul(PS.t[pb][:, 0:256], lhsT=hT[:, k, t * 128:(t + 1) * 128], rhs=wt[:, k, :], start=(k == 0), stop=(k == KC - 1)),
                       r=[wk, k_hT], w=[PS.k[pb]], inc=(k == KC - 1))
                if (q4 + t) % 2 == 0:
                    op("act", lambda e: e.copy(out=vt[:, q4 * 256:(q4 + 1) * 256], in_=PS.t[pb][:, 0:256]), r=[PS.k[pb]], w=[vk])
                else:
                    op("dve", lambda e: e.tensor_copy(out=vt[:, q4 * 256:(q4 + 1) * 256], in_=PS.t[pb][:, 0:256]), r=[PS.k[pb]], w=[vk])
                PS.rel(pb)
        for t in range(BLK // 128):
            vt, vk, vd = vts[t]
            op("sp", lambda e: e.dma_start(out=v_d[tok0 + t * 128:tok0 + (t + 1) * 128, :], in_=vt[:]), r=[vk], w=[k_vd[blk]], dma=vd)

        chk(4)
        def l2n(ch, qscale):
            sq, sqk, _ = W16.next()
            op("act", lambda e: e.activation(out=sq[:], in_=QKV[:, ch, :], func=AF.Square), r=[k_qkv[ch]], w=[sqk])
            pb = PS.get()
            op("pe", lambda e: e.matmul(PS.t[pb][:, 0:BLK], lhsT=ones_b[:], rhs=sq[:], start=True, stop=True), r=[kc, sqk], w=[PS.k[pb]])
            rn, rnk, _ = W32.next()
            op("act", lambda e: e.activation(out=rn[:], in_=PS.t[pb][:, 0:BLK], func=AF.Ln, bias=EPS), r=[PS.k[pb]], w=[rnk])
            PS.rel(pb)
            op("act", lambda e: e.activation(out=rn[:], in_=rn[:], func=AF.Exp, scale=-0.5), r=[rnk], w=[rnk])
            op("dve", lambda e: e.scalar_tensor_tensor(out=QKV[:, ch, :], in0=QKV[:, ch, :], scalar=qscale, in1=rn[:], op0=ALU.mult, op1=ALU.mult), r=[k_qkv[ch], rnk], w=[k_qkv[ch]])

        for h in range(H):
            if own:
                l2n(h, 128.0 ** -0.5)
            l2n(8 + h, 1.0)

        def fox_head(h):
            nkt = (blk + 1) * 2
            pO = PS.get(); pR = PS.get()
            pend = []

            def emit_pv(it):
                pT_, pTk, vr_, vrk_, kk_, kt_ = it
                first, last = (kt_ == 0), (kt_ == nkt - 1)
                op("pe", lambda e: e.matmul(PS.t[pO][:, 0:BLK], lhsT=vr_[:, kk_, :], rhs=pT_[:], start=first, stop=last), r=[vrk_, pTk], w=[PS.k[pO]], inc=last)
                op("pe", lambda e: e.matmul(PS.t[pR][:, 0:BLK], lhsT=ones_b[:], rhs=pT_[:], start=first, stop=last), r=[kc, pTk], w=[PS.k[pR]], inc=last)

            for g8 in range(0, nkt, 8):
                n8 = min(8, nkt - g8)
                kr, krk, krd = KR.next()
                vr, vrk, vrd = VR.next()
                bl = list(range(g8 * 128 // BLK, (g8 + n8) * 128 // BLK))
                op("sp", lambda e: e.dma_start(out=kr[:, 0:n8 * 128], in_=kt_d[h, :, g8 * 128:(g8 + n8) * 128]), r=[k_ktd[b] for b in bl], w=[krk], dma=krd)
                op("sp", lambda e: e.dma_start(out=vr[:, 0:n8, :], in_=v_d[g8 * 128:(g8 + n8) * 128, h * 128:(h + 1) * 128].rearrange("(k p) d -> p k d", p=128)), r=[k_vd[b] for b in bl], w=[vrk], dma=vrd)
                for kk in range(n8):
                    kt = g8 + kk
                    pS = PS.get()
                    op("pe", lambda e: e.matmul(PS.t[pS][:, 0:BLK], lhsT=kr[:, kk * 128:(kk + 1) * 128], rhs=QbT[:, h, :], start=True, stop=False), r=[krk, k_qb[h]], w=[PS.k[pS]], inc=False)
                    op("pe", lambda e: e.matmul(PS.t[pS][:, 0:BLK], lhsT=sel[:, h * 128:(h + 1) * 128], rhs=FqT[:], start=False, stop=True), r=[kc, k_fq], w=[PS.k[pS]])
                    pT_, pTk, _ = W16.next()
                    op("act", lambda e: e.activation(out=pT_[:], in_=PS.t[pS][:, 0:BLK], func=AF.Exp, bias=FK[:, kt, h:h + 1]), r=[PS.k[pS], k_fk[kt]], w=[pTk])
                    PS.rel(pS)
                    if kt >= blk * 2:
                        op("pool", lambda e: e.affine_select(out=pT_[:], in_=pT_[:], pattern=[[1, BLK]], compare_op=ALU.is_ge, fill=0.0, base=tok0 - kt * 128, channel_multiplier=-1), r=[pTk], w=[pTk])
                    pend.append((pT_, pTk, vr, vrk, kk, kt))
                    if len(pend) > 2:
                        emit_pv(pend.pop(0))
            while pend:
                emit_pv(pend.pop(0))
            ri, rik, _ = W32.next()
            op("dve", lambda e: e.reciprocal(out=ri[:], in_=PS.t[pR][:, 0:BLK]), r=[PS.k[pR]], w=[rik])
            op("dve", lambda e: e.tensor_tensor(out=mixT[:, 8 + h, :], in0=PS.t[pO][:, 0:BLK], in1=ri[:], op=ALU.mult), r=[PS.k[pO], rik, k_hT], w=[k_mix[8 + h]])
            PS.rel(pO); PS.rel(pR)


        fox_q = list(range(H)) if own else []

        def pump():
            if fox_q:
                fox_head(fox_q.pop(0))

        chk(41)
        for t in range(BLK // 128):
            g, gk, R, Rk = gres[t]
            ts_ = slice(t * 128, (t + 1) * 128)
            for hg in range(H // NSLOT):
                heads = [hg * NSLOT + i for i in range(NSLOT)]
                for hs, h in enumerate(heads):
                    sl = SL[hs]
                    knT = QKV[:, 8 + h, ts_]; kkn = k_qkv[8 + h]
                    gc_col = R[:, 32 + h:33 + h]; gcb_col = R[:, 40 + h:41 + h]
                    d1, d1k, _ = T32.next(); d2, d2k, _ = T32.next()
                    op("dve", lambda e: e.tensor_scalar(out=d1[:], in0=utri[:], scalar1=R[:, h:h + 1], scalar2=None, op0=ALU.mult), r=[kc, Rk], w=[d1k])
                    op("dve", lambda e: e.scalar_tensor_tensor(out=d2[:], in0=ident_f[:], scalar=R[:, 8 + h:9 + h], in1=d1[:], op0=ALU.mult, op1=ALU.subtract), r=[kc, Rk, d1k], w=[d2k])
                    pG = PS.get(); PG = PS.t[pG]
                    op("pe", lambda e: e.matmul(PG[:, 0:128], lhsT=ones_f[:], rhs=d1[:], start=True, stop=True), r=[kc, d1k], w=[PS.k[pG]])
                    op("pe", lambda e: e.matmul(PG[:, 128:256], lhsT=ones_f[:], rhs=d2[:], start=True, stop=True), r=[kc, d2k], w=[PS.k[pG]])
                    pK = PS.get(); PK = PS.t[pK]
                    op("pe", lambda e: e.matmul(PK[:, 0:128], lhsT=knT, rhs=knT, start=True, stop=True), r=[kkn], w=[PS.k[pK]])
                    if own:
                        op("pe", lambda e: e.matmul(PK[:, 128:256], lhsT=knT, rhs=QKV[:, h, ts_], start=True, stop=True), r=[kkn, k_qkv[h]], w=[PS.k[pK]])
                    KmL, KmLk = sl["Ul"]; KmU, KmUk = sl["W1s"]
                    op("dve", lambda e: e.tensor_tensor(out=KmL[:], in0=PK[:, 0:128], in1=m_posL[:], op=ALU.mult), r=[PS.k[pK], kc], w=[KmLk])
                    op("dve", lambda e: e.tensor_tensor(out=KmU[:], in0=PK[:, 0:128], in1=m_negUs[:], op=ALU.mult), r=[PS.k[pK], kc], w=[KmUk])
                    e1, e1k, _ = T32.next()
                    op("act", lambda e: e.activation(out=e1[:], in_=PG[:, 128:256], func=AF.Exp, bias=gc_col), r=[PS.k[pG], Rk], w=[e1k])
                    e2, e2k, _ = T32.next()
                    op("act", lambda e: e.activation(out=e2[:], in_=PG[:, 0:128], func=AF.Exp, bias=g[:, 48 + h:49 + h]), r=[PS.k[pG], gk], w=[e2k])
                    X, Xk = sl["X"]
                    op("dve", lambda e: e.scalar_tensor_tensor(out=X[:], in0=e1[:], scalar=1.0, in1=KmL[:], op0=ALU.min, op1=ALU.mult), r=[e1k, KmLk], w=[Xk])
                    Y, Yk = sl["Y"]
                    op("dve", lambda e: e.scalar_tensor_tensor(out=Y[:], in0=e2[:], scalar=1.0, in1=KmU[:], op0=ALU.min, op1=ALU.mult), r=[e2k, KmUk], w=[Yk])
                    if own:
                        QKm, QKmk = sl["ke"]
                        op("dve", lambda e: e.tensor_tensor(out=QKm[:], in0=PK[:, 128:256], in1=m_negUi[:], op=ALU.mult), r=[PS.k[pK], kc], w=[QKmk])
                        e3, e3k, _ = T32.next()
                        op("act", lambda e: e.activation(out=e3[:], in_=PG[:, 0:128], func=AF.Exp, bias=g[:, 56 + h:57 + h]), r=[PS.k[pG], gk], w=[e3k])
                        qkT, qkk = sl["qkT"]
                        op("dve", lambda e: e.scalar_tensor_tensor(out=qkT[:], in0=e3[:], scalar=1.0, in1=QKm[:], op0=ALU.min, op1=ALU.mult), r=[e3k, QKmk], w=[qkk])
                        e4, e4k, _ = T32.next()
                        op("act", lambda e: e.activation(out=e4[:], in_=PG[:, 0:128], func=AF.Exp), r=[PS.k[pG]], w=[e4k])
                        qdT, qdk = sl["qdT"]
                        op("dve", lambda e: e.tensor_tensor(out=qdT[:], in0=e4[:], in1=QKV[:, h, ts_], op=ALU.mult), r=[e4k, k_qkv[h]], w=[qdk])
                    PS.rel(pG); PS.rel(pK)
                    pump()
                chk(42)
                for hs, h in enumerate(heads):
                    sl = SL[hs]
                    X, Xk = sl["X"]; Y, Yk = sl["Y"]
                    m0, m0k = sl["Ul"]; m1, m1k = sl["W1s"]
                    Tm, Tmk = sl["TmA"]; Tt, Ttk = sl["TtA"]
                    op("pool", lambda e: e.tensor_tensor(out=m0[:], in0=X[:], in1=LM[:, 0, :], op=ALU.mult), r=[Xk, k_lm], w=[m0k])
                    op("dve", lambda e: e.tensor_tensor(out=Tm[:], in0=m0[:], in1=ident_f[:], op=ALU.add), r=[m0k, kc], w=[Tmk])
                    op("pool", lambda e: e.tensor_tensor(out=m1[:], in0=Y[:], in1=LM[:, 1, :], op=ALU.mult), r=[Yk, k_lm], w=[m1k])
                    op("dve", lambda e: e.tensor_tensor(out=Tt[:], in0=m1[:], in1=ident_f[:], op=ALU.add), r=[m1k, kc], w=[Ttk])
                cur = "A"
                for lvl in range(1, 7):
                    nxt = "B" if cur == "A" else "A"
                    for hs, h in enumerate(heads):
                        sl = SL[hs]
                        Y, Yk = sl["Y"]; Ul, Ulk = sl["Ul"]; W1s, W1k = sl["W1s"]; Tm, Tmk = sl["Tm" + cur]
                        op("pool", lambda e: e.tensor_tensor(out=Ul[:], in0=Y[:], in1=LM[:, 1 + lvl, :], op=ALU.mult), r=[Yk, k_lm], w=[Ulk])
                        pI = PS.get()
                        op("pe", lambda e: e.matmul(PS.t[pI][:, 0:128], lhsT=Ul[:], rhs=Tm[:], start=True, stop=True), r=[Ulk, Tmk], w=[PS.k[pI]])
                        op("act", lambda e: e.copy(out=W1s[:], in_=PS.t[pI][:, 0:128]), r=[PS.k[pI]], w=[W1k])
                        PS.rel(pI)
                    for hs, h in enumerate(heads):
                        sl = SL[hs]
                        W1s, W1k = sl["W1s"]; Tm, Tmk = sl["Tm" + cur]; Tt, Ttk = sl["Tt" + cur]; Tn, Tnk = sl["Tm" + nxt]
                        pJ = PS.get()
                        op("pe", lambda e: e.matmul(PS.t[pJ][:, 0:128], lhsT=Tt[:], rhs=W1s[:], start=True, stop=True), r=[Ttk, W1k], w=[PS.k[pJ]])
                        op("dve", lambda e: e.tensor_tensor(out=Tn[:], in0=PS.t[pJ][:, 0:128], in1=Tm[:], op=ALU.add), r=[PS.k[pJ], Tmk], w=[Tnk])
                        PS.rel(pJ)
                    for hs, h in enumerate(heads):
                        sl = SL[hs]
                        Tn, Tnk = sl["Tm" + nxt]; Ttn, Ttnk = sl["Tt" + nxt]
                        pL = PS.get()
                        PLb = PS.t[pL][:].bitcast(BF16)
                        op("pe", lambda e: e.transpose(out=PLb[:, 0:128], in_=Tn[:], identity=ident_b[:]), r=[Tnk, kc], w=[PS.k[pL]])
                        op("act", lambda e: e.copy(out=Ttn[:], in_=PLb[:, 0:128]), r=[PS.k[pL]], w=[Ttnk])
                        PS.rel(pL)
                    cur = nxt
                chk(43)
                for hs, h in enumerate(heads):
                    sl = SL[hs]
                    knT = QKV[:, 8 + h, ts_]; kkn = k_qkv[8 + h]
                    vT = QKV[:, 16 + h, ts_]; kvn = k_qkv[16 + h]
                    ke, kek = sl["ke"]; kd, kdk = sl["kd"]; vtk, vtkk = sl["vtk"]
                    pT = PS.get()
                    PTb = PS.t[pT][:].bitcast(BF16)
                    op("pe", lambda e: e.transpose(out=PTb[:, 0:128], in_=knT, identity=ident_b[:]), r=[kkn, kc], w=[PS.k[pT]])
                    op("dve", lambda e: e.tensor_scalar(out=ke[:], in0=PTb[:, 0:128], scalar1=R[:, 48 + h:49 + h], scalar2=None, op0=ALU.mult), r=[PS.k[pT], Rk], w=[kek])
                    op("dve", lambda e: e.tensor_scalar(out=kd[:], in0=PTb[:, 0:128], scalar1=R[:, 56 + h:57 + h], scalar2=None, op0=ALU.mult), r=[PS.k[pT], Rk], w=[kdk])
                    PS.rel(pT)
                    pT2 = PS.get()
                    PTb2 = PS.t[pT2][:].bitcast(BF16)
                    op("pe", lambda e: e.transpose(out=PTb2[:, 0:128], in_=vT, identity=ident_b[:]), r=[kvn, kc], w=[PS.k[pT2]])
                    op("act", lambda e: e.copy(out=vtk[:], in_=PTb2[:, 0:128]), r=[PS.k[pT2]], w=[vtkk])
                    PS.rel(pT2)
                chk(44)
                for hs, h in enumerate(heads):
                    sl = SL[hs]
                    Rm, Rmk = sl["Tt" + cur]
                    ke, kek = sl["ke"]; vtk, vtkk = sl["vtk"]; u2, u2k = sl["u2"]; wT, wTk = sl["wT"]
                    pU = PS.get()
                    op("pe", lambda e: e.matmul(PS.t[pU][:, 0:128], lhsT=Rm[:], rhs=vtk[:], start=True, stop=True), r=[Rmk, vtkk], w=[PS.k[pU]])
                    op("act", lambda e: e.activation(out=u2[:], in_=PS.t[pU][:, 0:128], func=AF.Identity, scale=R[:, 16 + h:17 + h]), r=[PS.k[pU], Rk], w=[u2k])
                    PS.rel(pU)
                    pU2 = PS.get()
                    op("pe", lambda e: e.matmul(PS.t[pU2][:, 0:128], lhsT=ke[:], rhs=Rm[:], start=True, stop=True), r=[Rmk, kek], w=[PS.k[pU2]])
                    op("dve", lambda e: e.tensor_copy(out=wT[:], in_=PS.t[pU2][:, 0:128]), r=[PS.k[pU2]], w=[wTk])
                    PS.rel(pU2)
                chk(45)
                for hs, h in enumerate(heads):
                    sl = SL[hs]
                    wT, wTk = sl["wT"]; vn, vnk = sl["vn"]; u2, u2k = sl["u2"]
                    pS1 = PS.get()
                    op("pe", lambda e: e.matmul(PS.t[pS1][:, 0:128], lhsT=wT[:], rhs=Sbf[:, h, :], start=True, stop=True), r=[wTk, k_Sb[h]], w=[PS.k[pS1]])
                    op("dve", lambda e: e.scalar_tensor_tensor(out=vn[:], in0=PS.t[pS1][:, 0:128], scalar=R[:, 24 + h:25 + h], in1=u2[:], op0=ALU.mult, op1=ALU.add), r=[PS.k[pS1], Rk, u2k], w=[vnk])
                    PS.rel(pS1)
                for hs, h in enumerate(heads):
                    sl = SL[hs]
                    vn, vnk = sl["vn"]; kd, kdk = sl["kd"]
                    if own:
                        qdT, qdk = sl["qdT"]; qkT, qkk = sl["qkT"]
                        pS2 = PS.get()
                        op("pe", lambda e: e.matmul(PS.t[pS2][:, 0:128], lhsT=Sbf[:, h, :], rhs=qdT[:], start=True, stop=False), r=[k_Sb[h], qdk], w=[PS.k[pS2]], inc=False)
                        op("pe", lambda e: e.matmul(PS.t[pS2][:, 0:128], lhsT=vn[:], rhs=qkT[:], start=False, stop=True), r=[vnk, qkk], w=[PS.k[pS2]])
                        op("act", lambda e: e.copy(out=OTB[:, h, ts_], in_=PS.t[pS2][:, 0:128]), r=[PS.k[pS2]], w=[k_ot[h]])
                        PS.rel(pS2)
                    pS3 = PS.get()
                    op("pe", lambda e: e.matmul(PS.t[pS3][:, 0:128], lhsT=kd[:], rhs=vn[:], start=True, stop=True), r=[kdk, vnk], w=[PS.k[pS3]])
                    op("dve", lambda e: e.scalar_tensor_tensor(out=Sst[:, h, :], in0=Sst[:, h, :], scalar=g[:, 32 + h:33 + h], in1=PS.t[pS3][:, 0:128], op0=ALU.mult, op1=ALU.add), r=[k_S[h], gk, PS.k[pS3]], w=[k_S[h]])
                    op("act", lambda e: e.copy(out=Sbf[:, h, :], in_=Sst[:, h, :]), r=[k_S[h]], w=[k_Sb[h]])
                    PS.rel(pS3)
                chk(46)
        if own:
            for h in range(H):
                sq, sqk, _ = W16.next()
                op("act", lambda e: e.activation(out=sq[:], in_=OTB[:, h, :], func=AF.Square), r=[k_ot[h]], w=[sqk])
                pb = PS.get()
                op("pe", lambda e: e.matmul(PS.t[pb][:, 0:BLK], lhsT=ones_b[:], rhs=sq[:], start=True, stop=True), r=[kc, sqk], w=[PS.k[pb]])
                rn, rnk, _ = W32.next()
                op("act", lambda e: e.activation(out=rn[:], in_=PS.t[pb][:, 0:BLK], func=AF.Ln, scale=1.0 / 128, bias=EPS), r=[PS.k[pb]], w=[rnk])
                PS.rel(pb)
                op("act", lambda e: e.activation(out=rn[:], in_=rn[:], func=AF.Exp, scale=-0.5), r=[rnk], w=[rnk])
                op("dve", lambda e: e.scalar_tensor_tensor(out=rn[:], in0=OTB[:, h, :], scalar=ong_s[:, 0:1], in1=rn[:], op0=ALU.mult, op1=ALU.mult), r=[k_ot[h], k_ong, rnk], w=[rnk])
                op("dve", lambda e: e.tensor_tensor(out=mixT[:, h, :], in0=rn[:], in1=ZS[:, h, :], op=ALU.mult), r=[rnk, k_zs[h]], w=[k_mix[h]])

        if dbg and blk == NB - 1:
            dbg_dump("qkv", QKV[:, :, :], k_qkv)

        chk(5)
        if own:
            chk(6)
        if own:
            while fox_q:
                pump()

            if dbg and blk == NBP:
                dbg_dump("mixT", mixT[:, :, :].rearrange("p c t -> p (c t)"), k_mix)

            chk(7)
            ob = blk - NBP
            for dblk in range(8):
                wt, wk = load_unit(dblk * 256, src=w_out)
                c0_, c1_ = dblk * 256, (dblk + 1) * 256
                for t in range(BLK // 128):
                    r0 = ob * BLK + t * 128
                    xpi, xpk, xpd = XP.next()
                    op("sp", lambda e: e.dma_start(out=xpi[:, 0:256], in_=xo[r0:r0 + 128, c0_:c1_]), w=[xpk], dma=xpd)
                    pb = PS.get()
                    for m in range(KC):
                        op("pe", lambda e: e.matmul(PS.t[pb][:, 0:256], lhsT=mixT[:, m, t * 128:(t + 1) * 128], rhs=wt[:, m, :], start=(m == 0), stop=(m == KC - 1)),
                           r=[k_mix[m], wk], w=[PS.k[pb]], inc=(m == KC - 1))
                    x1p, x1k, x1d = X1P.next()
                    op("dve", lambda e: e.tensor_tensor(out=x1p[:, 0:256], in0=PS.t[pb][:, 0:256], in1=G1B[:, c0_:c1_], op=ALU.mult), r=[PS.k[pb], k_G], w=[x1k])
                    PS.rel(pb)
                    op("pool", lambda e: e.tensor_tensor(out=x1p[:, 0:256], in0=x1p[:, 0:256], in1=xpi[:, 0:256], op=ALU.add), r=[x1k, xpk], w=[x1k])
                    op("sp", lambda e: e.dma_start(out=x1_d[r0:r0 + 128, c0_:c1_], in_=x1p[:, 0:256]), r=[x1k], w=[k_x1d[ob * 2 + t]], dma=x1d)
            chk(8)
            norm_to_T(x1_d[ob * BLK:(ob + 1) * BLK, :], h2T, k_h2T, A2, B2, x_from=[k_x1d[ob * 2], k_x1d[ob * 2 + 1]])
            op("sp", lambda e: e.dma_start(out=h2_d[:, :, ob * BLK:(ob + 1) * BLK].rearrange("c p t -> p c t"), in_=h2T[:]), r=[k_h2T], w=[k_h2d[ob]], dma=sem_h2)
            for t in range(BLK // 128):
                ti = ob * 2 + t
                pb = PS.get()
                for k in range(KC):
                    op("pe", lambda e: e.matmul(PS.t[pb][:, 0:36], lhsT=h2T[:, k, t * 128:(t + 1) * 128], rhs=wr_s[:, k, :], start=(k == 0), stop=(k == KC - 1)),
                       r=[k_h2T, k_wr], w=[PS.k[pb]], inc=(k == KC - 1))
                rt, rtk, _ = RT.next()
                op("dve", lambda e: e.tensor_tensor(out=rt[:, 0:36], in0=PS.t[pb][:, 0:36], in1=br_s[:], op=ALU.add), r=[PS.k[pb], k_br], w=[rtk])
                PS.rel(pb)
                op("dve", lambda e: e.tensor_reduce(out=rt[:, 40:41], in_=rt[:, 0:4], axis=AX.X, op=ALU.max), r=[rtk], w=[rtk])
                op("dve", lambda e: e.tensor_scalar(out=rt[:, 36:40], in0=rt[:, 0:4], scalar1=rt[:, 40:41], scalar2=None, op0=ALU.is_ge), r=[rtk], w=[rtk])
                op("dve", lambda e: e.tensor_scalar(out=rt[:, 41:42], in0=rt[:, 40:41], scalar1=-1.0, scalar2=None, op0=ALU.mult), r=[rtk], w=[rtk])
                op("act", lambda e: e.activation(out=rt[:, 44:48], in_=rt[:, 0:4], func=AF.Exp, bias=rt[:, 41:42], accum_out=rt[:, 42:43]), r=[rtk], w=[rtk])
                op("dve", lambda e: e.reciprocal(out=rt[:, 43:44], in_=rt[:, 42:43]), r=[rtk], w=[rtk])
                op("dve", lambda e: e.tensor_scalar(out=rt[:, 48:56], in0=rt[:, 4:12], scalar1=rt[:, 36:37], scalar2=None, op0=ALU.mult), r=[rtk], w=[rtk])
                for gi in range(1, 4):
                    op("dve", lambda e: e.scalar_tensor_tensor(out=rt[:, 48:56], in0=rt[:, 4 + gi * 8:12 + gi * 8], scalar=rt[:, 36 + gi:37 + gi], in1=rt[:, 48:56], op0=ALU.mult, op1=ALU.add), r=[rtk], w=[rtk])
                op("dve", lambda e: e.max(out=rt[:, 56:64], in_=rt[:, 48:56]), r=[rtk], w=[rtk])
                rt2, rt2k, _ = RT.next()
                op("dve", lambda e: e.tensor_scalar(out=rt2[:, 0:8], in0=rt[:, 48:56], scalar1=rt[:, 57:58], scalar2=None, op0=ALU.is_ge), r=[rtk], w=[rt2k])
                op("dve", lambda e: e.tensor_scalar(out=rt2[:, 8:9], in0=rt[:, 56:57], scalar1=-1.0, scalar2=None, op0=ALU.mult), r=[rtk], w=[rt2k])
                op("act", lambda e: e.activation(out=rt2[:, 16:24], in_=rt[:, 48:56], func=AF.Exp, bias=rt2[:, 8:9]), r=[rtk, rt2k], w=[rt2k])
                op("dve", lambda e: e.tensor_tensor(out=rt2[:, 16:24], in0=rt2[:, 16:24], in1=rt2[:, 0:8], op=ALU.mult), r=[rt2k], w=[rt2k])
                op("dve", lambda e: e.tensor_reduce(out=rt2[:, 9:10], in_=rt2[:, 16:24], axis=AX.X, op=ALU.add), r=[rt2k], w=[rt2k])
                op("dve", lambda e: e.reciprocal(out=rt2[:, 10:11], in_=rt2[:, 9:10]), r=[rt2k], w=[rt2k])
                op("dve", lambda e: e.tensor_tensor(out=rt2[:, 10:11], in0=rt2[:, 10:11], in1=rt[:, 43:44], op=ALU.mult), r=[rt2k, rtk], w=[rt2k])
                op("dve", lambda e: e.tensor_scalar(out=rt2[:, 16:24], in0=rt2[:, 16:24], scalar1=rt2[:, 10:11], scalar2=None, op0=ALU.mult), r=[rt2k], w=[rt2k])
                for gi in range(4):
                    op("dve", lambda e: e.tensor_scalar(out=CW[:, ti, gi * 8:(gi + 1) * 8], in0=rt2[:, 16:24], scalar1=rt[:, 36 + gi:37 + gi], scalar2=None, op0=ALU.mult), r=[rt2k, rtk], w=[k_cw[ti]])

    if dbg:
        dbg_dump("cw", CW[:, :, :], k_cw)
        dbg_dump("x1", x1_d, k_x1d)

    chk(9)
    S.barrier()
    stack_B.close()
    _STK[0] = stack_main
    fgb_s, k_fgb = load_small("fgb_s", fgb, [128, D])
    passes = []
    PT_MAX = min(4, NTO)
    passes = [(t0, min(t0 + PT_MAX, NTO)) for t0 in range(0, NTO, PT_MAX)]
    h2p = sb("h2p", [128, KC, PT_MAX * 128], BF16); k_h2p = Trk(); sem_h2p = S.newsem("d_h2p")
    acc = sb("acc", [128, PT_MAX, D]); k_acc = [Trk() for _ in range(PT_MAX)]
    HID = Ring(S, nc, "hid", [128, 4, PT_MAX * 128], BF16, 2)
    SG = Ring(S, nc, "sg", [128, 512], F32, 3)
    W2R = Ring(S, nc, "w2r", [128, 4, D], BF16, 2, dma=True)
    WRC = Ring(S, nc, "wrc", [128, KC, 512], BF16, 3, dma=True)
    X1L = Ring(S, nc, "x1l", [128, D], F32, 1, dma=True)
    OUTR = Ring(S, nc, "outr", [128, D], F32, 1, dma=True)
    for (t0, t1) in passes:
        ntl = t1 - t0
        ntok = ntl * 128
        op("sp", lambda e: e.dma_start(out=h2p[:, :, 0:ntok], in_=h2_d[:, :, t0 * 128:t1 * 128].rearrange("c p t -> p c t")), r=k_h2d, w=[k_h2p], dma=sem_h2p)
        for tl in range(ntl):
            op("pool", lambda e: e.memset(acc[:, tl, :], 0.0), w=[k_acc[tl]])
        cbs = [(c0, min(c0 + 512, ntok)) for c0 in range(0, ntok, 512)]
        for ex in range(NE):
            w1t, w1k, w1d = WRC.next()
            op("pool", lambda e: e.dma_start(out=w1t[:], in_=w1[ex].rearrange("(c p) f -> p c f", p=128)), w=[w1k], dma=w1d)
            w3t, w3k, w3d = WRC.next()
            op("pool", lambda e: e.dma_start(out=w3t[:], in_=w3[ex].rearrange("(c p) f -> p c f", p=128)), w=[w3k], dma=w3d)
            w2t, w2k, w2d = W2R.next()
            op("pool", lambda e: e.dma_start(out=w2t[:], in_=w2[ex].rearrange("(c p) f -> p c f", p=128)), w=[w2k], dma=w2d)
            hid, hidk, _ = HID.next()
            for fc in range(4):
                for (c0, c1) in cbs:
                    n = c1 - c0
                    pa = PS.get(); pb = PS.get()
                    for k in range(KC):
                        op("pe", lambda e: e.matmul(PS.t[pa][:, 0:n], lhsT=w1t[:, k, fc * 128:(fc + 1) * 128], rhs=h2p[:, k, c0:c1], start=(k == 0), stop=(k == KC - 1)),
                           r=[w1k, k_h2p], w=[PS.k[pa]], inc=(k == KC - 1))
                    for k in range(KC):
                        op("pe", lambda e: e.matmul(PS.t[pb][:, 0:n], lhsT=w3t[:, k, fc * 128:(fc + 1) * 128], rhs=h2p[:, k, c0:c1], start=(k == 0), stop=(k == KC - 1)),
                           r=[w3k, k_h2p], w=[PS.k[pb]], inc=(k == KC - 1))
                    sg, sgk, _ = SG.next()
                    op("act", lambda e: e.activation(out=sg[:, 0:n], in_=PS.t[pa][:, 0:n], func=AF.Silu), r=[PS.k[pa]], w=[sgk])
                    PS.rel(pa)
                    op("dve", lambda e: e.tensor_tensor(out=hid[:, fc, c0:c1], in0=sg[:, 0:n], in1=PS.t[pb][:, 0:n], op=ALU.mult), r=[sgk, PS.k[pb]], w=[hidk])
                    PS.rel(pb)
            for tl in range(ntl):
                ti = t0 + tl
                for dblk in range(4):
                    pb = PS.get()
                    for fc in range(4):
                        op("pe", lambda e: e.matmul(PS.t[pb][:], lhsT=hid[:, fc, tl * 128:(tl + 1) * 128], rhs=w2t[:, fc, dblk * 512:(dblk + 1) * 512], start=(fc == 0), stop=(fc == 3)),
                           r=[hidk, w2k], w=[PS.k[pb]], inc=(fc == 3))
                    op("dve", lambda e: e.scalar_tensor_tensor(out=acc[:, tl, dblk * 512:(dblk + 1) * 512], in0=PS.t[pb][:], scalar=CW[:, ti, ex:ex + 1], in1=acc[:, tl, dblk * 512:(dblk + 1) * 512], op0=ALU.mult, op1=ALU.add),
                       r=[PS.k[pb], k_cw[ti], k_acc[tl]], w=[k_acc[tl]])
                    PS.rel(pb)
        for tl in range(ntl):
            ti = t0 + tl
            x1t, x1k, x1dd = X1L.next()
            op("sp", lambda e: e.dma_start(out=x1t[:], in_=x1_d[ti * 128:(ti + 1) * 128, :]), r=[k_x1d[ti]], w=[x1k], dma=x1dd)
            op("pool", lambda e: e.tensor_tensor(out=acc[:, tl, :], in0=acc[:, tl, :], in1=G2B[:], op=ALU.mult), r=[k_acc[tl], k_G], w=[k_acc[tl]])
            op("dve", lambda e: e.tensor_tensor(out=x1t[:], in0=x1t[:], in1=acc[:, tl, :], op=ALU.add), r=[x1k, k_acc[tl]], w=[x1k])
            st, sk, _ = stat.next()
            ot_, otk_, otd = OUTR.next()
            op("act", lambda e: e.activation(out=ot_[:], in_=x1t[:], func=AF.Square, accum_out=st[:, 0:1]), r=[x1k], w=[otk_, sk])
            rstd_from_ssq(st, sk, 0, 1, D)
            op("dve", lambda e: e.scalar_tensor_tensor(out=ot_[:], in0=x1t[:], scalar=st[:, 1:2], in1=fgb_s[:], op0=ALU.mult, op1=ALU.mult), r=[x1k, sk, k_fgb], w=[otk_])
            op("sp", lambda e: e.dma_start(out=out[ti * 128:(ti + 1) * 128, :], in_=ot_[:]), r=[otk_], dma=otd)
    finalize()
    return nc


def _fm(v):
    v = np.asarray(v, np.float32)
    return np.ascontiguousarray(v.reshape(-1, 128).T)


def _rep(v):
    v = np.asarray(v, np.float32).reshape(1, -1)
    return np.ascontiguousarray(np.broadcast_to(v, (128, v.shape[1])))


def _level_masks():
    i = np.arange(128)[:, None]
    j = np.arange(128)[None, :]
    ms = []
    for l in range(7):
        s_ = 1 << l
        lmx = ((i // (2 * s_) == j // (2 * s_)) & (i % (2 * s_) >= s_) & (j % (2 * s_) < s_)).astype(np.float32)
        if l == 0:
            ms.append(lmx)
        ms.append(np.ascontiguousarray(lmx.T))
    return np.ascontiguousarray(np.concatenate(ms, axis=1))


def make_in_maps(inputs, NP, NO):
    x = np.asarray(inputs["x"], np.float32)
    B = x.shape[0]
    f = lambda k: np.asarray(inputs[k], np.float32)
    shared = {
        "w_ada": np.ascontiguousarray(f("w_ada")[0]),
        "b_adaT": _fm(f("b_ada")[0]),
        "n1g": _fm(f("norm1_g")[0]),
        "w_in": np.ascontiguousarray(f("w_in")[0]),
        "convw": np.ascontiguousarray(f("conv_w")[0].T.reshape(24, 128, 4).transpose(1, 0, 2).reshape(128, 96)),
        "alog": _rep(f("a_log")[0]), "dtb": _rep(f("dt_bias")[0]), "ong": _fm(f("dn_onorm_g")[0]),
        "ffb": _rep(f("fox_f_bias")[0]),
        "w_out": np.ascontiguousarray(f("w_out")[0]),
        "n2g": _fm(f("norm2_g")[0]),
        "w_r": np.ascontiguousarray(np.concatenate([f("w_router_group")[0], f("w_router_expert")[0]], axis=1)),
        "b_r": _rep(np.concatenate([f("b_router_group")[0], f("b_router_expert")[0]])),
        "w1": np.ascontiguousarray(f("w1")[0].reshape(NE, D, 512)),
        "w3": np.ascontiguousarray(f("w3")[0].reshape(NE, D, 512)),
        "w2": np.ascontiguousarray(f("w2")[0].reshape(NE, 512, D)),
        "fgb": _rep(f("final_g")),
        "lvlm": _level_masks(),
    }
    maps = []
    for b in range(B):
        for s in range(2):
            m = dict(shared)
            m["xp"] = np.ascontiguousarray(x[b, 0:NP])
            m["xo"] = np.ascontiguousarray(x[b, s * NP:s * NP + NO])
            m["cT"] = _fm(f("c")[b])
            pm = np.zeros((128, 2), np.float32)
            pm[:, 0] = float(s)
            pm[:, 1] = 0.0 if s == 1 else -30000.0
            m["pmv"] = pm
            maps.append(m)
    return maps


_NC_CACHE = {}


def kernel(**inputs):
    NP = NO = 2048
    if "nc" not in _NC_CACHE:
        _NC_CACHE["nc"] = build(NP, NO)
    nc = _NC_CACHE["nc"]
    maps = make_in_maps(inputs, NP, NO)
    res = run_bass_kernel_spmd(nc, maps, core_ids=list(range(8)))
    B = 4
    outp = np.zeros((B, 2 * NO, D), np.float32)
    for b in range(B):
        for s in range(2):
            outp[b, s * NO:(s + 1) * NO] = res.results[b * 2 + s]["out"]
    return outp
```
